# Optimizing a Trainium2 kernel written in Bass

```python
import math
import jax
import jax.numpy as jnp
from jax import lax
import numpy as np

D_MODEL = 1024
BATCH = 8
SEQ = 4096
DEPTH = 2

GRID_W = 64
CTX_LEN = 256
EPS = 1e-6

SHORT_CONV = 4
CONV_LEFT = 2

LRU_HEADS = 4
LRU_HEAD_DIM = 64
LRU_WIDTH = LRU_HEADS * LRU_HEAD_DIM
LRU_C = 8.0

SSD_HEADS = 4
SSD_HEAD_DIM = 64
SSD_WIDTH = SSD_HEADS * SSD_HEAD_DIM
SSD_GROUPS = 2
SSD_STATE = 128
SSD_CHUNK = 128
SSD_CONV_DIM = SSD_WIDTH + 2 * SSD_GROUPS * SSD_STATE

DA_HEADS = 4
DA_QK_DIM = 64
DA_V_DIM = 2 * DA_QK_DIM
DA_QK_WIDTH = DA_HEADS * 2 * DA_QK_DIM
DA_WIDTH = DA_HEADS * DA_V_DIM
DA_SCALE = DA_QK_DIM ** -0.5
Q_BLOCK = 128
ROPE_BASE = 10000.0

D_MIX = LRU_WIDTH + SSD_WIDTH + DA_WIDTH

OFF_LRU_X = LRU_WIDTH
OFF_SSD_Z = 2 * LRU_WIDTH
OFF_SSD_XBC = OFF_SSD_Z + SSD_WIDTH
OFF_SSD_DT = OFF_SSD_XBC + SSD_CONV_DIM
OFF_DA_Q = OFF_SSD_DT + 2 * SSD_HEADS
OFF_DA_K = OFF_DA_Q + DA_QK_WIDTH
OFF_DA_V = OFF_DA_K + DA_QK_WIDTH
D_IN = OFF_DA_V + DA_WIDTH
IN_SPLITS = (OFF_LRU_X, OFF_SSD_Z, OFF_SSD_XBC, OFF_SSD_DT, OFF_DA_Q, OFF_DA_K, OFF_DA_V)

N_EXPERTS = 32
TOP_K = 4
D_EXPERT = 1024
SWIGLU_ALPHA = 1.702
SWIGLU_LIMIT = 7.0
MOE_BLOCK = 256

kernel_name = 'hymba_lru_ssd_diffattn_moe_dit'


def rmsnorm(x, g):
    xf = x.astype(jnp.float32)
    y = xf * lax.rsqrt(jnp.mean(xf * xf, axis=-1, keepdims=True) + EPS)
    return (y * g.astype(jnp.float32)).astype(x.dtype)


def modulate(h, shift, scale):
    return h * (1.0 + scale) + shift


def dwconv(x, w, b):
    k = w.shape[0]
    y = lax.conv_general_dilated(x, w[:, None, :].astype(x.dtype), window_strides=(1,),
                                 padding=[(CONV_LEFT, k - 1 - CONV_LEFT)],
                                 dimension_numbers=('NWC', 'WIO', 'NWC'),
                                 feature_group_count=x.shape[-1])
    return y + b.astype(x.dtype)


def rglru_coeffs(xc, wa, ba, wx, bx, lam):
    b, l, w = xc.shape
    xh = xc.reshape(b, l, LRU_HEADS, LRU_HEAD_DIM)
    r = jax.nn.sigmoid(jnp.einsum('blhi,hij->blhj', xh, wa.astype(jnp.float32)).reshape(b, l, w) + ba.astype(jnp.float32))
    i = jax.nn.sigmoid(jnp.einsum('blhi,hij->blhj', xh, wx.astype(jnp.float32)).reshape(b, l, w) + bx.astype(jnp.float32))
    log_a = -LRU_C * r * jax.nn.softplus(-lam.astype(jnp.float32))
    a = jnp.exp(log_a)
    u = jnp.sqrt(-jnp.expm1(2.0 * log_a)) * (i * xc)
    return a, u


def _lin_combine(lhs, rhs):
    a1, b1 = lhs
    a2, b2 = rhs
    return a1 * a2, a2 * b1 + b2


def linear_scan(a, u, h0, reverse):
    edge = -1 if reverse else 0
    u = u.at[:, edge].add(a[:, edge] * h0)
    _, h = lax.associative_scan(_lin_combine, (a, u), reverse=reverse, axis=1)
    return h


def lru_mix(gate_in, x_in, conv_w, conv_b, wa, ba, wx, bx, lam, h0f, h0b, need_y):
    xc = dwconv(x_in, conv_w, conv_b).astype(jnp.float32)
    af, uf = rglru_coeffs(xc, wa[0], ba[0], wx[0], bx[0], lam[0])
    ab, ub = rglru_coeffs(xc, wa[1], ba[1], wx[1], bx[1], lam[1])
    hf = linear_scan(af, uf, h0f, False)
    hb = linear_scan(ab, ub, h0b, True)
    if not need_y:
        return None, hf[:, -1], hb[:, 0]
    y = jax.nn.gelu(gate_in.astype(jnp.float32)) * (hf + hb)
    return y.astype(gate_in.dtype), hf[:, -1], hb[:, 0]


def segsum(x):
    t = x.shape[-1]
    xx = jnp.broadcast_to(x[..., None], x.shape + (t,))
    xx = jnp.where(jnp.tril(jnp.ones((t, t), bool), -1), xx, 0.0)
    cs = jnp.cumsum(xx, axis=-2)
    return jnp.where(jnp.tril(jnp.ones((t, t), bool)), cs, -jnp.inf)


def ssd_scan(xdt, adt, bm, cm, h0, need_y):
    b, l, h, p = xdt.shape
    n = bm.shape[-1]
    nc = l // SSD_CHUNK
    X = xdt.reshape(b, nc, SSD_CHUNK, h, p)
    Bc = bm.reshape(b, nc, SSD_CHUNK, h, n)
    Cc = cm.reshape(b, nc, SSD_CHUNK, h, n)
    A = adt.reshape(b, nc, SSD_CHUNK, h).transpose(0, 3, 1, 2)
    A_cs = jnp.cumsum(A, axis=-1)
    decay_states = jnp.exp(A_cs[..., -1:] - A_cs)
    states = jnp.einsum('bclhn,bhcl,bclhp->bchpn', Bc, decay_states, X)
    states = jnp.concatenate([h0[:, None], states], axis=1)
    decay_chunk = jnp.exp(segsum(jnp.pad(A_cs[..., -1], ((0, 0), (0, 0), (1, 0)))))
    new_states = jnp.einsum('bhzc,bchpn->bzhpn', decay_chunk, states)
    final = new_states[:, -1]
    if not need_y:
        return None, final
    states = new_states[:, :-1]
    Lmat = jnp.exp(segsum(A))
    y_diag = jnp.einsum('bclhn,bcshn,bhcls,bcshp->bclhp', Cc, Bc, Lmat, X)
    y_off = jnp.einsum('bclhn,bchpn,bhcl->bclhp', Cc, states, jnp.exp(A_cs))
    return (y_diag + y_off).reshape(b, l, h, p), final


def ssd_mix(z, xbc, dt_raw, conv_w, conv_b, a_log, dt_bias, d_skip, norm_g, h0f, h0b, need_y):
    b, l, _ = xbc.shape
    xbc = jax.nn.silu(dwconv(xbc, conv_w, conv_b)).astype(jnp.float32)
    xs, bm, cm = jnp.split(xbc, [SSD_WIDTH, SSD_WIDTH + SSD_GROUPS * SSD_STATE], axis=-1)
    xs = xs.reshape(b, l, SSD_HEADS, SSD_HEAD_DIM)
    rep = SSD_HEADS // SSD_GROUPS
    bm = jnp.repeat(bm.reshape(b, l, SSD_GROUPS, SSD_STATE), rep, axis=2)
    cm = jnp.repeat(cm.reshape(b, l, SSD_GROUPS, SSD_STATE), rep, axis=2)
    dt_all = jax.nn.softplus(dt_raw.astype(jnp.float32).reshape(b, l, 2, SSD_HEADS) + dt_bias.astype(jnp.float32))
    a = -jnp.exp(a_log.astype(jnp.float32))
    dtf, dtb = dt_all[:, :, 0], dt_all[:, :, 1]
    yf, ff = ssd_scan(xs * dtf[..., None], dtf * a[0], bm, cm, h0f, need_y)
    yb, fb = ssd_scan((xs * dtb[..., None])[:, ::-1], (dtb * a[1])[:, ::-1], bm[:, ::-1], cm[:, ::-1], h0b, need_y)
    if not need_y:
        return None, ff, fb
    y = yf + yb[:, ::-1] + d_skip.astype(jnp.float32)[:, None] * xs
    y = y.reshape(b, l, SSD_WIDTH) * jax.nn.silu(z.astype(jnp.float32))
    return rmsnorm(y, norm_g).astype(z.dtype), ff, fb


def axial_rope(rows):
    r, col = jnp.meshgrid(jnp.arange(rows, dtype=jnp.float32), jnp.arange(GRID_W, dtype=jnp.float32), indexing='ij')
    per_axis = DA_QK_DIM // 4
    inv = ROPE_BASE ** (-jnp.arange(per_axis, dtype=jnp.float32) / per_axis)
    ang = jnp.concatenate([r.reshape(-1, 1) * inv, col.reshape(-1, 1) * inv], axis=-1)
    return jnp.cos(ang), jnp.sin(ang)


def apply_rope(t, cos, sin):
    tf = t.astype(jnp.float32)
    half = DA_QK_DIM // 2
    t1, t2 = tf[..., :half], tf[..., half:]
    c = cos[:, None, None, :]
    s = sin[:, None, None, :]
    return jnp.concatenate([t1 * c - t2 * s, t2 * c + t1 * s], axis=-1).astype(t.dtype)


def da_qk(t, g, cos, sin):
    b, l, _ = t.shape
    t = rmsnorm(t.reshape(b, l, DA_HEADS, 2, DA_QK_DIM), g)
    if cos is not None:
        t = apply_rope(t, cos, sin)
    return t


def diff_lambda(lam_q, lam_k, lam_init):
    lq = lam_q.astype(jnp.float32)
    lk = lam_k.astype(jnp.float32)
    return jnp.exp(jnp.sum(lq[0] * lk[0])) - jnp.exp(jnp.sum(lq[1] * lk[1])) + lam_init


def diff_block(q, k, v, lam):
    s = jnp.einsum('bqhtd,bkhtd->bhtqk', q, k, preferred_element_type=jnp.float32) * DA_SCALE
    p = jax.nn.softmax(s, axis=-1)
    w = p[:, :, 0] - lam * p[:, :, 1]
    return jnp.einsum('bhqk,bkhe->bqhe', w.astype(v.dtype), v)


def diff_attn_latent(q, k, v, k_ctx, v_ctx, lam):
    b, l = q.shape[:2]
    k_all = jnp.concatenate([k_ctx, k], axis=1)
    v_all = jnp.concatenate([v_ctx, v], axis=1)
    nb = l // Q_BLOCK
    qb = q.reshape(b, nb, Q_BLOCK, DA_HEADS, 2, DA_QK_DIM).swapaxes(0, 1)
    o = lax.map(lambda qi: diff_block(qi, k_all, v_all, lam), qb)
    return o.swapaxes(0, 1).reshape(b, l, DA_HEADS, DA_V_DIM)


def diff_heads_out(o, g, lam_init):
    b, l = o.shape[:2]
    return (rmsnorm(o, g) * (1.0 - lam_init)).reshape(b, l, DA_WIDTH)


def expert_swiglu(xb, wgu, bgu, wd, bd):
    gu = jnp.matmul(xb, wgu) + bgu
    gate, up = gu[..., ::2], gu[..., 1::2]
    gate = jnp.minimum(gate, SWIGLU_LIMIT)
    up = jnp.clip(up, -SWIGLU_LIMIT, SWIGLU_LIMIT)
    glu = gate * jax.nn.sigmoid(SWIGLU_ALPHA * gate)
    return jnp.matmul((up + 1.0) * glu, wd) + bd


def moe_ffn(h, router_w, router_b, w_gu, b_gu, w_down, b_down):
    t, d = h.shape
    n = t * TOP_K
    logits = jnp.matmul(h, router_w, preferred_element_type=jnp.float32) + router_b.astype(jnp.float32)
    top_v, top_e = lax.top_k(logits, TOP_K)
    gates = jax.nn.softmax(top_v, axis=-1)
    flat_e = top_e.reshape(n)
    order = jnp.argsort(flat_e)
    e_sorted = flat_e[order]
    tok_sorted = order // TOP_K
    gate_sorted = gates.reshape(n)[order]
    counts = jnp.bincount(flat_e, length=N_EXPERTS)
    padded = (counts + MOE_BLOCK - 1) // MOE_BLOCK * MOE_BLOCK
    start = jnp.cumsum(counts) - counts
    pend = jnp.cumsum(padded)
    pstart = pend - padded
    dest = pstart[e_sorted] + jnp.arange(n) - start[e_sorted]
    nb = (n + N_EXPERTS * (MOE_BLOCK - 1)) // MOE_BLOCK
    slot_tok = jnp.full((nb * MOE_BLOCK,), t, jnp.int32).at[dest].set(tok_sorted.astype(jnp.int32))
    block_e = jnp.minimum(jnp.searchsorted(pend, jnp.arange(nb) * MOE_BLOCK, side='right'), N_EXPERTS - 1)
    h_pad = jnp.concatenate([h, jnp.zeros((1, d), h.dtype)], axis=0)
    xb = h_pad[slot_tok].reshape(nb, MOE_BLOCK, d)
    yb = lax.map(lambda args: expert_swiglu(args[0], w_gu[args[1]], b_gu[args[1]], w_down[args[1]], b_down[args[1]]), (xb, block_e))
    y = yb.reshape(nb * MOE_BLOCK, d)[dest] * gate_sorted[:, None].astype(h.dtype)
    return jnp.zeros((t, d), h.dtype).at[tok_sorted].add(y)


def setup_inputs(seed: int = 0) -> dict:
    key = jax.random.key(seed)
    ks = iter(jax.random.split(key, 48))
    f32 = jnp.float32

    def nrm(shape, scale):
        return jax.random.normal(next(ks), shape, f32) * scale

    def gain(shape):
        return 1.0 + nrm(shape, 0.02)

    L = DEPTH
    u_lru = jax.random.uniform(next(ks), (L, 2, LRU_WIDTH), f32, 0.9, 0.999)
    a_lru = u_lru ** (1.0 / LRU_C)
    dt0 = jnp.exp(jax.random.uniform(next(ks), (L, 2, SSD_HEADS), f32, math.log(1e-3), math.log(1e-1)))
    return {
        'x': nrm((BATCH, SEQ, D_MODEL), 1.0),
        'c': nrm((BATCH, D_MODEL), 1.0),
        'ctx': nrm((BATCH, CTX_LEN, D_MODEL), 1.0),
        'c_ctx': nrm((D_MODEL,), 1.0),
        'w_mod': nrm((L, D_MODEL, 6 * D_MODEL), D_MODEL ** -0.5),
        'b_mod': nrm((L, 6 * D_MODEL), 0.02),
        'norm1_g': gain((L, D_MODEL)),
        'norm2_g': gain((L, D_MODEL)),
        'w_in': nrm((L, D_MODEL, D_IN), D_MODEL ** -0.5),
        'w_out': nrm((L, D_MIX, D_MODEL), D_MIX ** -0.5),
        'lru_conv_w': nrm((L, SHORT_CONV, LRU_WIDTH), SHORT_CONV ** -0.5),
        'lru_conv_b': nrm((L, LRU_WIDTH), 0.02),
        'lru_wa': nrm((L, 2, LRU_HEADS, LRU_HEAD_DIM, LRU_HEAD_DIM), LRU_HEAD_DIM ** -0.5),
        'lru_ba': nrm((L, 2, LRU_WIDTH), 0.02),
        'lru_wx': nrm((L, 2, LRU_HEADS, LRU_HEAD_DIM, LRU_HEAD_DIM), LRU_HEAD_DIM ** -0.5),
        'lru_bx': nrm((L, 2, LRU_WIDTH), 0.02),
        'lru_lam': jnp.log(a_lru) - jnp.log1p(-a_lru),
        'ssd_conv_w': nrm((L, SHORT_CONV, SSD_CONV_DIM), SHORT_CONV ** -0.5),
        'ssd_conv_b': nrm((L, SSD_CONV_DIM), 0.02),
        'ssd_a_log': jnp.log(jax.random.uniform(next(ks), (L, 2, SSD_HEADS), f32, 1.0, 16.0)),
        'ssd_dt_bias': dt0 + jnp.log(-jnp.expm1(-dt0)),
        'ssd_d': 1.0 + nrm((L, SSD_HEADS), 0.1),
        'ssd_norm_g': gain((L, SSD_WIDTH)),
        'da_q_norm': gain((L, DA_QK_DIM)),
        'da_k_norm': gain((L, DA_QK_DIM)),
        'da_lam_q': nrm((L, 2, DA_QK_DIM), 0.1),
        'da_lam_k': nrm((L, 2, DA_QK_DIM), 0.1),
        'da_subln_g': gain((L, DA_V_DIM)),
        'router_w': nrm((L, D_MODEL, N_EXPERTS), D_MODEL ** -0.5),
        'router_b': nrm((L, N_EXPERTS), 0.01),
        'exp_w_gu': nrm((L, N_EXPERTS, D_MODEL, 2 * D_EXPERT), D_MODEL ** -0.5),
        'exp_b_gu': nrm((L, N_EXPERTS, 2 * D_EXPERT), 0.02),
        'exp_w_down': nrm((L, N_EXPERTS, D_EXPERT, D_MODEL), D_EXPERT ** -0.5),
        'exp_b_down': nrm((L, N_EXPERTS, D_MODEL), 0.02),
    }


def reference(x, c, ctx, c_ctx, w_mod, b_mod, norm1_g, norm2_g, w_in, w_out,
              lru_conv_w, lru_conv_b, lru_wa, lru_ba, lru_wx, lru_bx, lru_lam,
              ssd_conv_w, ssd_conv_b, ssd_a_log, ssd_dt_bias, ssd_d, ssd_norm_g,
              da_q_norm, da_k_norm, da_lam_q, da_lam_k, da_subln_g,
              router_w, router_b, exp_w_gu, exp_b_gu, exp_w_down, exp_b_down):
    b, seq_len, d = x.shape
    ctx_len = ctx.shape[1]
    rows = seq_len // GRID_W
    cos, sin = axial_rope(rows)
    x_lat, x_ctx = x, ctx
    for l in range(DEPTH):
        need_ctx = l < DEPTH - 1
        lam_init = 0.8 - 0.6 * math.exp(-0.3 * l)
        mod = jnp.matmul(jax.nn.silu(c), w_mod[l]) + b_mod[l]
        mod_c = jnp.matmul(jax.nn.silu(c_ctx), w_mod[l]) + b_mod[l]
        sh1, sc1, g1, sh2, sc2, g2 = jnp.split(mod[:, None, :], 6, axis=-1)
        sh1c, sc1c, g1c, sh2c, sc2c, g2c = jnp.split(mod_c, 6, axis=-1)

        h_lat = modulate(rmsnorm(x_lat, norm1_g[l]), sh1, sc1)
        h_ctx = modulate(rmsnorm(x_ctx, norm1_g[l]), sh1c, sc1c)
        lg_l, lx_l, sz_l, sxbc_l, sdt_l, q_l, k_l, v_l = jnp.split(jnp.matmul(h_lat, w_in[l]), IN_SPLITS, axis=-1)
        lg_c, lx_c, sz_c, sxbc_c, sdt_c, q_c, k_c, v_c = jnp.split(jnp.matmul(h_ctx, w_in[l]), IN_SPLITS, axis=-1)

        zl = jnp.zeros((b, LRU_WIDTH), jnp.float32)
        lru_c, lru_sf, lru_sb = lru_mix(lg_c, lx_c, lru_conv_w[l], lru_conv_b[l], lru_wa[l], lru_ba[l],
                                        lru_wx[l], lru_bx[l], lru_lam[l], zl, zl, need_ctx)
        lru_y, _, _ = lru_mix(lg_l, lx_l, lru_conv_w[l], lru_conv_b[l], lru_wa[l], lru_ba[l],
                              lru_wx[l], lru_bx[l], lru_lam[l], lru_sf, lru_sb, True)

        zs = jnp.zeros((b, SSD_HEADS, SSD_HEAD_DIM, SSD_STATE), jnp.float32)
        ssd_c, ssd_sf, ssd_sb = ssd_mix(sz_c, sxbc_c, sdt_c, ssd_conv_w[l], ssd_conv_b[l], ssd_a_log[l],
                                        ssd_dt_bias[l], ssd_d[l], ssd_norm_g[l], zs, zs, need_ctx)
        ssd_y, _, _ = ssd_mix(sz_l, sxbc_l, sdt_l, ssd_conv_w[l], ssd_conv_b[l], ssd_a_log[l],
                              ssd_dt_bias[l], ssd_d[l], ssd_norm_g[l], ssd_sf, ssd_sb, True)

        lam = diff_lambda(da_lam_q[l], da_lam_k[l], lam_init)
        kc = da_qk(k_c, da_k_norm[l], None, None)
        vc = v_c.reshape(b, ctx_len, DA_HEADS, DA_V_DIM)
        ql = da_qk(q_l, da_q_norm[l], cos, sin)
        kl = da_qk(k_l, da_k_norm[l], cos, sin)
        vl = v_l.reshape(b, seq_len, DA_HEADS, DA_V_DIM)
        da_y = diff_heads_out(diff_attn_latent(ql, kl, vl, kc, vc, lam), da_subln_g[l], lam_init)

        o_lat = jnp.matmul(jnp.concatenate([lru_y, ssd_y, da_y], axis=-1), w_out[l])
        if need_ctx:
            qc = da_qk(q_c, da_q_norm[l], None, None)
            da_c = diff_heads_out(diff_block(qc, kc, vc, lam), da_subln_g[l], lam_init)
            o_ctx = jnp.matmul(jnp.concatenate([lru_c, ssd_c, da_c], axis=-1), w_out[l])
            x_ctx = x_ctx + g1c * o_ctx
        x_lat = x_lat + g1 * o_lat

        h2_lat = modulate(rmsnorm(x_lat, norm2_g[l]), sh2, sc2)
        if need_ctx:
            h2_ctx = modulate(rmsnorm(x_ctx, norm2_g[l]), sh2c, sc2c)
            tok = jnp.concatenate([h2_ctx, h2_lat], axis=1).reshape(-1, d)
            f = moe_ffn(tok, router_w[l], router_b[l], exp_w_gu[l], exp_b_gu[l],
                        exp_w_down[l], exp_b_down[l]).reshape(b, ctx_len + seq_len, d)
            x_ctx = x_ctx + g2c * f[:, :ctx_len]
            x_lat = x_lat + g2 * f[:, ctx_len:]
        else:
            f = moe_ffn(h2_lat.reshape(-1, d), router_w[l], router_b[l], exp_w_gu[l], exp_b_gu[l],
                        exp_w_down[l], exp_b_down[l]).reshape(b, seq_len, d)
            x_lat = x_lat + g2 * f
    return x_lat
```

```python
import contextlib
import numpy as np
import concourse.bass as bass
import concourse.mybir as mybir
from concourse.bass_utils import run_bass_kernel_spmd
from concourse.alu_op_type import AluOpType as ALU

AF = mybir.ActivationFunctionType
AX = mybir.AxisListType
F32 = mybir.dt.float32
BF16 = mybir.dt.bfloat16
I32 = mybir.dt.int32
U32 = mybir.dt.uint32

EPOCH = 30000
N_DMA_SEMS = 24


class Dep:
    __slots__ = ("w", "r")

    def __init__(self):
        self.w = None
        self.r = {}


class V:
    __slots__ = ("ap", "deps")

    def __init__(self, ap, deps):
        self.ap = ap
        self.deps = deps

    def __getitem__(self, idx):
        return V(self.ap[idx], self.deps)

    def m(self, fn):
        return V(fn(self.ap), self.deps)

    def re(self, s, **kw):
        return V(self.ap.rearrange(s, **kw), self.deps)

    def bc(self, shape):
        return V(self.ap.broadcast_to(shape), self.deps)

    def bitcast(self, dt):
        return V(self.ap.bitcast(dt), self.deps)


class T:
    def __init__(self, handle, name):
        self.h = handle
        self.name = name
        self.dep = Dep()
        self.subs = {}

    def __getitem__(self, idx):
        return V(self.h[idx], [self.dep])

    def all(self):
        return V(self.h[:], [self.dep] + [s.dep for s in self.subs.values()])

    def part(self, idx):
        return V(self.h[idx], [Dep()])

    def sub(self, key):
        if key not in self.subs:
            self.subs[key] = T(self.h, "%s.%s" % (self.name, key))
        return self.subs[key]


class Prog:
    ENGS = ("tensor", "vector", "scalar", "gpsimd", "sync")

    def __init__(self, nc, stack):
        self.nc = nc
        self.stack = stack
        self.q = {e: [] for e in self.ENGS}
        self.cnt = {e: 0 for e in self.ENGS}
        self.seen = {e: {} for e in self.ENGS}
        self.sems = {}
        self.dma_sems = [stack.enter_context(nc.semaphore("dma%d" % i)) for i in range(2 * N_DMA_SEMS)]
        self.dma_cnt = [0] * (2 * N_DMA_SEMS)
        self.dma_rr = {False: 0, True: 0}
        self.n_ops = 0

    def sb(self, name, shape, dt):
        return T(self.stack.enter_context(self.nc.sbuf_tensor(name, list(shape), dt)), name)

    def ps(self, name, shape, dt=F32):
        return T(self.stack.enter_context(self.nc.psum_tensor(name, list(shape), dt)), name)

    def dram(self, name, shape, dt, kind="Internal"):
        return T(self.nc.dram_tensor(name, list(shape), dt, kind=kind), name)

    def _eng_sem(self, eng, epoch):
        k = (eng, epoch)
        if k not in self.sems:
            self.sems[k] = self.stack.enter_context(self.nc.semaphore("s_%s%d" % (eng, epoch)))
        return k

    def _sem_handle(self, key):
        if key[0] == "dma":
            return self.dma_sems[key[1]]
        return self.sems[key]

    def _collect(self, eng, reads, writes):
        need = {}

        def add(tok):
            if tok is None:
                return
            k, v = tok
            if need.get(k, 0) < v:
                need[k] = v

        for v_ in reads:
            for d in v_.deps:
                add(d.w)
        for v_ in writes:
            for d in v_.deps:
                add(d.w)
                for k, val in d.r.items():
                    add((k, val))
        out = []
        seen = self.seen[eng]
        for k, val in need.items():
            if eng == "tensor" and k[0] == "tensor":
                continue
            if seen.get(k, 0) >= val:
                continue
            seen[k] = val
            out.append((k, val))
        return out

    def _commit(self, tok, reads, writes):
        k, val = tok
        for v_ in reads:
            for d in v_.deps:
                if d.r.get(k, 0) < val:
                    d.r[k] = val
        for v_ in writes:
            for d in v_.deps:
                d.w = tok
                d.r = {}

    def op(self, eng, fn, reads=(), writes=()):
        waits = self._collect(eng, reads, writes)
        self.cnt[eng] += 1
        n = self.cnt[eng]
        epoch = (n - 1) // EPOCH
        k = self._eng_sem(eng, epoch)
        tok = (k, n - epoch * EPOCH)
        self.q[eng].append((waits, fn, k, 1))
        self._commit(tok, reads, writes)
        self.n_ops += 1
        return tok

    def dma(self, eng, out, in_, extra_reads=(), fn=None, **kw):
        reads = [in_] + list(extra_reads)
        writes = [out]
        waits = self._collect(eng, reads, writes)
        sw = (eng == "gpsimd")
        i = self.dma_rr[sw] + (N_DMA_SEMS if sw else 0)
        self.dma_rr[sw] = (self.dma_rr[sw] + 1) % N_DMA_SEMS
        k = ("dma", i)
        if self.dma_cnt[i] > 0 and self.seen[eng].get(k, 0) < self.dma_cnt[i]:
            self.seen[eng][k] = self.dma_cnt[i]
            waits.append((k, self.dma_cnt[i]))
        self.dma_cnt[i] += 16
        tok = (k, self.dma_cnt[i])
        if fn is None:
            o_ap, i_ap = out.ap, in_.ap

            def fn(e, o_ap=o_ap, i_ap=i_ap, kw=kw):
                return e.dma_start(out=o_ap, in_=i_ap, **kw)
        self.q[eng].append((waits, fn, k, 16))
        self._commit(tok, reads, writes)
        self.n_ops += 1
        return tok

    def finish(self, eng, views):
        waits = self._collect(eng, views, ())
        self.q[eng].append((waits, None, None, 0))

    def emit(self):
        nc = self.nc
        with nc.Block() as block:
            def mk(name):
                def body(e):
                    for waits, fn, k, inc in self.q[name]:
                        for wk, wv in waits:
                            e.wait_ge(self._sem_handle(wk), wv)
                        if fn is not None:
                            ins = fn(e)
                            ins.then_inc(self._sem_handle(k), inc)
                return body
            block.tensor(mk("tensor"))
            block.vector(mk("vector"))
            block.scalar(mk("scalar"))
            block.gpsimd(mk("gpsimd"))
            block.sync(mk("sync"))

    def mm(self, out, lhsT, rhs, start=True, stop=True, **kw):
        o, l, r = out.ap, lhsT.ap, rhs.ap
        return self.op("tensor", lambda e: e.matmul(o, l, r, start=start, stop=stop, **kw),
                       reads=[lhsT, rhs], writes=[out])

    def tr(self, out, in_, ident):
        o, i, d = out.ap, in_.ap, ident.ap
        return self.op("tensor", lambda e: e.transpose(o, i, d), reads=[in_, ident], writes=[out])

    def act(self, out, in_, func, bias=None, scale=None, accum_out=None, eng="scalar"):
        o, i = out.ap, in_.ap
        reads = [in_]
        writes = [out]
        kw = {}
        if bias is not None:
            if isinstance(bias, V):
                reads.append(bias)
                kw["bias"] = bias.ap
            else:
                kw["bias"] = bias
        if scale is not None:
            if isinstance(scale, V):
                reads.append(scale)
                kw["scale"] = scale.ap
            else:
                kw["scale"] = scale
        if accum_out is not None:
            writes.append(accum_out)
            kw["accum_out"] = accum_out.ap
        return self.op("scalar", lambda e: e.activation(o, i, func, **kw), reads=reads, writes=writes)

    def tt(self, out, in0, in1, op, eng="vector"):
        o, a, b = out.ap, in0.ap, in1.ap
        return self.op(eng, lambda e: e.tensor_tensor(out=o, in0=a, in1=b, op=op),
                       reads=[in0, in1], writes=[out])

    def ts(self, out, in0, s1, op0, s2=None, op1=None, accum_out=None, eng="vector"):
        o, a = out.ap, in0.ap
        reads = [in0]
        writes = [out]
        if isinstance(s1, V):
            reads.append(s1)
            s1 = s1.ap
        if isinstance(s2, V):
            reads.append(s2)
            s2 = s2.ap
        kw = {}
        if op1 is not None:
            kw["op1"] = op1
        if accum_out is not None:
            writes.append(accum_out)
            kw["accum_out"] = accum_out.ap
        return self.op(eng, lambda e: e.tensor_scalar(out=o, in0=a, scalar1=s1, scalar2=s2, op0=op0, **kw),
                       reads=reads, writes=writes)

    def stt(self, out, in0, scalar, in1, op0, op1, eng="vector"):
        o, a, b = out.ap, in0.ap, in1.ap
        reads = [in0, in1]
        if isinstance(scalar, V):
            reads.append(scalar)
            scalar = scalar.ap
        return self.op(eng, lambda e: e.scalar_tensor_tensor(out=o, in0=a, scalar=scalar, in1=b, op0=op0, op1=op1),
                       reads=reads, writes=[out])

    def copy(self, out, in_, eng="vector"):
        o, i = out.ap, in_.ap
        if eng == "scalar":
            return self.op(eng, lambda e: e.copy(o, i), reads=[in_], writes=[out])
        return self.op(eng, lambda e: e.tensor_copy(out=o, in_=i), reads=[in_], writes=[out])

    def memset(self, out, val, eng="vector"):
        o = out.ap
        return self.op(eng, lambda e: e.memset(o, val), writes=[out])

    def reduce(self, out, in_, op, axis=AX.X, eng="vector"):
        o, i = out.ap, in_.ap
        return self.op(eng, lambda e: e.tensor_reduce(out=o, in_=i, axis=axis, op=op), reads=[in_], writes=[out])

    def recip(self, out, in_):
        o, i = out.ap, in_.ap
        return self.op("vector", lambda e: e.reciprocal(out=o, in_=i), reads=[in_], writes=[out])

    def scan(self, out, d0, d1, initial, op0=ALU.mult, op1=ALU.add):
        o, a, b = out.ap, d0.ap, d1.ap
        reads = [d0, d1]
        if isinstance(initial, V):
            reads.append(initial)
            initial = initial.ap
        return self.op("vector", lambda e: e.tensor_tensor_scan(out=o, data0=a, data1=b, initial=initial, op0=op0, op1=op1),
                       reads=reads, writes=[out])


def _phase(self):
    prog = self

    class _Ph:
        def __enter__(s):
            s.old = getattr(prog, "stack_local", None)
            s.st = contextlib.ExitStack()
            s.st.__enter__()
            prog.stack_local = s.st
            return s

        def __exit__(s, *a):
            prog.barrier()
            prog.stack_local = s.old
            return s.st.__exit__(*a)
    return _Ph()


def _barrier(self):
    latest = {}
    for e in self.ENGS:
        n = self.cnt[e]
        if n > 0:
            epoch = (n - 1) // EPOCH
            latest[(e, epoch)] = n - epoch * EPOCH
    for i in range(2 * N_DMA_SEMS):
        if self.dma_cnt[i] > 0:
            latest[("dma", i)] = self.dma_cnt[i]
    for e in self.ENGS:
        waits = []
        seen = self.seen[e]
        for k, val in latest.items():
            if k[0] == e:
                continue
            if seen.get(k, 0) >= val:
                continue
            seen[k] = val
            waits.append((k, val))
        if waits:
            self.q[e].append((waits, None, None, 0))


def _lsb(self, name, shape, dt):
    self._uid = getattr(self, "_uid", 0) + 1
    nm = "%s_%d" % (name, self._uid)
    return T(self.stack_local.enter_context(self.nc.sbuf_tensor(nm, list(shape), dt)), nm)


def _lps(self, name, shape, dt=F32):
    self._uid = getattr(self, "_uid", 0) + 1
    nm = "%s_%d" % (name, self._uid)
    return T(self.stack_local.enter_context(self.nc.psum_tensor(nm, list(shape), dt)), nm)


Prog.phase = _phase
Prog.barrier = _barrier
Prog.lsb = _lsb
Prog.lps = _lps

D = 1024
SEQ = 4096
CTX = 256
TALL = SEQ + CTX
NT = TALL // 128
DEPTH = 2
D_IN = 3080
EPS = 1e-6
N_EXP = 32
BLK = 256
FM_COLS = [0, 128, 256, 384, 768, 896, 1024, 1152, 1280, 1408]
OFF_Z, OFF_DT, OFF_Q, OFF_K, OFF_V = 512, 1536, 1544, 2056, 2568


def drow(t, r0, n):
    return t[r0:r0 + n, :]


def bcast_rows(P, eng, dst, src_row_view):
    p = dst.ap.shape[0]
    n = dst.ap.shape[1]
    P.dma(eng, dst, src_row_view.m(lambda a: a.broadcast_to([p, n])))


class Ctx:
    pass


def declare_io(P, C, ext_in=(), ext_out=(), skip=()):
    C.in_names = []

    def inp(name, shape, dt=F32):
        if name in skip:
            return None
        C.in_names.append(name)
        return P.dram(name, shape, dt, kind="ExternalInput")

    def scr(name, shape, dt):
        kind = "Internal"
        if name in ext_in:
            kind = "ExternalInput"
        if name in ext_out:
            kind = "ExternalOutput"
        return P.dram(name, shape, dt, kind=kind)

    C.x = inp("x", [SEQ, D])
    C.c = inp("c", [1, D])
    C.ctx = inp("ctx", [CTX, D])
    C.c_ctx = inp("c_ctx", [1, D])
    C.w_mod = inp("w_mod", [DEPTH, D, 6 * D])
    C.b_mod = inp("b_mod", [DEPTH, 6 * D])
    C.norm1_g = inp("norm1_g", [DEPTH, D])
    C.norm2_g = inp("norm2_g", [DEPTH, D])
    C.w_in = inp("w_in", [DEPTH, D, D_IN])
    C.w_out = inp("w_out", [DEPTH, D, D])
    C.lru_conv_w = inp("lru_conv_w", [DEPTH, 4, 256])
    C.lru_conv_b = inp("lru_conv_b", [DEPTH, 256])
    C.lru_wa = inp("lru_wa", [DEPTH, 2, 4, 64, 64])
    C.lru_ba = inp("lru_ba", [DEPTH, 2, 256])
    C.lru_wx = inp("lru_wx", [DEPTH, 2, 4, 64, 64])
    C.lru_bx = inp("lru_bx", [DEPTH, 2, 256])
    C.lru_lam = inp("lru_lam", [DEPTH, 2, 256])
    C.ssd_conv_w = inp("ssd_conv_w", [DEPTH, 4, 768])
    C.ssd_conv_b = inp("ssd_conv_b", [DEPTH, 768])
    C.ssd_a_log = inp("ssd_a_log", [DEPTH, 8])
    C.ssd_dt_bias = inp("ssd_dt_bias", [DEPTH, 8])
    C.ssd_d = inp("ssd_d", [DEPTH, 4])
    C.ssd_norm_g = inp("ssd_norm_g", [DEPTH, 256])
    C.da_q_norm = inp("da_q_norm", [DEPTH, 64])
    C.da_k_norm = inp("da_k_norm", [DEPTH, 64])
    C.da_lam_q = inp("da_lam_q", [DEPTH, 128])
    C.da_lam_k = inp("da_lam_k", [DEPTH, 128])
    C.da_subln_g = inp("da_subln_g", [DEPTH, 128])
    C.router_w = inp("router_w", [DEPTH, D, N_EXP])
    C.router_b = inp("router_b", [DEPTH, N_EXP])
    C.exp_w_gu = inp("exp_w_gu", [DEPTH * N_EXP * D, 2 * D])
    C.exp_b_gu = inp("exp_b_gu", [DEPTH, N_EXP, 2 * D])
    C.exp_w_down = inp("exp_w_down", [DEPTH * N_EXP * D, D])
    C.exp_b_down = inp("exp_b_down", [DEPTH, N_EXP, D])
    C.k_ident = inp("k_ident", [128, 128])
    C.k_tri = inp("k_tri", [128, 128])
    C.k_triu = inp("k_triu", [128, 128])
    C.k_trils = inp("k_trils", [128, 128])
    C.k_negf = inp("k_negf", [128, 128])
    C.k_negb = inp("k_negb", [128, 128])
    C.k_cos = inp("k_cos", [128, SEQ // 128, 32])
    C.k_sin = inp("k_sin", [128, SEQ // 128, 32])
    C.k_iota32 = inp("k_iota32", [128, 32])
    C.k_iotap = inp("k_iotap", [128, 1])
    C.k_rowidx = inp("k_rowidx", [128, 8])
    C.k_iopb = inp("k_iopb", [128, BLK])
    C.k_thr = inp("k_thr", [128, (TALL * 4) // BLK + 1])
    C.k_bthr = inp("k_bthr", [128, (TALL * 4 + N_EXP * (BLK - 1)) // BLK])

    C.out = P.dram("out", [SEQ, D], F32, kind="ExternalOutput")
    C.modv = scr("modv", [2, 6 * D], F32)
    C.fm = scr("fm", [10 * 128, TALL], F32)
    C.qT = scr("qT", [4, 128, TALL], BF16)
    C.kT = scr("kT", [4, 128, TALL], BF16)
    C.vv = scr("vv", [TALL, 512], BF16)
    C.zz = scr("zz", [TALL, 256], F32)
    C.dtr = scr("dtr", [TALL, 8], F32)
    C.mixT = scr("mixT", [D, TALL], BF16)
    C.xa = scr("xa", [TALL, D], F32)
    C.xb = scr("xb", [TALL, D], F32)
    C.h2 = scr("h2", [TALL, D], BF16)
    nslots = ((TALL * 4 + N_EXP * (BLK - 1)) // BLK) * BLK
    C.nslots = nslots
    C.xblk = scr("xblk", [nslots, D], BF16)
    C.yblk = scr("yblk", [nslots, D], F32)
    C.wgu_bf = scr("wgu_bf", [N_EXP * 128, 8 * 2 * D], BF16)
    C.wd_bf = scr("wd_bf", [N_EXP * 128, 8 * D], BF16)


def load_consts(P, C):
    C.dt_sb = P.sb("dt_sb", [128, NT, 8], F32)
    C.ident_f = P.sb("ident_f", [128, 128], F32)
    C.ident_b = P.sb("ident_b", [128, 128], BF16)
    P.dma("sync", C.ident_f[:], C.k_ident[:])
    P.copy(C.ident_b[:], C.ident_f[:])


def xrows(C, l, i):
    if l == 0:
        if i < 2:
            return C.ctx[i * 128:(i + 1) * 128, :]
        return C.x[(i - 2) * 128:(i - 1) * 128, :]
    return C.xb[i * 128:(i + 1) * 128, :]


def phase_A(P, C, l):
    with P.phase():
        cc = P.lsb("cc", [128, 2, 8], F32)
        cs = P.lsb("cs", [128, 2, 8], F32)
        P.dma("sync", cc[:, 0, :], C.c[0:1, :].re("o (p k) -> (o p) k", k=8))
        P.dma("sync", cc[:, 1, :], C.c_ctx[0:1, :].re("o (p k) -> (o p) k", k=8))
        P.act(cs[:], cc[:], AF.Silu)
        bm = P.lsb("bm", [2, 6 * D], F32)
        P.dma("sync", bm[0:1, :], C.b_mod[l:l + 1, :])
        P.dma("sync", bm[1:2, :], C.b_mod[l:l + 1, :])
        ng = P.lsb("ng", [2, 2, D], F32)
        for r in range(2):
            P.dma("sync", ng[r:r + 1, 0, :], C.norm1_g[l:l + 1, :])
            P.dma("sync", ng[r:r + 1, 1, :], C.norm2_g[l:l + 1, :])
        mrow = P.lsb("mrow", [2, 6 * D], F32)
        wm = [P.lsb("wm%d" % j, [128, 8, 512], F32) for j in range(2)]
        pm = [P.lps("pm%d" % j, [2, 512], F32) for j in range(2)]
        wv = C.w_mod[l].re("(p k) n -> p k n", k=8)
        for j in range(12):
            w = wm[j % 2]
            P.dma("sync", w[:], wv[:, :, j * 512:(j + 1) * 512])
            ps = pm[j % 2]
            for k in range(8):
                P.mm(ps[:], cs[:, :, k], w[:, k, :], start=(k == 0), stop=(k == 7))
            P.tt(mrow[:, j * 512:(j + 1) * 512], ps[:], bm[:, j * 512:(j + 1) * 512], ALU.add)
        P.stt(mrow[:, D:2 * D], mrow[:, D:2 * D], 1.0, ng[:, 0, :], ALU.add, ALU.mult)
        P.stt(mrow[:, 4 * D:5 * D], mrow[:, 4 * D:5 * D], 1.0, ng[:, 1, :], ALU.add, ALU.mult)
        P.dma("sync", C.modv[:], mrow[:])


def rstd_from_ss(P, rstd, ss, n, tmp):
    P.ts(tmp, ss, 1.0 / n, ALU.mult, EPS, ALU.add)
    P.act(tmp, tmp, AF.Sqrt)
    P.recip(rstd, tmp)


B_MODE = 0
B_LANES = 2
B_CUT = 99


def run_lanes(tasks, nlanes=2):
    lanes = [None] * nlanes
    it = iter(tasks)
    pending = True
    while True:
        for li in range(nlanes):
            if lanes[li] is None and pending:
                f = next(it, None)
                if f is None:
                    pending = False
                else:
                    lanes[li] = f(li)
        if all(g is None for g in lanes):
            break
        for li in range(nlanes):
            g = lanes[li]
            if g is None:
                continue
            try:
                next(g)
            except StopIteration:
                lanes[li] = None


def phase_B(P, C, l):
    with P.phase():
        win = P.lsb("win", [128, 8, D_IN + 264], BF16)
        wv = C.w_in[l].re("(k p) n -> p k n", p=128)
        for kc in range(8):
            for ci, (c0, c1) in enumerate(((0, 1540), (1540, 3080))):
                P.dma("gpsimd", win[:, kc, c0:c1], wv[:, kc, c0:c1])
            P.dma("gpsimd", win[:, kc, D_IN:D_IN + 256], wv[:, kc, OFF_Z:OFF_Z + 256])
            P.dma("gpsimd", win[:, kc, D_IN + 256:D_IN + 264], wv[:, kc, OFF_DT:OFF_DT + 8])
        winv = win.all()
        rows = {}
        for r in range(2):
            sh = P.lsb("shrow%d" % r, [128, D], F32)
            sc = P.lsb("scrow%d" % r, [128, D], F32)
            bcast_rows(P, "sync", sh[:], C.modv[r:r + 1, 0:D])
            bcast_rows(P, "sync", sc[:], C.modv[r:r + 1, D:2 * D])
            rows[r] = (sh, sc)
        gq = P.lsb("gq", [128, 64], F32)
        gk = P.lsb("gk", [128, 64], F32)
        bcast_rows(P, "sync", gq[:], C.da_q_norm[l:l + 1, :])
        bcast_rows(P, "sync", gk[:], C.da_k_norm[l:l + 1, :])
        cos = P.lsb("cos", [128, SEQ // 128, 32], F32)
        sin = P.lsb("sin", [128, SEQ // 128, 32], F32)
        P.dma("sync", cos[:], C.k_cos[:])
        P.dma("sync", sin[:], C.k_sin[:])
        mhalf = P.lsb("bmhalf", [128, 8], F32)
        P.memset(mhalf[:], -0.5)

        NL = 2
        L = []
        for li in range(NL):
            d = dict(
                xt=P.lsb("xt%d" % li, [128, D], F32), junk=P.lsb("junk%d" % li, [128, D], F32),
                hf=P.lsb("hf%d" % li, [128, D], F32), hb=P.lsb("hb%d" % li, [128, D], BF16),
                ss=P.lsb("ss%d" % li, [128, 1], F32), t1=P.lsb("t1%d" % li, [128, 1], F32),
                rstd=P.lsb("rstd%d" % li, [128, 1], F32),
                qsq=P.lsb("qsq%d" % li, [128, 512], F32), qss=P.lsb("qss%d" % li, [128, 8], F32),
                qt8=P.lsb("qt8%d" % li, [128, 8], F32), qrs=P.lsb("qrs%d" % li, [128, 8], F32),
                qn=P.lsb("qn%d" % li, [128, 8, 64], F32), ra=P.lsb("ra%d" % li, [128, 8, 32], F32),
                rb=P.lsb("rb%d" % li, [128, 8, 32], F32), rc=P.lsb("rc%d" % li, [128, 8, 32], F32),
                rd=P.lsb("rd%d" % li, [128, 8, 32], F32), qr=P.lsb("qr%d" % li, [128, 8, 64], BF16),
                qTs=P.lsb("qTs%d" % li, [128, 4, 128], BF16), vb=P.lsb("vb%d" % li, [128, 512], BF16),
                zs=P.lsb("zs%d" % li, [128, 256], F32), dts=P.lsb("dts%d" % li, [128, 8], F32),
                pA=P.lps("pA%d" % li, [128, 8, 128], BF16), pB=P.lps("pB%d" % li, [128, 512], F32),
                pC=P.lps("pC%d" % li, [128, 512], F32))
            L.append(d)
        hTs = [P.lsb("hT%d" % j, [128, 8, 512], BF16) for j in range(2)]
        fms = [P.lsb("fms%d" % j, [128, 512], F32) for j in range(2)]
        pfm = [P.lps("pfm%d" % j, [128, 512], F32) for j in range(2)]

        def qk_post(d, psum, gain, dst, i, t0):
            latent = i >= 2
            qsq, qss, qt8, qrs, qn, ra, rb, rc, rd, qr, qTs, pA = (d[k] for k in (
                "qsq", "qss", "qt8", "qrs", "qn", "ra", "rb", "rc", "rd", "qr", "qTs", "pA"))
            P.act(qsq[:], psum[:], AF.Square)
            yield
            P.reduce(qss[:], qsq[:].re("p (g d) -> p g d", d=64), ALU.add)
            rstd_from_ss(P, qrs[:], qss[:], 64, qt8[:])
            yield
            P.tt(qn[:], psum[:].re("p (g d) -> p g d", d=64),
                 qrs[:].m(lambda a: a.unsqueeze(2).broadcast_to([128, 8, 64])), ALU.mult)
            gb = gain[:].m(lambda a: a.unsqueeze(1).broadcast_to([128, 8, 64]))
            yield
            if not latent:
                P.tt(qr[:], qn[:], gb, ALU.mult)
            else:
                P.tt(qn[:], qn[:], gb, ALU.mult)
                yield
                cb = cos[:, i - 2, :].m(lambda a: a.unsqueeze(1).broadcast_to([128, 8, 32]))
                sb_ = sin[:, i - 2, :].m(lambda a: a.unsqueeze(1).broadcast_to([128, 8, 32]))
                q1 = qn[:, :, 0:32]
                q2 = qn[:, :, 32:64]
                P.tt(ra[:], q1, cb, ALU.mult)
                P.tt(rc[:], q2, cb, ALU.mult, eng="gpsimd")
                yield
                P.tt(rb[:], q2, sb_, ALU.mult)
                P.tt(rd[:], q1, sb_, ALU.mult, eng="gpsimd")
                yield
                P.tt(qr[:, :, 0:32], ra[:], rb[:], ALU.subtract)
                P.tt(qr[:, :, 32:64], rc[:], rd[:], ALU.add, eng="gpsimd")
            yield
            qrf = qr[:].re("p g d -> p (g d)")
            for h in range(4):
                P.tr(pA[:, h, :], qrf[:, h * 128:(h + 1) * 128], C.ident_b[:])
            yield
            P.copy(qTs[:], pA[:, 0:4, :])
            P.dma("sync", dst.part(slice(None)).re("h p t -> p h t")[:, :, t0:t0 + 128], qTs[:])
            yield

        def tile_task(i, gi, j):
            def gen(li):
                d = L[li]
                r = 1 if i < 2 else 0
                sh, sc = rows[r]
                hT = hTs[gi % 2]
                t0 = i * 128
                x = d["xt"]
                P.dma("sync", x[:], xrows(C, l, i))
                P.act(d["junk"][:], x[:], AF.Square, accum_out=d["ss"][:])
                yield
                rstd_from_ss(P, d["rstd"][:], d["ss"][:], D, d["t1"][:])
                yield
                P.stt(d["hf"][:], x[:], d["rstd"][:], sc[:], ALU.mult, ALU.mult)
                yield
                P.tt(d["hb"][:], d["hf"][:], sh[:], ALU.add)
                yield
                for kc in range(8):
                    P.tr(d["pA"][:, kc, :], d["hb"][:, kc * 128:(kc + 1) * 128], C.ident_b[:])
                yield
                P.copy(hT[:, :, j * 128:(j + 1) * 128], d["pA"][:])
                yield
                hs = hT[:, :, j * 128:(j + 1) * 128]

                def proj(ps, c0, n, o0):
                    for kc in range(8):
                        P.mm(ps[:, o0:o0 + n], hs[:, kc, :], winv[:, kc, c0:c0 + n], start=(kc == 0), stop=(kc == 7))
                if B_CUT <= 1:
                    return
                proj(d["pB"], OFF_Q, 512, 0)
                yield
                proj(d["pC"], OFF_K, 512, 0)
                yield
                if B_CUT <= 2:
                    return
                for _ in qk_post(d, d["pB"], gq, C.qT, i, t0):
                    yield
                if B_CUT <= 3:
                    return
                proj(d["pB"], OFF_V, 512, 0)
                yield
                for _ in qk_post(d, d["pC"], gk, C.kT, i, t0):
                    yield
                if B_CUT <= 4:
                    return
                proj(d["pC"], D_IN, 264, 0)
                yield
                P.copy(d["vb"][:], d["pB"][:], eng="scalar")
                P.dma("sync", C.vv.part((slice(t0, t0 + 128), slice(None))), d["vb"][:])
                yield
                if B_CUT <= 5:
                    return
                P.act(d["zs"][:], d["pC"][:, 0:256], AF.Silu)
                P.dma("sync", C.zz.part((slice(t0, t0 + 128), slice(None))), d["zs"][:])
                if B_CUT <= 6:
                    return
                P.copy(C.dt_sb[:, i, :], d["pC"][:, 256:264], eng="scalar")
                yield
            return gen

        def fm_task(gi, grp):
            def gen(li):
                hT = hTs[gi % 2]
                ntok = 128 * len(grp)
                t0 = grp[0] * 128
                for ci, c0 in enumerate(FM_COLS):
                    ps = pfm[ci % 2]
                    for kc in range(8):
                        P.mm(ps[:, 0:ntok], winv[:, kc, c0:c0 + 128], hT[:, kc, 0:ntok], start=(kc == 0), stop=(kc == 7))
                    f = fms[ci % 2]
                    P.copy(f[:, 0:ntok], ps[:, 0:ntok], eng=("scalar" if ci % 2 else "vector"))
                    P.dma("sync", C.fm.part((slice(ci * 128, (ci + 1) * 128), slice(t0, t0 + ntok))), f[:, 0:ntok])
                    yield
            return gen

        groups = [[0, 1]] + [[2 + 4 * g + j for j in range(4)] for g in range(8)]
        tasks = []
        pending_fm = None
        for gi, grp in enumerate(groups):
            for j, i in enumerate(grp):
                tasks.append(tile_task(i, gi, j))
                if j == 1 and pending_fm is not None:
                    tasks.append(pending_fm)
                    pending_fm = None
            pending_fm = fm_task(gi, grp)
        def nop_task(li):
            return iter(())
        tasks.append(lambda li: iter(()))
        tasks.append(lambda li: iter(()))
        if B_MODE == 0:
            run_lanes(tasks, nlanes=2)
            run_lanes([pending_fm], nlanes=1)
        else:
            for gi, grp in enumerate(groups):
                run_lanes([tile_task(i, gi, j) for j, i in enumerate(grp)], nlanes=B_LANES)
                run_lanes([fm_task(gi, grp)], nlanes=1)


def host_consts():
    k = {}
    k["k_ident"] = np.eye(128, dtype=np.float32)
    a = np.arange(128)
    k["k_tri"] = (a[:, None] <= a[None, :]).astype(np.float32)
    k["k_triu"] = (a[:, None] >= a[None, :]).astype(np.float32)
    k["k_trils"] = (a[:, None] < a[None, :]).astype(np.float32)
    k["k_negf"] = np.where(a[None, :] >= a[:, None], 0.0, -30000.0).astype(np.float32)
    k["k_negb"] = np.where(a[None, :] <= a[:, None], 0.0, -30000.0).astype(np.float32)
    t = np.arange(SEQ)
    r = (t // 64).astype(np.float32)
    col = (t % 64).astype(np.float32)
    inv = (10000.0 ** (-np.arange(16, dtype=np.float32) / 16)).astype(np.float32)
    ang = np.concatenate([r[:, None] * inv[None, :], col[:, None] * inv[None, :]], axis=-1).astype(np.float32)
    cos = np.cos(ang).astype(np.float32).reshape(SEQ // 128, 128, 32).transpose(1, 0, 2)
    sin = np.sin(ang).astype(np.float32).reshape(SEQ // 128, 128, 32).transpose(1, 0, 2)
    k["k_cos"] = np.ascontiguousarray(cos)
    k["k_sin"] = np.ascontiguousarray(sin)
    k["k_iota32"] = np.tile(np.arange(32, dtype=np.float32)[None, :], (128, 1))
    k["k_iotap"] = np.arange(128, dtype=np.float32).reshape(128, 1)
    k["k_rowidx"] = (np.arange(8)[None, :] * 128 + np.arange(128)[:, None]).astype(np.float32)
    k["k_iopb"] = np.tile(np.arange(128, dtype=np.float32)[:, None], (1, BLK))
    k["k_thr"] = np.tile((np.arange((TALL * 4) // BLK + 1, dtype=np.float32) * BLK)[None, :], (128, 1))
    nbmax = (TALL * 4 + N_EXP * (BLK - 1)) // BLK
    k["k_bthr"] = np.tile((np.arange(nbmax, dtype=np.float32) * BLK)[None, :], (128, 1))
    return k


def core_inputs(inputs, b, big=True):
    m = {}
    m["x"] = np.ascontiguousarray(inputs["x"][b])
    m["c"] = np.ascontiguousarray(inputs["c"][b:b + 1])
    m["ctx"] = np.ascontiguousarray(inputs["ctx"][b])
    m["c_ctx"] = np.ascontiguousarray(inputs["c_ctx"].reshape(1, D))
    for k in ("w_mod", "b_mod", "norm1_g", "norm2_g", "w_in", "w_out", "lru_conv_w", "lru_conv_b",
              "lru_wa", "lru_ba", "lru_wx", "lru_bx", "lru_lam", "ssd_conv_w", "ssd_conv_b",
              "ssd_norm_g", "da_q_norm", "da_k_norm", "da_subln_g", "router_w", "router_b",
              "exp_b_gu", "exp_b_down", "ssd_d"):
        m[k] = np.ascontiguousarray(inputs[k])
    m["ssd_a_log"] = np.ascontiguousarray(inputs["ssd_a_log"].reshape(DEPTH, 8))
    m["ssd_dt_bias"] = np.ascontiguousarray(inputs["ssd_dt_bias"].reshape(DEPTH, 8))
    m["da_lam_q"] = np.ascontiguousarray(inputs["da_lam_q"].reshape(DEPTH, 128))
    m["da_lam_k"] = np.ascontiguousarray(inputs["da_lam_k"].reshape(DEPTH, 128))
    if big:
        m["exp_w_gu"] = inputs["exp_w_gu"].reshape(DEPTH * N_EXP * D, 2 * D)
        m["exp_w_down"] = inputs["exp_w_down"].reshape(DEPTH * N_EXP * D, D)
    m.update(host_consts())
    return m


def phase_C(P, C, l, need_ctx):
    with P.phase():
        LMAX = SEQ
        B = [P.lsb("lb%d" % j, [128, LMAX + 4], F32) for j in range(6)]
        xcb = P.lsb("xcb", [128, LMAX], BF16)
        yb = P.lsb("ylru", [128, LMAX], BF16)
        pg = [P.lps("pg%d" % j, [128, 512], F32) for j in range(2)]
        for ct in range(2):
            ch = slice(ct * 128, (ct + 1) * 128)
            cw = P.lsb("cw", [128, 4], F32)
            cbias = P.lsb("cbias", [128, 1], F32)
            P.dma("sync", cw[:], C.lru_conv_w[l][:, ch].re("j c -> c j"), allow_slow_non_contiguous=True)
            P.dma("sync", cbias[:], C.lru_conv_b[l:l + 1, ch].re("o c -> c o"), allow_slow_non_contiguous=True)
            wg = {}
            bg = {}
            sp = {}
            for d in range(2):
                for gi, (wsrc, bsrc) in enumerate(((C.lru_wa, C.lru_ba), (C.lru_wx, C.lru_bx))):
                    wf = P.lsb("wf", [128, 128], F32)
                    P.memset(wf[:], 0.0)
                    for hh in range(2):
                        P.dma("sync", wf[hh * 64:(hh + 1) * 64, hh * 64:(hh + 1) * 64],
                              wsrc[l][d][2 * ct + hh])
                    wb = P.lsb("wb", [128, 128], BF16)
                    P.copy(wb[:], wf[:])
                    wg[(d, gi)] = wb
                    bb = P.lsb("bb", [128, 1], F32)
                    P.dma("sync", bb[:], bsrc[l][d:d + 1, ch].re("o c -> c o"), allow_slow_non_contiguous=True)
                    bg[(d, gi)] = bb
                lam = P.lsb("lam", [128, 1], F32)
                P.dma("sync", lam[:], C.lru_lam[l][d:d + 1, ch].re("o c -> c o"), allow_slow_non_contiguous=True)
                e1 = P.lsb("e1", [128, 1], F32)
                P.act(e1[:], lam[:], AF.Exp, scale=-1.0)
                P.act(e1[:], e1[:], AF.Ln, bias=1.0)
                s8 = P.lsb("s8", [128, 1], F32)
                s16 = P.lsb("s16", [128, 1], F32)
                P.ts(s8[:], e1[:], -8.0, ALU.mult)
                P.ts(s16[:], e1[:], -16.0, ALU.mult)
                sp[d] = (s8, s16)
            h0 = {0: None, 1: None}
            hfin = [P.lsb("hfin%d" % d, [128, 1], F32) for d in range(2)]
            for (t0, L, is_ctx) in ((0, CTX, True), (CTX, SEQ, False)):
                xp, xc, b2, b3, b4, b5 = B
                P.memset(xp[:, 0:2], 0.0)
                P.memset(xp[:, L + 2:L + 4], 0.0)
                P.dma("sync", xp[:, 2:L + 2], C.fm[(2 + ct) * 128:(3 + ct) * 128, t0:t0 + L])
                P.ts(xc[:, 0:L], xp[:, 0:L], cw[:, 0:1], ALU.mult, cbias[:], ALU.add)
                for j in range(1, 4):
                    P.stt(xc[:, 0:L], xp[:, j:j + L], cw[:, j:j + 1], xc[:, 0:L], ALU.mult, ALU.add)
                P.copy(xcb[:, 0:L], xc[:, 0:L], eng="gpsimd")
                hs = {}
                for d in range(2):
                    if d == 0:
                        br, bi, ba = b2, b3, b4
                    else:
                        br, bi, ba = b3, b4, b5
                    nchunk = (L + 511) // 512
                    for gi, dst in ((0, br), (1, bi)):
                        for cix in range(nchunk):
                            n = min(512, L - cix * 512)
                            ps = pg[(cix + gi) % 2]
                            P.mm(ps[:, 0:n], wg[(d, gi)][:], xcb[:, cix * 512:cix * 512 + n])
                            P.act(dst[:, cix * 512:cix * 512 + n], ps[:, 0:n], AF.Sigmoid, bias=bg[(d, gi)][:])
                    s8, s16 = sp[d]
                    P.tt(bi[:, 0:L], bi[:, 0:L], xc[:, 0:L], ALU.mult, eng="gpsimd")
                    P.act(ba[:, 0:L], br[:, 0:L], AF.Exp, scale=s8[:])
                    P.act(br[:, 0:L], br[:, 0:L], AF.Exp, scale=s16[:])
                    P.act(br[:, 0:L], br[:, 0:L], AF.Sqrt, scale=-1.0, bias=1.0)
                    P.tt(bi[:, 0:L], bi[:, 0:L], br[:, 0:L], ALU.mult)
                    init = 0.0 if is_ctx else hfin[d][:]
                    if d == 0:
                        P.scan(br[:, 0:L], ba[:, 0:L], bi[:, 0:L], init)
                        if is_ctx:
                            P.copy(hfin[0][:], br[:, L - 1:L])
                    else:
                        P.scan(br[:, 0:L][:, ::-1], ba[:, 0:L][:, ::-1], bi[:, 0:L][:, ::-1], init)
                        if is_ctx:
                            P.copy(hfin[1][:], br[:, 0:1])
                    hs[d] = br
                if is_ctx and not need_ctx:
                    continue
                P.tt(b2[:, 0:L], b2[:, 0:L], b3[:, 0:L], ALU.add)
                P.dma("sync", b4[:, 0:L], C.fm[ct * 128:(ct + 1) * 128, t0:t0 + L])
                P.tt(b5[:, 0:L], b4[:, 0:L], b4[:, 0:L], ALU.mult, eng="gpsimd")
                P.ts(b5[:, 0:L], b5[:, 0:L], 0.044715, ALU.mult, 1.0, ALU.add)
                P.tt(b5[:, 0:L], b5[:, 0:L], b4[:, 0:L], ALU.mult, eng="gpsimd")
                P.act(b5[:, 0:L], b5[:, 0:L], AF.Sigmoid, scale=1.5957691216057308)
                P.tt(b4[:, 0:L], b4[:, 0:L], b5[:, 0:L], ALU.mult, eng="gpsimd")
                P.tt(yb[:, 0:L], b2[:, 0:L], b4[:, 0:L], ALU.mult)
                P.dma("sync", C.mixT.part((slice(ct * 128, (ct + 1) * 128), slice(t0, t0 + L))), yb[:, 0:L])


def phase_D(P, C, l, need_ctx, stop=99, nog=False):
    GP = "vector" if nog else "gpsimd"
    with P.phase():
        cv = [P.lsb("cv%d" % j, [128, TALL], BF16) for j in range(6)]
        with P.phase():
            xp = P.lsb("sxp", [128, SEQ + 4], F32)
            acc = P.lsb("sacc", [128, SEQ], F32)
            for j in range(6):
                ch = slice(j * 128, (j + 1) * 128)
                cw = P.lsb("scw", [128, 4], F32)
                cbias = P.lsb("scb", [128, 1], F32)
                P.dma("sync", cw[:], C.ssd_conv_w[l][:, ch].re("j c -> c j"), allow_slow_non_contiguous=True)
                P.dma("sync", cbias[:], C.ssd_conv_b[l:l + 1, ch].re("o c -> c o"), allow_slow_non_contiguous=True)
                for (t0, L) in ((0, CTX), (CTX, SEQ)):
                    P.memset(xp[:, 0:2], 0.0)
                    P.memset(xp[:, L + 2:L + 4], 0.0)
                    P.dma("sync", xp[:, 2:L + 2], C.fm[(4 + j) * 128:(5 + j) * 128, t0:t0 + L])
                    P.ts(acc[:, 0:L], xp[:, 0:L], cw[:, 0:1], ALU.mult, cbias[:], ALU.add)
                    for t in range(1, 4):
                        P.stt(acc[:, 0:L], xp[:, t:t + L], cw[:, t:t + 1], acc[:, 0:L], ALU.mult, ALU.add)
                    P.act(cv[j][:, t0:t0 + L], acc[:, 0:L], AF.Silu)
        if stop <= 1:
            return
        dt = P.lsb("dt", [128, NT, 8], F32)
        A = P.lsb("A", [128, NT, 8], F32)
        brow = P.lsb("brow", [128, 8], F32)
        arow = P.lsb("arow", [128, 8], F32)
        P.copy(dt[:], C.dt_sb[:], eng="gpsimd")
        bcast_rows(P, "sync", brow[:], C.ssd_dt_bias[l:l + 1, :])
        bcast_rows(P, "sync", arow[:], C.ssd_a_log[l:l + 1, :])
        P.tt(dt[:], dt[:], brow[:].m(lambda a: a.unsqueeze(1).broadcast_to([128, NT, 8])), ALU.add)
        P.act(dt[:], dt[:], AF.Exp)
        P.act(dt[:], dt[:], AF.Ln, bias=1.0)
        P.act(arow[:], arow[:], AF.Exp)
        P.ts(arow[:], arow[:], -1.0, ALU.mult)
        P.tt(A[:], dt[:], arow[:].m(lambda a: a.unsqueeze(1).broadcast_to([128, NT, 8])), ALU.mult)
        ones_f = P.lsb("ones_f", [128, 128], F32)
        P.memset(ones_f[:], 1.0)
        tri = [P.lsb("tri%d" % d, [128, 128], F32) for d in range(2)]
        neg = [P.lsb("neg%d" % d, [128, 128], F32) for d in range(2)]
        P.dma("sync", tri[0][:], C.k_tri[:])
        P.dma("sync", tri[1][:], C.k_triu[:])
        P.dma("sync", neg[0][:], C.k_negf[:])
        P.dma("sync", neg[1][:], C.k_negb[:])
        yacc = P.lsb("yacc", [128, NT, 256], F32)
        P.memset(yacc[:], 0.0, eng="gpsimd")
        xsave = P.lsb("xsave", [128, NT, 256], BF16)
        S = [P.lsb("S%d" % d, [128, 4, 64], F32) for d in range(2)]
        Sb = [P.lsb("Sb%d" % d, [128, 4, 64], BF16) for d in range(2)]
        DL = []
        for d in range(2):
            bankB = P.lps("dbB%d" % d, [128, 512], F32)
            bankC = P.lps("dbC%d" % d, [128, 512], F32)
            DL.append(dict(
                rA=P.lsb("rA%d" % d, [128, 4, 128], F32), tmp=P.lsb("stmp%d" % d, [128, 4, 128], F32),
                LT=P.lsb("LT%d" % d, [128, 4, 128], F32), EB=P.lsb("EB%d" % d, [128, 4, 128], F32),
                MT=P.lsb("MT%d" % d, [128, 4, 128], BF16), CTs=P.lsb("CTs%d" % d, [128, 4, 128], BF16),
                XB=P.lsb("XB%d" % d, [128, 4, 128], BF16), Xw=P.lsb("Xw%d" % d, [128, 4, 64], BF16),
                ncs=P.lsb("ncs%d" % d, [128, 4], F32), tot=P.lsb("tot%d" % d, [128, 4], F32),
                w=P.lsb("w%d" % d, [128, 4], F32),
                pcsB=P.lps("pcsB%d" % d, [128, 4, 128], F32),
                pGT=bankB[:, 0:256].re("p (g l) -> p g l", l=128),
                pst=bankB[:, 256:512].re("p (h c) -> p h c", c=64),
                py=bankC[:, 0:256].re("p (h c) -> p h c", c=64),
                pcs=bankC[:, 256:260],
                pXBt=P.lps("pXB%d" % d, [128, 8, 128], BF16)))
        pXB = DL[0]["pXBt"][:, 0:4, :]

        order = {0: list(range(NT)), 1: [1, 0] + list(range(NT - 1, 1, -1))}
        if stop <= 2:
            return
        for d in range(2):
            P.memset(S[d][:], 0.0)
            P.memset(Sb[d][:], 0.0)

        def sweep(d):
            L = DL[d]
            rA, tmp, LT, EB, MT, CTs, XB, Xw, ncs, tot, w = (L[k] for k in (
                "rA", "tmp", "LT", "EB", "MT", "CTs", "XB", "Xw", "ncs", "tot", "w"))
            pcsB, pGT, pst, py, pcs = L["pcsB"], L["pGT"], L["pst"], L["py"], L["pcs"]
            pXBl = L["pXBt"][:, 0:4, :]
            last = 127 if d == 0 else 0
            cols = slice(d * 4, d * 4 + 4)
            for i in order[d]:
                tk = slice(i * 128, (i + 1) * 128)
                need_y = (i >= 2) or need_ctx
                P.tt(rA[:], tri[d][:].m(lambda a: a.unsqueeze(1).broadcast_to([128, 4, 128])),
                     A[:, i, cols].m(lambda a: a.unsqueeze(2).broadcast_to([128, 4, 128])), ALU.mult, eng=GP)
                yield
                P.mm(pcsB[:].re("p h l -> p (h l)"), ones_f[:], rA[:].re("p h l -> p (h l)"))
                P.mm(pcs, tri[d][:], A[:, i, cols])
                yield
                P.ts(ncs[:], pcs, -1.0, ALU.mult)
                P.ts(EB[:], pcsB[:], -80.0, ALU.max)
                yield
                P.act(EB[:], EB[:], AF.Exp)
                P.copy(tot[:], pcsB[:, :, last])
                yield
                P.tt(w[:], ncs[:], tot[:], ALU.add)
                P.ts(w[:], w[:], -80.0, ALU.max)
                yield
                P.act(w[:], w[:], AF.Exp)
                for j in range(4):
                    P.tr(pXBl[:, j, :], cv[j][:, tk], C.ident_b[:])
                yield
                P.tt(w[:], w[:], dt[:, i, cols], ALU.mult)
                P.copy(XB[:], pXBl)
                Xv = XB[:, 0:2, :].re("p a (b c) -> p (a b) c", c=64)
                if d == 0:
                    P.copy(xsave[:, i, :], XB[:, 0:2, :].re("p a b -> p (a b)"), eng=GP)
                yield
                P.tt(Xw[:], Xv, w[:].m(lambda a: a.unsqueeze(2).broadcast_to([128, 4, 64])), ALU.mult)
                yield
                for h in range(4):
                    P.mm(pst[:, h, :], XB[:, 2 + h // 2, :], Xw[:, h, :])
                yield
                if need_y:
                    P.tt(tmp[:], pcsB[:], neg[d][:].m(lambda a: a.unsqueeze(1).broadcast_to([128, 4, 128])), ALU.add)
                    yield
                    for h in range(4):
                        P.ts(tmp[:, h, :], tmp[:, h, :], ncs[:, h:h + 1], ALU.add, -80.0, ALU.max)
                    yield
                    P.act(LT[:], tmp[:], AF.Exp)
                    for g in range(2):
                        P.mm(pGT[:, g, :], cv[2 + g][:, tk], cv[4 + g][:, tk])
                    yield
                    for h in range(4):
                        P.stt(MT[:, h, :], pGT[:, h // 2, :], dt[:, i, d * 4 + h:d * 4 + h + 1], LT[:, h, :],
                              ALU.mult, ALU.mult)
                    for g in range(2):
                        P.tt(CTs[:, 2 * g:2 * g + 2, :],
                             cv[4 + g][:, tk].m(lambda a: a.unsqueeze(1).broadcast_to([128, 2, 128])),
                             EB[:, 2 * g:2 * g + 2, :], ALU.mult, eng=GP)
                    yield
                    for h in range(4):
                        P.mm(py[:, h, :], MT[:, h, :], Xv[:, h, :], start=True, stop=False)
                        P.mm(py[:, h, :], CTs[:, h, :], Sb[d][:, h, :], start=False, stop=True)
                    yield
                    yv = yacc[:, i, :].re("p (h c) -> p h c", c=64)
                    P.tt(yv, yv, py, ALU.add)
                    yield
                for h in range(4):
                    P.stt(S[d][:, h, :], S[d][:, h, :], EB[:, h, last:last + 1], pst[:, h, :], ALU.mult, ALU.add)
                P.copy(Sb[d][:], S[d][:], eng=GP)
                yield

        run_lanes([lambda li: sweep(0), lambda li: sweep(1)], nlanes=2)
        if stop <= 3:
            return
        dsk = P.lsb("dsk", [128, 4], F32)
        bcast_rows(P, "sync", dsk[:], C.ssd_d[l:l + 1, :])
        gn = P.lsb("gn", [128, 256], F32)
        bcast_rows(P, "sync", gn[:], C.ssd_norm_g[l:l + 1, :])
        zt = [P.lsb("zt%d" % j, [128, 256], F32) for j in range(2)]
        t2 = P.lsb("t2", [128, 256], F32)
        junk = P.lsb("sjunk", [128, 256], F32)
        ss = P.lsb("sss", [128, 1], F32)
        t1 = P.lsb("st1", [128, 1], F32)
        rstd = P.lsb("srstd", [128, 1], F32)
        yo = P.lsb("yo", [128, 256], BF16)
        yT = P.lsb("yT", [128, 2, 128], BF16)
        for i in range(NT):
            if i < 2 and not need_ctx:
                continue
            z = zt[i % 2]
            P.dma("sync", z[:], C.zz[i * 128:(i + 1) * 128, :])
            P.tt(t2[:].re("p (h c) -> p h c", c=64), xsave[:, i, :].re("p (h c) -> p h c", c=64),
                 dsk[:].m(lambda a: a.unsqueeze(2).broadcast_to([128, 4, 64])), ALU.mult)
            P.tt(t2[:], t2[:], yacc[:, i, :], ALU.add)
            P.tt(t2[:], t2[:], z[:], ALU.mult)
            P.act(junk[:], t2[:], AF.Square, accum_out=ss[:])
            rstd_from_ss(P, rstd[:], ss[:], 256, t1[:])
            P.stt(yo[:], t2[:], rstd[:], gn[:], ALU.mult, ALU.mult)
            for j in range(2):
                P.tr(pXB[:, j, :], yo[:, j * 128:(j + 1) * 128], C.ident_b[:])
            P.copy(yT[:], pXB[:, 0:2, :])
            P.dma("sync", C.mixT.part((slice(256, 512), slice(i * 128, (i + 1) * 128))).re("(j p) t -> p j t", p=128), yT[:])


def phase_E(P, C, l, need_ctx):
    lam_init = 0.8 - 0.6 * float(np.exp(-0.3 * l))
    with P.phase():
        lq = P.lsb("lq", [128, 128], F32)
        lk = P.lsb("lk", [128, 128], F32)
        bcast_rows(P, "sync", lq[:], C.da_lam_q[l:l + 1, :])
        bcast_rows(P, "sync", lk[:], C.da_lam_k[l:l + 1, :])
        P.tt(lq[:], lq[:], lk[:], ALU.mult)
        l2 = P.lsb("l2", [128, 2], F32)
        P.reduce(l2[:], lq[:].re("p (a d) -> p a d", d=64), ALU.add)
        P.act(l2[:], l2[:], AF.Exp)
        nlam = P.lsb("nlam", [128, 1], F32)
        P.stt(nlam[:], l2[:, 1:2], -lam_init, l2[:, 0:1], ALU.add, ALU.subtract)
        gs = P.lsb("gs", [128, 128], F32)
        bcast_rows(P, "sync", gs[:], C.da_subln_g[l:l + 1, :])
        P.ts(gs[:], gs[:], 1.0 - lam_init, ALU.mult)

        KT = [P.lsb("KT%d" % j, [128, TALL], BF16) for j in range(2)]
        QT = [P.lsb("QT%d" % j, [128, 2, TALL], BF16) for j in range(2)]
        for j in range(2):
            P.memset(QT[j][64:128, 0, :], 0.0)
            P.memset(QT[j][0:64, 1, :], 0.0)
        VA = [[P.lsb("VA%d_%d" % (j, i), [128, 129], BF16) for i in range(NT)] for j in range(2)]
        for j in range(2):
            for i in range(NT):
                P.memset(VA[j][i][:, 128:129], 1.0, eng=("gpsimd" if i % 2 else "vector"))
        NBUF = 2
        Pt = [P.lsb("Pt%d" % j, [128, 2, 2, 256], BF16) for j in range(NBUF)]
        pS = [P.lps("pS%d" % j, [128, 2, 2, 256], F32) for j in range(NBUF)]
        pOb = [P.lps("pO%d" % q_, [128, 512], F32) for q_ in range(2)]
        pTr = P.lps("pTr", [128, 8, 128], BF16)
        pDum = P.lps("pDum", [128, 512], F32)
        mhalf = P.lsb("mhalf", [128, 1], F32)
        P.memset(mhalf[:], -0.5)
        oacc = [P.lsb("oacc%d" % j, [128, 4, 129], F32) for j in range(2)]
        fin = []
        for j in range(4):
            fin.append(dict(
                r1=P.lsb("r1_%d" % j, [128, 1], F32), r2=P.lsb("r2_%d" % j, [128, 1], F32),
                o1=P.lsb("o1_%d" % j, [128, 128], F32), o2=P.lsb("o2_%d" % j, [128, 128], F32),
                junk=P.lsb("aj_%d" % j, [128, 128], F32), ss=P.lsb("ass_%d" % j, [128, 1], F32),
                t1=P.lsb("at1_%d" % j, [128, 1], F32), rstd=P.lsb("ars_%d" % j, [128, 1], F32),
                ob=P.lsb("aob_%d" % j, [128, 128], BF16), oT=P.lsb("aoT_%d" % j, [128, 128], BF16)))

        def load_head(h):
            j = h % 2
            P.dma("sync", KT[j][:], C.kT[h])
            P.dma("sync", QT[j][0:64, 0, :], C.qT[h][0:64, :])
            P.dma("sync", QT[j][64:128, 1, :], C.qT[h][64:128, :])
            for i in range(NT):
                P.dma("sync", VA[j][i][:, 0:128], C.vv[i * 128:(i + 1) * 128, h * 128:(h + 1) * 128])

        conv = []
        for e in range(N_EXP if C.exp_w_gu is not None else 0):
            r0 = (l * N_EXP + e) * D
            for kc in range(8):
                conv.append((C.wgu_bf.part((slice(e * 128, (e + 1) * 128), slice(kc * 2 * D, (kc + 1) * 2 * D))),
                             C.exp_w_gu[r0 + kc * 128:r0 + (kc + 1) * 128, :]))
            for kc in range(8):
                conv.append((C.wd_bf.part((slice(e * 128, (e + 1) * 128), slice(kc * D, (kc + 1) * D))),
                             C.exp_w_down[r0 + kc * 128:r0 + (kc + 1) * 128, :]))
        conv_pos = [0]

        def trickle(n):
            for _ in range(n):
                if conv_pos[0] < len(conv):
                    o, i_ = conv[conv_pos[0]]
                    conv_pos[0] += 1
                    P.dma("gpsimd", o, i_)

        load_head(0)
        nfin = 0
        for h in range(4):
            if h + 1 < 4:
                load_head(h + 1)
            j = h % 2
            chunks = []
            if need_ctx:
                chunks.append((0, [0, 1]))
            for qc in range(SEQ // 256):
                chunks.append((CTX + qc * 256, list(range(NT))))
            its = []
            for ci, (q0, kts) in enumerate(chunks):
                for ki in range(0, len(kts), 2):
                    its.append((ci, q0, ki // 2, (kts[ki], kts[ki + 1]), len(kts) // 2))

            def issue_S(n):
                ci, q0, ki, ktp, nk = its[n]
                ps = pS[n % NBUF]
                for t_, kt in enumerate(ktp):
                    ks = slice(kt * 128, (kt + 1) * 128)
                    for s_ in range(2):
                        P.mm(ps[:, t_, s_, :], KT[j][:, ks], QT[j][:, s_, q0:q0 + 256])

            deferred = []
            issue_S(0)
            for n, (ci, q0, ki, ktp, nk) in enumerate(its):
                if n + 1 < len(its):
                    issue_S(n + 1)
                ps = pS[n % NBUF]
                pt = Pt[n % NBUF]
                for _ in range(N_DUMMY):
                    P.mm(pDum[:, 0:128], C.ident_b[:], C.ident_b[:])
                P.act(pt[:], ps[:], AF.Exp, scale=0.125)
                for t_, kt in enumerate(ktp):
                    for q_ in range(2):
                        for s_ in range(2):
                            first = (ki == 0 and t_ == 0)
                            P.mm(pOb[q_][:, s_ * 256:s_ * 256 + 129], pt[:, t_, s_, q_ * 128:(q_ + 1) * 128], VA[j][kt][:],
                                 start=(first and s_ == 0), stop=(ki == nk - 1 and t_ == 1),
                                 skip_group_check=True)
                if n % 3 == 0:
                    trickle(1)
                if ki == nk - 1:
                    oa = oacc[ci % 2]
                    for s_ in range(2):
                        for q_ in range(2):
                            P.copy(oa[:, s_ * 2 + q_, :], pOb[q_][:, s_ * 256:s_ * 256 + 129])
                    for q_ in range(2):
                        f = fin[nfin % 4]
                        nfin += 1
                        a1 = oa[:, q_, :]
                        a2 = oa[:, 2 + q_, :]
                        G = "gpsimd"
                        P.recip(f["r1"][:], a1[:, 128:129])
                        P.recip(f["r2"][:], a2[:, 128:129])
                        P.tt(f["r2"][:], f["r2"][:], nlam[:], ALU.mult)
                        P.ts(f["o1"][:], a1[:, 0:128], f["r1"][:], ALU.mult, eng=G)
                        P.stt(f["o2"][:], a2[:, 0:128], f["r2"][:], f["o1"][:], ALU.mult, ALU.add)
                        P.tt(f["junk"][:], f["o2"][:], f["o2"][:], ALU.mult, eng=G)
                        P.reduce(f["ss"][:], f["junk"][:], ALU.add)
                        P.ts(f["t1"][:], f["ss"][:], 1.0 / 128, ALU.mult, EPS, ALU.add)
                        P.tt(f["rstd"][:], f["t1"][:], mhalf[:], ALU.pow, eng=G)
                        P.stt(f["ob"][:], f["o2"][:], f["rstd"][:], gs[:], ALU.mult, ALU.mult)
                        tq = q0 + q_ * 128

                        def late(f=f, tq=tq, h=h):
                            P.tr(pTr[:, 0, :], f["ob"][:], C.ident_b[:])
                            P.copy(f["oT"][:], pTr[:, 0, :])
                            P.dma("sync", C.mixT.part((slice(512 + h * 128, 512 + (h + 1) * 128), slice(tq, tq + 128))),
                                  f["oT"][:])
                        deferred.append((n + 12 + q_, late))
                while deferred and deferred[0][0] <= n:
                    deferred.pop(0)[1]()
            for _, fn_ in deferred:
                fn_()
        trickle(len(conv))


N_DUMMY = 0
NBMAX = (TALL * 4 + N_EXP * (BLK - 1)) // BLK
JMAX = (TALL * 4) // BLK + 1


def phase_MoE(P, C, l, toks, last, stop=99):
    ntok = len(toks) * 128
    NB = (ntok * 4 + N_EXP * (BLK - 1)) // BLK
    NA = BLK // 128
    with P.phase():
        gates = P.lsb("gates", [128, NT, 4], F32)
        dest = P.lsb("dest", [128, NT, 4], I32)
        be = P.lsb("be", [128, NBMAX], F32)
        berow = P.lsb("berow", [128, NBMAX], F32)
        with P.phase():
            wout = P.lsb("wout", [128, 8, D], BF16)
            wov = C.w_out[l].re("(k p) n -> p k n", p=128)
            for kc in range(8):
                P.dma("gpsimd", wout[:, kc, :], wov[:, kc, :])
            rows = {}
            for r in sorted(set(1 if i < 2 else 0 for i in toks)):
                g1 = P.lsb("g1row%d" % r, [128, D], F32)
                s2 = P.lsb("s2row%d" % r, [128, D], F32)
                h2r = P.lsb("h2row%d" % r, [128, D], F32)
                bcast_rows(P, "sync", g1[:], C.modv[r:r + 1, 2 * D:3 * D])
                bcast_rows(P, "sync", h2r[:], C.modv[r:r + 1, 3 * D:4 * D])
                bcast_rows(P, "sync", s2[:], C.modv[r:r + 1, 4 * D:5 * D])
                rows[r] = (g1, s2, h2r)
            rw = P.lsb("rw", [128, 8, N_EXP], F32)
            P.dma("sync", rw[:], C.router_w[l].re("(k p) e -> p k e", p=128))
            rb = P.lsb("rb", [128, N_EXP], F32)
            bcast_rows(P, "sync", rb[:], C.router_b[l:l + 1, :])
            iota = P.lsb("iota", [128, N_EXP], F32)
            P.dma("sync", iota[:], C.k_iota32[:])
            trils = P.lsb("trils", [128, 128], F32)
            P.dma("sync", trils[:], C.k_trils[:])
            ones_f = P.lsb("ones_f2", [128, 128], F32)
            P.memset(ones_f[:], 1.0)
            OH = P.lsb("OH", [128, NT, 4, N_EXP], F32)
            POS = P.lsb("POS", [128, NT, N_EXP], F32)
            Acc = P.lsb("Acc", [128, N_EXP], F32)
            P.memset(Acc[:], 0.0)
            Ai = P.lsb("Ai", [128, N_EXP], F32)
            FL = []
            for li in range(2):
                FL.append(dict(
                    mx=P.lsb("mx%d" % li, [128, 8, 128], BF16), xt=P.lsb("fxt%d" % li, [128, D], F32),
                    tmp=P.lsb("ftmp%d" % li, [128, D], F32), xn=P.lsb("fxn%d" % li, [128, D], F32),
                    junk=P.lsb("fjunk%d" % li, [128, D], F32), h2f=P.lsb("h2f%d" % li, [128, D], F32),
                    h2b=P.lsb("h2b%d" % li, [128, D], BF16), h2T=P.lsb("h2T%d" % li, [128, 8, 128], F32),
                    ss=P.lsb("fss%d" % li, [128, 1], F32), t1=P.lsb("ft1%d" % li, [128, 1], F32),
                    rstd=P.lsb("frstd%d" % li, [128, 1], F32), lg=P.lsb("lg%d" % li, [128, N_EXP], F32),
                    t8=P.lsb("t8%d" % li, [128, 8], F32), i8=P.lsb("i8%d" % li, [128, 8], U32),
                    idxf=P.lsb("idxf%d" % li, [128, 4], F32), e4=P.lsb("e4%d" % li, [128, 4], F32),
                    es=P.lsb("es%d" % li, [128, 1], F32), Ai=P.lsb("Ai%d" % li, [128, N_EXP], F32),
                    po=[P.lps("po%d_%d" % (li, j), [128, 512], F32) for j in range(2)],
                    pTf=P.lps("pTf%d" % li, [128, 4, 128], F32), plp=P.lps("plp%d" % li, [128, 512], F32)))
            fmhalf = P.lsb("fmhalf", [128, 1], F32)
            P.memset(fmhalf[:], -0.5)

            def f_task(i):
                def gen(li):
                    d = FL[li]
                    r = 1 if i < 2 else 0
                    g1, s2, h2r = rows[r]
                    tk = slice(i * 128, (i + 1) * 128)
                    m_, x, tmp, xn, h2f, h2b, h2T = d["mx"], d["xt"], d["tmp"], d["xn"], d["h2f"], d["h2b"], d["h2T"]
                    po, pTf = d["po"], d["pTf"]
                    pl = d["plp"][:, 0:N_EXP]
                    ppos = d["plp"][:, 64:64 + N_EXP]
                    lg, t8, i8, idxf, e4, es, Ai = d["lg"], d["t8"], d["i8"], d["idxf"], d["e4"], d["es"], d["Ai"]
                    P.dma("sync", m_[:], C.mixT[:, tk].re("(k p) t -> p k t", p=128))
                    P.dma("sync", x[:], xrows(C, l, i))
                    for n in range(2):
                        for kc in range(8):
                            P.mm(po[n][:], m_[:, kc, :], wout[:, kc, n * 512:(n + 1) * 512], start=(kc == 0), stop=(kc == 7))
                        yield
                    for n in range(2):
                        P.tt(tmp[:, n * 512:(n + 1) * 512], po[n][:], g1[:, n * 512:(n + 1) * 512], ALU.mult)
                    yield
                    P.tt(xn[:], tmp[:], x[:], ALU.add, eng="gpsimd")
                    P.dma("sync", C.xa.part((tk, slice(None))), xn[:])
                    yield
                    P.act(d["junk"][:], xn[:], AF.Square, accum_out=d["ss"][:])
                    yield
                    P.ts(d["t1"][:], d["ss"][:], 1.0 / D, ALU.mult, EPS, ALU.add)
                    P.tt(d["rstd"][:], d["t1"][:], fmhalf[:], ALU.pow, eng="gpsimd")
                    yield
                    P.stt(tmp[:], xn[:], d["rstd"][:], s2[:], ALU.mult, ALU.mult)
                    yield
                    P.tt(h2f[:], tmp[:], h2r[:], ALU.add)
                    yield
                    P.copy(h2b[:], h2f[:], eng="gpsimd")
                    P.dma("sync", C.h2.sub(i)[tk, :], h2b[:])
                    for hh in range(2):
                        for kc in range(4):
                            P.tr(pTf[:, kc, :], h2f[:, (hh * 4 + kc) * 128:(hh * 4 + kc + 1) * 128], C.ident_f[:])
                        yield
                        P.copy(h2T[:, hh * 4:(hh + 1) * 4, :], pTf[:], eng="scalar")
                        yield
                    for kc in range(8):
                        P.mm(pl, h2T[:, kc, :], rw[:, kc, :], start=(kc == 0), stop=(kc == 7))
                    yield
                    P.tt(lg[:], pl, rb[:], ALU.add)
                    yield
                    t8a, i8a, lga = t8[:].ap, i8[:].ap, lg[:].ap
                    P.op("vector", lambda e, t8a=t8a, lga=lga: e.max(out=t8a, in_=lga), reads=[lg[:]], writes=[t8[:]])
                    P.op("vector", lambda e, t8a=t8a, lga=lga, i8a=i8a: e.max_index(out=i8a, in_max=t8a, in_values=lga),
                         reads=[lg[:], t8[:]], writes=[i8[:]])
                    yield
                    P.ts(e4[:], t8[:, 0:4], t8[:, 0:1], ALU.subtract)
                    P.act(e4[:], e4[:], AF.Exp, accum_out=es[:])
                    yield
                    P.recip(es[:], es[:])
                    P.ts(gates[:, i, :], e4[:], es[:], ALU.mult)
                    yield
                    P.copy(idxf[:], i8[:, 0:4])
                    for k in range(4):
                        P.ts(OH[:, i, k, :], iota[:], idxf[:, k:k + 1], ALU.is_equal)
                    yield
                    P.reduce(Ai[:], OH[:, i, :, :].re("p k e -> p e k"), ALU.add)
                    yield
                    P.mm(ppos, trils[:], Ai[:], start=True, stop=False)
                    P.mm(ppos, ones_f[:], Acc[:], start=False, stop=True)
                    P.copy(POS[:, i, :], ppos)
                    P.tt(Acc[:], Acc[:], Ai[:], ALU.add)
                    yield
                return gen

            run_lanes([f_task(i) for i in toks], nlanes=2)
            ppos_ = FL[0]["plp"]
            ppos = ppos_[:, 64:64 + N_EXP]
            cnt = P.lsb("cnt", [128, N_EXP], F32)
            P.mm(ppos, ones_f[:], Acc[:])
            P.copy(cnt[:], ppos)
            thr = P.lsb("thr", [128, JMAX], F32)
            P.dma("sync", thr[:], C.k_thr[:])
            cmpt = P.lsb("cmpt", [128, N_EXP, JMAX], F32)
            P.tt(cmpt[:], cnt[:].m(lambda a: a.unsqueeze(2).broadcast_to([128, N_EXP, JMAX])),
                 thr[:].m(lambda a: a.unsqueeze(1).broadcast_to([128, N_EXP, JMAX])), ALU.is_gt)
            padded = P.lsb("padded", [128, N_EXP], F32)
            P.reduce(padded[:], cmpt[:], ALU.add)
            P.ts(padded[:], padded[:], float(BLK), ALU.mult)
            pend = P.lsb("pend", [128, N_EXP], F32)
            ones32 = P.lsb("ones32", [128, N_EXP], F32)
            P.memset(ones32[:], 1.0)
            P.scan(pend[:], ones32[:], padded[:], 0.0)
            pstart = P.lsb("pstart", [128, N_EXP], F32)
            P.tt(pstart[:], pend[:], padded[:], ALU.subtract)
            bthr = P.lsb("bthr", [128, NBMAX], F32)
            P.dma("sync", bthr[:], C.k_bthr[:])
            cmpb = P.lsb("cmpb", [128, NBMAX, N_EXP], F32)
            P.tt(cmpb[:], pend[:].m(lambda a: a.unsqueeze(1).broadcast_to([128, NBMAX, N_EXP])),
                 bthr[:].m(lambda a: a.unsqueeze(2).broadcast_to([128, NBMAX, N_EXP])), ALU.is_le)
            P.reduce(be[:], cmpb[:], ALU.add)
            P.ts(be[:], be[:], float(N_EXP - 1), ALU.min)
            P.ts(berow[:], be[:], 128.0, ALU.mult)
            zt = P.lsb("zfill", [128, 8192], BF16)
            P.memset(zt[:], 0.0)
            per = NB * BLK * D // 128
            xv = C.xblk[0:NB * BLK, :].re("(p a) d -> p (a d)", p=128)
            for c0 in range(0, per, 8192):
                n = min(8192, per - c0)
                P.dma("sync", xv[:, c0:c0 + n], zt[:, 0:n])
            base = P.lsb("base", [128, N_EXP], F32)
            prod = P.lsb("prod", [128, 4, N_EXP], F32)
            destf = P.lsb("destf", [128, 4], F32)
            h2t = [P.lsb("h2t%d" % j, [128, D], BF16) for j in range(2)]
            for n_, i in enumerate(toks):
                tk = slice(i * 128, (i + 1) * 128)
                P.tt(base[:], pstart[:], POS[:, i, :], ALU.add)
                P.tt(prod[:], OH[:, i, :, :], base[:].m(lambda a: a.unsqueeze(1).broadcast_to([128, 4, N_EXP])), ALU.mult)
                P.reduce(destf[:], prod[:], ALU.add)
                P.copy(dest[:, i, :], destf[:])
                ht = h2t[n_ % 2]
                P.dma("sync", ht[:], C.h2.sub(i)[tk, :])
                for k in range(4):
                    o_ap = C.xblk[:].ap
                    i_ap = ht[:].ap
                    d_ap = dest[:, i, k:k + 1].ap
                    P.dma("gpsimd", C.xblk.part(slice(None)), ht[:], extra_reads=[dest[:], C.xblk[:]],
                          fn=lambda e, o_ap=o_ap, i_ap=i_ap, d_ap=d_ap: e.indirect_dma_start(
                              out=o_ap, out_offset=bass.IndirectOffsetOnAxis(ap=d_ap, axis=0), in_=i_ap, in_offset=None))
        if stop <= 1:
            return
        with P.phase():
            wgu = [P.lsb("wgu%d" % j, [128, 8, 2 * D], BF16) for j in range(2)]
            wd = [P.lsb("wd%d" % j, [128, 8, D], BF16) for j in range(2)]
            ridx_all = P.lsb("ridx_all", [128, NBMAX], I32)
            ridf = P.lsb("ridf", [128, NBMAX], F32)
            rowidx = P.lsb("rowidx", [128, 1], F32)
            P.dma("sync", rowidx[:], C.k_iotap[:])
            P.ts(ridf[:], berow[:], rowidx[:, 0:1], ALU.add)
            P.copy(ridx_all[:], ridf[:])
            bgu = P.lsb("bgu", [N_EXP, 2 * D], BF16)
            bd = P.lsb("bd", [N_EXP, D], BF16)
            P.dma("gpsimd", bgu[:], C.exp_b_gu[l])
            P.dma("gpsimd", bd[:], C.exp_b_down[l])
            iop = P.lsb("iop", [N_EXP, BLK], F32)
            P.dma("sync", iop[:], C.k_iopb[0:N_EXP, :])
            sel = [P.lsb("sel%d" % j, [N_EXP, BLK], BF16) for j in range(2)]
            xs = [P.lsb("xs%d" % j, [128, NA, D], BF16) for j in range(3)]
            xTs = [P.lsb("xT%d" % j, [128, 8, BLK], BF16) for j in range(2)]
            actT = [P.lsb("actT%d" % m, [128, BLK], BF16) for m in range(8)]
            gg = P.lsb("gg", [128, BLK], F32)
            sg = P.lsb("sg", [128, BLK], F32)
            uu = P.lsb("uu", [128, BLK], F32)
            yb = [P.lsb("yb%d" % j, [128, D], F32) for j in range(2)]
            pT = P.lps("gpT", [128, 8, 128], BF16)
            pg = [P.lps("gpg%d" % j, [128, 512], F32) for j in range(2)]
            pu = [P.lps("gpu%d" % j, [128, 512], F32) for j in range(2)]
            py = [P.lps("gpy%d" % j, [128, 512], F32) for j in range(2)]

            def load_block(b):
                j = b % 2
                for (wt, src) in ((wgu[j], C.wgu_bf), (wd[j], C.wd_bf)):
                    o_ap = wt[:].re("p k n -> p (k n)").ap
                    s_ap = src[:].ap
                    d_ap = ridx_all[:, b:b + 1].ap
                    P.dma("gpsimd", wt[:], src[:], extra_reads=[ridx_all[:]],
                          fn=lambda e, o_ap=o_ap, s_ap=s_ap, d_ap=d_ap: e.indirect_dma_start(
                              out=o_ap, out_offset=None, in_=s_ap,
                              in_offset=bass.IndirectOffsetOnAxis(ap=d_ap, axis=0)))
                P.ts(sel[j][:], iop[:], be[0:N_EXP, b:b + 1], ALU.is_equal)

            def load_x(b):
                P.dma("sync", xs[b % 3][:], C.xblk[b * BLK:(b + 1) * BLK, :].re("(a p) d -> p a d", p=128))

            def prep_x(b):
                jj = b % 2
                for a in range(NA):
                    for kc in range(8):
                        P.tr(pT[:, kc, :], xs[b % 3][:, a, kc * 128:(kc + 1) * 128], C.ident_b[:])
                    P.copy(xTs[jj][:, :, a * 128:(a + 1) * 128], pT[:], eng="scalar")

            load_x(0)
            if NB > 1:
                load_x(1)
            load_block(0)
            prep_x(0)
            ny = 0
            for b in range(NB):
                if b + 2 < NB:
                    load_x(b + 2)
                if b + 1 < NB:
                    load_block(b + 1)
                j = b % 2
                xT = xTs[j]
                for m in range(8):
                    if m == 3 and b + 1 < NB:
                        prep_x(b + 1)
                    pgm = pg[m % 2]
                    pum = pu[m % 2]
                    for (ps, off) in ((pgm, 0), (pum, 1)):
                        for kc in range(8):
                            P.mm(ps[:, 0:BLK], wgu[j][:, kc, m * 256 + off:(m + 1) * 256:2], xT[:, kc, :],
                                 start=(kc == 0), stop=False)
                        P.mm(ps[:, 0:BLK], bgu[:, m * 256 + off:(m + 1) * 256:2], sel[j][:], start=False, stop=True)
                    P.ts(gg[:], pgm[:, 0:BLK], 7.0, ALU.min)
                    P.act(sg[:], gg[:], AF.Sigmoid, scale=1.702)
                    P.ts(uu[:], pum[:, 0:BLK], 7.0, ALU.min, -7.0, ALU.max)
                    P.stt(uu[:], uu[:], 1.0, gg[:], ALU.add, ALU.mult)
                    P.tt(actT[m][:], uu[:], sg[:], ALU.mult)
                for a in range(NA):
                    y = yb[ny % 2]
                    ny += 1
                    for n in range(2):
                        ps = py[n]
                        for m in range(8):
                            P.mm(ps[:], actT[m][:, a * 128:(a + 1) * 128], wd[j][:, m, n * 512:(n + 1) * 512],
                                 start=(m == 0), stop=False)
                        P.mm(ps[:], sel[j][:, a * 128:(a + 1) * 128], bd[:, n * 512:(n + 1) * 512], start=False, stop=True)
                        P.copy(y[:, n * 512:(n + 1) * 512], ps[:], eng=("scalar" if n else "vector"))
                    P.dma("sync", C.yblk.part((slice(b * BLK + a * 128, b * BLK + (a + 1) * 128), slice(None))), y[:])
        if stop <= 2:
            return
        with P.phase():
            rows = {}
            for r in sorted(set(1 if i < 2 else 0 for i in toks)):
                g2 = P.lsb("g2row%d" % r, [128, D], F32)
                bcast_rows(P, "sync", g2[:], C.modv[r:r + 1, 5 * D:6 * D])
                rows[r] = g2
            xat = [P.lsb("xat%d" % j, [128, D], F32) for j in range(2)]
            yk = [[P.lsb("yk%d_%d" % (j, k), [128, D], F32) for k in range(4)] for j in range(2)]
            f = P.lsb("hf_", [128, D], F32)
            xo = [P.lsb("xo%d" % j, [128, D], F32) for j in range(2)]
            def h_loads(n_):
                i = toks[n_]
                tk = slice(i * 128, (i + 1) * 128)
                j = n_ % 2
                P.dma("sync", xat[j][:], C.xa[tk, :])
                for k in range(4):
                    o_ap = yk[j][k][:].ap
                    s_ap = C.yblk[:].ap
                    d_ap = dest[:, i, k:k + 1].ap
                    P.dma("gpsimd", yk[j][k][:], C.yblk[:], extra_reads=[dest[:]],
                          fn=lambda e, o_ap=o_ap, s_ap=s_ap, d_ap=d_ap: e.indirect_dma_start(
                              out=o_ap, out_offset=None, in_=s_ap,
                              in_offset=bass.IndirectOffsetOnAxis(ap=d_ap, axis=0)))

            h_loads(0)
            for n_, i in enumerate(toks):
                r = 1 if i < 2 else 0
                tk = slice(i * 128, (i + 1) * 128)
                j = n_ % 2
                if n_ + 1 < len(toks):
                    h_loads(n_ + 1)
                P.ts(f[:], yk[j][0][:], gates[:, i, 0:1], ALU.mult)
                for k in range(1, 4):
                    P.stt(f[:], yk[j][k][:], gates[:, i, k:k + 1], f[:], ALU.mult, ALU.add)
                P.tt(f[:], f[:], rows[r][:], ALU.mult, eng="gpsimd")
                P.tt(xo[j][:], f[:], xat[j][:], ALU.add)
                if last:
                    P.dma("sync", C.out.part((slice((i - 2) * 128, (i - 1) * 128), slice(None))), xo[j][:])
                else:
                    P.dma("sync", C.xb.part((tk, slice(None))), xo[j][:])


def layer(P, C, l, stop=99):
    need_ctx = l < DEPTH - 1
    last = l == DEPTH - 1
    phase_A(P, C, l)
    phase_B(P, C, l)
    phase_C(P, C, l, need_ctx)
    phase_D(P, C, l, need_ctx)
    phase_E(P, C, l, need_ctx)
    toks = list(range(NT)) if need_ctx else list(range(2, NT))
    phase_MoE(P, C, l, toks, last, stop=stop)


def build_program(layers=(0, 1), ext_in=(), ext_out=(), skip=(), stop=99):
    nc = bass.Bass("TRN2", target_bir_lowering=False)
    with contextlib.ExitStack() as st:
        P = Prog(nc, st)
        C = Ctx()
        declare_io(P, C, ext_in=ext_in, ext_out=ext_out, skip=skip)
        load_consts(P, C)
        for l in layers:
            layer(P, C, l, stop=stop)
        outs = [C.out.all()] + [getattr(C, n).all() for n in ext_out]
        P.finish("sync", outs)
        P.barrier()
        P.emit()
        C.n_ops = P.n_ops
    return nc, C


def kernel(**inputs):
    inputs = {k: np.asarray(v) for k, v in inputs.items()}
    nb = inputs["x"].shape[0]
    nc, C = build_program()
    in_maps = []
    shared = None
    for b in range(nb):
        m = core_inputs(inputs, b)
        if shared is None:
            shared = m
        else:
            for k in m:
                if k not in ("x", "c", "ctx"):
                    m[k] = shared[k]
        in_maps.append({k: v for k, v in m.items() if k in C.in_names})
    res = run_bass_kernel_spmd(nc, in_maps, core_ids=list(range(nb)))
    out = np.stack([np.asarray(r["out"]) for r in res.results], axis=0)
    return out.astype(np.float32)
```

```python
import contextlib
import numpy as np
import concourse.bass as bass
import concourse.mybir as mybir
from concourse.bass_utils import run_bass_kernel_spmd
from concourse.alu_op_type import AluOpType as ALU

AF = mybir.ActivationFunctionType
AX = mybir.AxisListType
F32 = mybir.dt.float32
BF16 = mybir.dt.bfloat16
I32 = mybir.dt.int32
U32 = mybir.dt.uint32

EPOCH = 30000
N_DMA_SEMS = 24


class Dep:
    __slots__ = ("w", "r")

    def __init__(self):
        self.w = None
        self.r = {}


class V:
    __slots__ = ("ap", "deps")

    def __init__(self, ap, deps):
        self.ap = ap
        self.deps = deps

    def __getitem__(self, idx):
        return V(self.ap[idx], self.deps)

    def m(self, fn):
        return V(fn(self.ap), self.deps)

    def re(self, s, **kw):
        return V(self.ap.rearrange(s, **kw), self.deps)

    def bc(self, shape):
        return V(self.ap.broadcast_to(shape), self.deps)

    def bitcast(self, dt):
        return V(self.ap.bitcast(dt), self.deps)


class T:
    def __init__(self, handle, name):
        self.h = handle
        self.name = name
        self.dep = Dep()
        self.subs = {}

    def __getitem__(self, idx):
        return V(self.h[idx], [self.dep])

    def all(self):
        return V(self.h[:], [self.dep] + [s.dep for s in self.subs.values()])

    def part(self, idx):
        return V(self.h[idx], [Dep()])

    def sub(self, key):
        if key not in self.subs:
            self.subs[key] = T(self.h, "%s.%s" % (self.name, key))
        return self.subs[key]


class Prog:
    ENGS = ("tensor", "vector", "scalar", "gpsimd", "sync")

    def __init__(self, nc, stack):
        self.nc = nc
        self.stack = stack
        self.q = {e: [] for e in self.ENGS}
        self.cnt = {e: 0 for e in self.ENGS}
        self.seen = {e: {} for e in self.ENGS}
        self.sems = {}
        self.dma_sems = [stack.enter_context(nc.semaphore("dma%d" % i)) for i in range(2 * N_DMA_SEMS)]
        self.dma_cnt = [0] * (2 * N_DMA_SEMS)
        self.dma_rr = {False: 0, True: 0}
        self.n_ops = 0

    def sb(self, name, shape, dt):
        return T(self.stack.enter_context(self.nc.sbuf_tensor(name, list(shape), dt)), name)

    def ps(self, name, shape, dt=F32):
        return T(self.stack.enter_context(self.nc.psum_tensor(name, list(shape), dt)), name)

    def dram(self, name, shape, dt, kind="Internal"):
        return T(self.nc.dram_tensor(name, list(shape), dt, kind=kind), name)

    def _eng_sem(self, eng, epoch):
        k = (eng, epoch)
        if k not in self.sems:
            self.sems[k] = self.stack.enter_context(self.nc.semaphore("s_%s%d" % (eng, epoch)))
        return k

    def _sem_handle(self, key):
        if key[0] == "dma":
            return self.dma_sems[key[1]]
        return self.sems[key]

    def _collect(self, eng, reads, writes):
        need = {}

        def add(tok):
            if tok is None:
                return
            k, v = tok
            if need.get(k, 0) < v:
                need[k] = v

        for v_ in reads:
            for d in v_.deps:
                add(d.w)
        for v_ in writes:
            for d in v_.deps:
                add(d.w)
                for k, val in d.r.items():
                    add((k, val))
        out = []
        seen = self.seen[eng]
        for k, val in need.items():
            if eng == "tensor" and k[0] == "tensor":
                continue
            if seen.get(k, 0) >= val:
                continue
            seen[k] = val
            out.append((k, val))
        return out

    def _commit(self, tok, reads, writes):
        k, val = tok
        for v_ in reads:
            for d in v_.deps:
                if d.r.get(k, 0) < val:
                    d.r[k] = val
        for v_ in writes:
            for d in v_.deps:
                d.w = tok
                d.r = {}

    def op(self, eng, fn, reads=(), writes=()):
        waits = self._collect(eng, reads, writes)
        self.cnt[eng] += 1
        n = self.cnt[eng]
        epoch = (n - 1) // EPOCH
        k = self._eng_sem(eng, epoch)
        tok = (k, n - epoch * EPOCH)
        self.q[eng].append((waits, fn, k, 1))
        self._commit(tok, reads, writes)
        self.n_ops += 1
        return tok

    def dma(self, eng, out, in_, extra_reads=(), fn=None, **kw):
        reads = [in_] + list(extra_reads)
        writes = [out]
        waits = self._collect(eng, reads, writes)
        sw = (eng == "gpsimd")
        i = self.dma_rr[sw] + (N_DMA_SEMS if sw else 0)
        self.dma_rr[sw] = (self.dma_rr[sw] + 1) % N_DMA_SEMS
        k = ("dma", i)
        if self.dma_cnt[i] > 0 and self.seen[eng].get(k, 0) < self.dma_cnt[i]:
            self.seen[eng][k] = self.dma_cnt[i]
            waits.append((k, self.dma_cnt[i]))
        self.dma_cnt[i] += 16
        tok = (k, self.dma_cnt[i])
        if fn is None:
            o_ap, i_ap = out.ap, in_.ap

            def fn(e, o_ap=o_ap, i_ap=i_ap, kw=kw):
                return e.dma_start(out=o_ap, in_=i_ap, **kw)
        self.q[eng].append((waits, fn, k, 16))
        self._commit(tok, reads, writes)
        self.n_ops += 1
        return tok

    def finish(self, eng, views):
        waits = self._collect(eng, views, ())
        self.q[eng].append((waits, None, None, 0))

    def emit(self):
        nc = self.nc
        with nc.Block() as block:
            def mk(name):
                def body(e):
                    for waits, fn, k, inc in self.q[name]:
                        for wk, wv in waits:
                            e.wait_ge(self._sem_handle(wk), wv)
                        if fn is not None:
                            ins = fn(e)
                            ins.then_inc(self._sem_handle(k), inc)
                return body
            block.tensor(mk("tensor"))
            block.vector(mk("vector"))
            block.scalar(mk("scalar"))
            block.gpsimd(mk("gpsimd"))
            block.sync(mk("sync"))

    def mm(self, out, lhsT, rhs, start=True, stop=True, **kw):
        o, l, r = out.ap, lhsT.ap, rhs.ap
        return self.op("tensor", lambda e: e.matmul(o, l, r, start=start, stop=stop, **kw),
                       reads=[lhsT, rhs], writes=[out])

    def tr(self, out, in_, ident):
        o, i, d = out.ap, in_.ap, ident.ap
        return self.op("tensor", lambda e: e.transpose(o, i, d), reads=[in_, ident], writes=[out])

    def act(self, out, in_, func, bias=None, scale=None, accum_out=None, eng="scalar"):
        o, i = out.ap, in_.ap
        reads = [in_]
        writes = [out]
        kw = {}
        if bias is not None:
            if isinstance(bias, V):
                reads.append(bias)
                kw["bias"] = bias.ap
            else:
                kw["bias"] = bias
        if scale is not None:
            if isinstance(scale, V):
                reads.append(scale)
                kw["scale"] = scale.ap
            else:
                kw["scale"] = scale
        if accum_out is not None:
            writes.append(accum_out)
            kw["accum_out"] = accum_out.ap
        return self.op("scalar", lambda e: e.activation(o, i, func, **kw), reads=reads, writes=writes)

    def tt(self, out, in0, in1, op, eng="vector", extra_reads=()):
        o, a, b = out.ap, in0.ap, in1.ap
        return self.op(eng, lambda e: e.tensor_tensor(out=o, in0=a, in1=b, op=op),
                       reads=[in0, in1] + list(extra_reads), writes=[out])

    def ts(self, out, in0, s1, op0, s2=None, op1=None, accum_out=None, eng="vector"):
        o, a = out.ap, in0.ap
        reads = [in0]
        writes = [out]
        if isinstance(s1, V):
            reads.append(s1)
            s1 = s1.ap
        if isinstance(s2, V):
            reads.append(s2)
            s2 = s2.ap
        kw = {}
        if op1 is not None:
            kw["op1"] = op1
        if accum_out is not None:
            writes.append(accum_out)
            kw["accum_out"] = accum_out.ap
        return self.op(eng, lambda e: e.tensor_scalar(out=o, in0=a, scalar1=s1, scalar2=s2, op0=op0, **kw),
                       reads=reads, writes=writes)

    def stt(self, out, in0, scalar, in1, op0, op1, eng="vector"):
        o, a, b = out.ap, in0.ap, in1.ap
        reads = [in0, in1]
        if isinstance(scalar, V):
            reads.append(scalar)
            scalar = scalar.ap
        return self.op(eng, lambda e: e.scalar_tensor_tensor(out=o, in0=a, scalar=scalar, in1=b, op0=op0, op1=op1),
                       reads=reads, writes=[out])

    def copy(self, out, in_, eng="vector"):
        o, i = out.ap, in_.ap
        if eng == "scalar":
            return self.op(eng, lambda e: e.copy(o, i), reads=[in_], writes=[out])
        return self.op(eng, lambda e: e.tensor_copy(out=o, in_=i), reads=[in_], writes=[out])

    def memset(self, out, val, eng="vector"):
        o = out.ap
        return self.op(eng, lambda e: e.memset(o, val), writes=[out])

    def reduce(self, out, in_, op, axis=AX.X, eng="vector"):
        o, i = out.ap, in_.ap
        return self.op(eng, lambda e: e.tensor_reduce(out=o, in_=i, axis=axis, op=op), reads=[in_], writes=[out])

    def recip(self, out, in_):
        o, i = out.ap, in_.ap
        return self.op("vector", lambda e: e.reciprocal(out=o, in_=i), reads=[in_], writes=[out])

    def scan(self, out, d0, d1, initial, op0=ALU.mult, op1=ALU.add):
        o, a, b = out.ap, d0.ap, d1.ap
        reads = [d0, d1]
        if isinstance(initial, V):
            reads.append(initial)
            initial = initial.ap
        return self.op("vector", lambda e: e.tensor_tensor_scan(out=o, data0=a, data1=b, initial=initial, op0=op0, op1=op1),
                       reads=reads, writes=[out])


def _phase(self):
    prog = self

    class _Ph:
        def __enter__(s):
            s.old = getattr(prog, "stack_local", None)
            s.st = contextlib.ExitStack()
            s.st.__enter__()
            prog.stack_local = s.st
            return s

        def __exit__(s, *a):
            prog.barrier()
            prog.stack_local = s.old
            return s.st.__exit__(*a)
    return _Ph()


def _barrier(self):
    latest = {}
    for e in self.ENGS:
        n = self.cnt[e]
        if n > 0:
            epoch = (n - 1) // EPOCH
            latest[(e, epoch)] = n - epoch * EPOCH
    for i in range(2 * N_DMA_SEMS):
        if self.dma_cnt[i] > 0:
            latest[("dma", i)] = self.dma_cnt[i]
    for e in self.ENGS:
        waits = []
        seen = self.seen[e]
        for k, val in latest.items():
            if k[0] == e:
                continue
            if seen.get(k, 0) >= val:
                continue
            seen[k] = val
            waits.append((k, val))
        if waits:
            self.q[e].append((waits, None, None, 0))


def _lsb(self, name, shape, dt):
    self._uid = getattr(self, "_uid", 0) + 1
    nm = "%s_%d" % (name, self._uid)
    return T(self.stack_local.enter_context(self.nc.sbuf_tensor(nm, list(shape), dt)), nm)


def _lps(self, name, shape, dt=F32):
    self._uid = getattr(self, "_uid", 0) + 1
    nm = "%s_%d" % (name, self._uid)
    return T(self.stack_local.enter_context(self.nc.psum_tensor(nm, list(shape), dt)), nm)


Prog.phase = _phase
Prog.barrier = _barrier
Prog.lsb = _lsb
Prog.lps = _lps

D = 1024
SEQ = 4096
CTX = 256
TALL = SEQ + CTX
NT = TALL // 128
DEPTH = 2
D_IN = 3080
EPS = 1e-6
N_EXP = 32
BLK = 256
FM_COLS = [0, 128, 256, 384, 768, 896, 1024, 1152, 1280, 1408]
OFF_Z, OFF_DT, OFF_Q, OFF_K, OFF_V = 512, 1536, 1544, 2056, 2568


def drow(t, r0, n):
    return t[r0:r0 + n, :]


def bcast_rows(P, eng, dst, src_row_view):
    p = dst.ap.shape[0]
    n = dst.ap.shape[1]
    P.dma(eng, dst, src_row_view.m(lambda a: a.broadcast_to([p, n])))


class Ctx:
    pass


def declare_io(P, C, ext_in=(), ext_out=(), skip=()):
    C.in_names = []

    def inp(name, shape, dt=F32):
        if name in skip:
            return None
        C.in_names.append(name)
        return P.dram(name, shape, dt, kind="ExternalInput")

    def scr(name, shape, dt):
        kind = "Internal"
        if name in ext_in:
            kind = "ExternalInput"
        if name in ext_out:
            kind = "ExternalOutput"
        return P.dram(name, shape, dt, kind=kind)

    C.x = inp("x", [SEQ, D])
    C.c = inp("c", [1, D])
    C.ctx = inp("ctx", [CTX, D])
    C.c_ctx = inp("c_ctx", [1, D])
    C.w_mod = inp("w_mod", [DEPTH, D, 6 * D])
    C.b_mod = inp("b_mod", [DEPTH, 6 * D])
    C.norm1_g = inp("norm1_g", [DEPTH, D])
    C.norm2_g = inp("norm2_g", [DEPTH, D])
    C.w_in = inp("w_in", [DEPTH, D, D_IN])
    C.w_out = inp("w_out", [DEPTH, D, D])
    C.lru_conv_w = inp("lru_conv_w", [DEPTH, 4, 256])
    C.lru_conv_b = inp("lru_conv_b", [DEPTH, 256])
    C.lru_wa = inp("lru_wa", [DEPTH, 2, 4, 64, 64])
    C.lru_ba = inp("lru_ba", [DEPTH, 2, 256])
    C.lru_wx = inp("lru_wx", [DEPTH, 2, 4, 64, 64])
    C.lru_bx = inp("lru_bx", [DEPTH, 2, 256])
    C.lru_lam = inp("lru_lam", [DEPTH, 2, 256])
    C.ssd_conv_w = inp("ssd_conv_w", [DEPTH, 4, 768])
    C.ssd_conv_b = inp("ssd_conv_b", [DEPTH, 768])
    C.ssd_a_log = inp("ssd_a_log", [DEPTH, 8])
    C.ssd_dt_bias = inp("ssd_dt_bias", [DEPTH, 8])
    C.ssd_d = inp("ssd_d", [DEPTH, 4])
    C.ssd_norm_g = inp("ssd_norm_g", [DEPTH, 256])
    C.da_q_norm = inp("da_q_norm", [DEPTH, 64])
    C.da_k_norm = inp("da_k_norm", [DEPTH, 64])
    C.da_lam_q = inp("da_lam_q", [DEPTH, 128])
    C.da_lam_k = inp("da_lam_k", [DEPTH, 128])
    C.da_subln_g = inp("da_subln_g", [DEPTH, 128])
    C.router_w = inp("router_w", [DEPTH, D, N_EXP])
    C.router_b = inp("router_b", [DEPTH, N_EXP])
    C.exp_w_gu = inp("exp_w_gu", [DEPTH * N_EXP * D, 2 * D])
    C.exp_b_gu = inp("exp_b_gu", [DEPTH, N_EXP, 2 * D])
    C.exp_w_down = inp("exp_w_down", [DEPTH * N_EXP * D, D])
    C.exp_b_down = inp("exp_b_down", [DEPTH, N_EXP, D])
    C.k_ident = inp("k_ident", [128, 128])
    C.k_tri = inp("k_tri", [128, 128])
    C.k_triu = inp("k_triu", [128, 128])
    C.k_trils = inp("k_trils", [128, 128])
    C.k_negf = inp("k_negf", [128, 128])
    C.k_negb = inp("k_negb", [128, 128])
    C.k_cos = inp("k_cos", [128, SEQ // 128, 32])
    C.k_sin = inp("k_sin", [128, SEQ // 128, 32])
    C.k_iota32 = inp("k_iota32", [128, 32])
    C.k_iotap = inp("k_iotap", [128, 1])
    C.k_rowidx = inp("k_rowidx", [128, 8])
    C.k_iopb = inp("k_iopb", [128, BLK])
    C.k_thr = inp("k_thr", [128, (TALL * 4) // BLK + 1])
    C.k_bthr = inp("k_bthr", [128, (TALL * 4 + N_EXP * (BLK - 1)) // BLK])

    C.out = P.dram("out", [SEQ, D], F32, kind="ExternalOutput")
    C.modv = scr("modv", [2, 6 * D], F32)
    C.fm = scr("fm", [10 * 128, TALL], F32)
    C.qT = scr("qT", [4, 128, TALL], BF16)
    C.kT = scr("kT", [4, 128, TALL], BF16)
    C.vv = scr("vv", [TALL, 512], BF16)
    C.zz = scr("zz", [TALL, 256], F32)
    C.dtr = scr("dtr", [TALL, 8], F32)
    C.mixT = scr("mixT", [D, TALL], BF16)
    C.xa = scr("xa", [TALL, D], F32)
    C.xb = scr("xb", [TALL, D], F32)
    C.h2 = scr("h2", [TALL, D], BF16)
    nslots = ((TALL * 4 + N_EXP * (BLK - 1)) // BLK) * BLK
    C.nslots = nslots
    C.xblk = scr("xblk", [nslots, D], BF16)
    C.yblk = scr("yblk", [nslots, D], F32)
    C.wgu_bf = scr("wgu_bf", [N_EXP * 128, 8 * 2 * D], BF16)
    C.wd_bf = scr("wd_bf", [N_EXP * 128, 8 * D], BF16)


def load_consts(P, C):
    C.dt_sb = P.sb("dt_sb", [128, NT, 8], F32)
    C.ident_f = P.sb("ident_f", [128, 128], F32)
    C.ident_b = P.sb("ident_b", [128, 128], BF16)
    P.dma("sync", C.ident_f[:], C.k_ident[:])
    P.copy(C.ident_b[:], C.ident_f[:])


def xrows(C, l, i):
    if l == 0:
        if i < 2:
            return C.ctx[i * 128:(i + 1) * 128, :]
        return C.x[(i - 2) * 128:(i - 1) * 128, :]
    return C.xb[i * 128:(i + 1) * 128, :]


def phase_A(P, C, l):
    with P.phase():
        cc = P.lsb("cc", [128, 2, 8], F32)
        cs = P.lsb("cs", [128, 2, 8], F32)
        P.dma("sync", cc[:, 0, :], C.c[0:1, :].re("o (p k) -> (o p) k", k=8))
        P.dma("sync", cc[:, 1, :], C.c_ctx[0:1, :].re("o (p k) -> (o p) k", k=8))
        P.act(cs[:], cc[:], AF.Silu)
        bm = P.lsb("bm", [2, 6 * D], F32)
        P.dma("sync", bm[0:1, :], C.b_mod[l:l + 1, :])
        P.dma("sync", bm[1:2, :], C.b_mod[l:l + 1, :])
        ng = P.lsb("ng", [2, 2, D], F32)
        for r in range(2):
            P.dma("sync", ng[r:r + 1, 0, :], C.norm1_g[l:l + 1, :])
            P.dma("sync", ng[r:r + 1, 1, :], C.norm2_g[l:l + 1, :])
        mrow = P.lsb("mrow", [2, 6 * D], F32)
        wm = [P.lsb("wm%d" % j, [128, 8, 512], F32) for j in range(2)]
        pm = [P.lps("pm%d" % j, [2, 512], F32) for j in range(2)]
        wv = C.w_mod[l].re("(p k) n -> p k n", k=8)
        for j in range(12):
            w = wm[j % 2]
            P.dma("sync", w[:], wv[:, :, j * 512:(j + 1) * 512])
            ps = pm[j % 2]
            for k in range(8):
                P.mm(ps[:], cs[:, :, k], w[:, k, :], start=(k == 0), stop=(k == 7))
            P.tt(mrow[:, j * 512:(j + 1) * 512], ps[:], bm[:, j * 512:(j + 1) * 512], ALU.add)
        P.stt(mrow[:, D:2 * D], mrow[:, D:2 * D], 1.0, ng[:, 0, :], ALU.add, ALU.mult)
        P.stt(mrow[:, 4 * D:5 * D], mrow[:, 4 * D:5 * D], 1.0, ng[:, 1, :], ALU.add, ALU.mult)
        P.dma("sync", C.modv[:], mrow[:])


def rstd_from_ss(P, rstd, ss, n, tmp):
    P.ts(tmp, ss, 1.0 / n, ALU.mult, EPS, ALU.add)
    P.act(tmp, tmp, AF.Sqrt)
    P.recip(rstd, tmp)


B_MODE = 0
B_LANES = 2
B_CUT = 99


def run_lanes(tasks, nlanes=2):
    lanes = [None] * nlanes
    it = iter(tasks)
    pending = True
    while True:
        for li in range(nlanes):
            if lanes[li] is None and pending:
                f = next(it, None)
                if f is None:
                    pending = False
                else:
                    lanes[li] = f(li)
        if all(g is None for g in lanes):
            break
        for li in range(nlanes):
            g = lanes[li]
            if g is None:
                continue
            try:
                next(g)
            except StopIteration:
                lanes[li] = None


def phase_B(P, C, l):
    with P.phase():
        win = P.lsb("win", [128, 8, D_IN + 264], BF16)
        wv = C.w_in[l].re("(k p) n -> p k n", p=128)
        for kc in range(8):
            for ci, (c0, c1) in enumerate(((0, 1540), (1540, 3080))):
                P.dma("gpsimd", win[:, kc, c0:c1], wv[:, kc, c0:c1])
            P.dma("gpsimd", win[:, kc, D_IN:D_IN + 256], wv[:, kc, OFF_Z:OFF_Z + 256])
            P.dma("gpsimd", win[:, kc, D_IN + 256:D_IN + 264], wv[:, kc, OFF_DT:OFF_DT + 8])
        winv = win.all()
        rows = {}
        for r in range(2):
            sh = P.lsb("shrow%d" % r, [128, D], F32)
            sc = P.lsb("scrow%d" % r, [128, D], F32)
            bcast_rows(P, "sync", sh[:], C.modv[r:r + 1, 0:D])
            bcast_rows(P, "sync", sc[:], C.modv[r:r + 1, D:2 * D])
            rows[r] = (sh, sc)
        gq = P.lsb("gq", [128, 64], F32)
        gk = P.lsb("gk", [128, 64], F32)
        bcast_rows(P, "sync", gq[:], C.da_q_norm[l:l + 1, :])
        bcast_rows(P, "sync", gk[:], C.da_k_norm[l:l + 1, :])
        cos = P.lsb("cos", [128, SEQ // 128, 32], F32)
        sin = P.lsb("sin", [128, SEQ // 128, 32], F32)
        P.dma("sync", cos[:], C.k_cos[:])
        P.dma("sync", sin[:], C.k_sin[:])
        mhalf = P.lsb("bmhalf", [128, 8], F32)
        P.memset(mhalf[:], -0.5)

        NL = 2
        L = []
        for li in range(NL):
            d = dict(
                xt=P.lsb("xt%d" % li, [128, D], F32), junk=P.lsb("junk%d" % li, [128, D], F32),
                hf=P.lsb("hf%d" % li, [128, D], F32), hb=P.lsb("hb%d" % li, [128, D], BF16),
                ss=P.lsb("ss%d" % li, [128, 1], F32), t1=P.lsb("t1%d" % li, [128, 1], F32),
                rstd=P.lsb("rstd%d" % li, [128, 1], F32),
                qsq=P.lsb("qsq%d" % li, [128, 512], F32), qss=P.lsb("qss%d" % li, [128, 8], F32),
                qt8=P.lsb("qt8%d" % li, [128, 8], F32), qrs=P.lsb("qrs%d" % li, [128, 8], F32),
                qn=P.lsb("qn%d" % li, [128, 8, 64], F32), ra=P.lsb("ra%d" % li, [128, 8, 32], F32),
                rb=P.lsb("rb%d" % li, [128, 8, 32], F32), rc=P.lsb("rc%d" % li, [128, 8, 32], F32),
                rd=P.lsb("rd%d" % li, [128, 8, 32], F32), qr=P.lsb("qr%d" % li, [128, 8, 64], BF16),
                qTs=P.lsb("qTs%d" % li, [128, 4, 128], BF16), vb=P.lsb("vb%d" % li, [128, 512], BF16),
                zs=P.lsb("zs%d" % li, [128, 256], F32), dts=P.lsb("dts%d" % li, [128, 8], F32),
                pA=P.lps("pA%d" % li, [128, 8, 128], BF16), pB=P.lps("pB%d" % li, [128, 512], F32),
                pC=P.lps("pC%d" % li, [128, 512], F32))
            L.append(d)
        hTs = [P.lsb("hT%d" % j, [128, 8, 512], BF16) for j in range(2)]
        fms = [P.lsb("fms%d" % j, [128, 512], F32) for j in range(2)]
        pfm = [P.lps("pfm%d" % j, [128, 512], F32) for j in range(2)]

        def qk_post(d, psum, gain, dst, i, t0):
            latent = i >= 2
            qsq, qss, qt8, qrs, qn, ra, rb, rc, rd, qr, qTs, pA = (d[k] for k in (
                "qsq", "qss", "qt8", "qrs", "qn", "ra", "rb", "rc", "rd", "qr", "qTs", "pA"))
            P.act(qsq[:], psum[:], AF.Square)
            yield
            P.reduce(qss[:], qsq[:].re("p (g d) -> p g d", d=64), ALU.add)
            rstd_from_ss(P, qrs[:], qss[:], 64, qt8[:])
            yield
            P.tt(qn[:], psum[:].re("p (g d) -> p g d", d=64),
                 qrs[:].m(lambda a: a.unsqueeze(2).broadcast_to([128, 8, 64])), ALU.mult)
            gb = gain[:].m(lambda a: a.unsqueeze(1).broadcast_to([128, 8, 64]))
            yield
            if not latent:
                P.tt(qr[:], qn[:], gb, ALU.mult)
            else:
                P.tt(qn[:], qn[:], gb, ALU.mult)
                yield
                cb = cos[:, i - 2, :].m(lambda a: a.unsqueeze(1).broadcast_to([128, 8, 32]))
                sb_ = sin[:, i - 2, :].m(lambda a: a.unsqueeze(1).broadcast_to([128, 8, 32]))
                q1 = qn[:, :, 0:32]
                q2 = qn[:, :, 32:64]
                P.tt(ra[:], q1, cb, ALU.mult)
                P.tt(rc[:], q2, cb, ALU.mult, eng="gpsimd")
                yield
                P.tt(rb[:], q2, sb_, ALU.mult)
                P.tt(rd[:], q1, sb_, ALU.mult, eng="gpsimd")
                yield
                P.tt(qr[:, :, 0:32], ra[:], rb[:], ALU.subtract)
                P.tt(qr[:, :, 32:64], rc[:], rd[:], ALU.add, eng="gpsimd")
            yield
            qrf = qr[:].re("p g d -> p (g d)")
            for h in range(4):
                P.tr(pA[:, h, :], qrf[:, h * 128:(h + 1) * 128], C.ident_b[:])
            yield
            P.copy(qTs[:], pA[:, 0:4, :])
            P.dma("sync", dst.part(slice(None)).re("h p t -> p h t")[:, :, t0:t0 + 128], qTs[:])
            yield

        def tile_task(i, gi, j):
            def gen(li):
                d = L[li]
                r = 1 if i < 2 else 0
                sh, sc = rows[r]
                hT = hTs[gi % 2].sub(j)
                t0 = i * 128
                x = d["xt"]
                P.dma("sync", x[:], xrows(C, l, i))
                P.act(d["junk"][:], x[:], AF.Square, accum_out=d["ss"][:])
                yield
                rstd_from_ss(P, d["rstd"][:], d["ss"][:], D, d["t1"][:])
                yield
                P.stt(d["hf"][:], x[:], d["rstd"][:], sc[:], ALU.mult, ALU.mult)
                yield
                P.tt(d["hb"][:], d["hf"][:], sh[:], ALU.add)
                yield
                for kc in range(8):
                    P.tr(d["pA"][:, kc, :], d["hb"][:, kc * 128:(kc + 1) * 128], C.ident_b[:])
                yield
                P.copy(hT[:, :, j * 128:(j + 1) * 128], d["pA"][:])
                yield
                hs = hT[:, :, j * 128:(j + 1) * 128]

                def proj(ps, c0, n, o0):
                    for kc in range(8):
                        P.mm(ps[:, o0:o0 + n], hs[:, kc, :], winv[:, kc, c0:c0 + n], start=(kc == 0), stop=(kc == 7))
                if B_CUT <= 1:
                    return
                proj(d["pB"], OFF_Q, 512, 0)
                yield
                proj(d["pC"], OFF_K, 512, 0)
                yield
                if B_CUT <= 2:
                    return
                for _ in qk_post(d, d["pB"], gq, C.qT, i, t0):
                    yield
                if B_CUT <= 3:
                    return
                proj(d["pB"], OFF_V, 512, 0)
                yield
                for _ in qk_post(d, d["pC"], gk, C.kT, i, t0):
                    yield
                if B_CUT <= 4:
                    return
                proj(d["pC"], D_IN, 264, 0)
                yield
                P.copy(d["vb"][:], d["pB"][:], eng="scalar")
                P.dma("sync", C.vv.part((slice(t0, t0 + 128), slice(None))), d["vb"][:])
                yield
                if B_CUT <= 5:
                    return
                P.act(d["zs"][:], d["pC"][:, 0:256], AF.Silu)
                P.dma("sync", C.zz.part((slice(t0, t0 + 128), slice(None))), d["zs"][:])
                if B_CUT <= 6:
                    return
                P.copy(C.dt_sb[:, i, :], d["pC"][:, 256:264], eng="scalar")
                yield
            return gen

        def fm_task(gi, grp):
            def gen(li):
                hT = hTs[gi % 2].all()
                ntok = 128 * len(grp)
                t0 = grp[0] * 128
                for ci, c0 in enumerate(FM_COLS):
                    ps = pfm[ci % 2]
                    for kc in range(8):
                        P.mm(ps[:, 0:ntok], winv[:, kc, c0:c0 + 128], hT[:, kc, 0:ntok], start=(kc == 0), stop=(kc == 7))
                    f = fms[ci % 2]
                    P.copy(f[:, 0:ntok], ps[:, 0:ntok], eng=("scalar" if ci % 2 else "vector"))
                    P.dma("sync", C.fm.part((slice(ci * 128, (ci + 1) * 128), slice(t0, t0 + ntok))), f[:, 0:ntok])
                    yield
            return gen

        groups = [[0, 1]] + [[2 + 4 * g + j for j in range(4)] for g in range(8)]
        tasks = []
        pending_fm = None
        for gi, grp in enumerate(groups):
            for j, i in enumerate(grp):
                tasks.append(tile_task(i, gi, j))
                if j == 1 and pending_fm is not None:
                    tasks.append(pending_fm)
                    pending_fm = None
            pending_fm = fm_task(gi, grp)
        def nop_task(li):
            return iter(())
        tasks.append(lambda li: iter(()))
        tasks.append(lambda li: iter(()))
        if B_MODE == 0:
            run_lanes(tasks, nlanes=2)
            run_lanes([pending_fm], nlanes=1)
        else:
            for gi, grp in enumerate(groups):
                run_lanes([tile_task(i, gi, j) for j, i in enumerate(grp)], nlanes=B_LANES)
                run_lanes([fm_task(gi, grp)], nlanes=1)


def host_consts():
    k = {}
    k["k_ident"] = np.eye(128, dtype=np.float32)
    a = np.arange(128)
    k["k_tri"] = (a[:, None] <= a[None, :]).astype(np.float32)
    k["k_triu"] = (a[:, None] >= a[None, :]).astype(np.float32)
    k["k_trils"] = (a[:, None] < a[None, :]).astype(np.float32)
    k["k_negf"] = np.where(a[None, :] >= a[:, None], 0.0, -30000.0).astype(np.float32)
    k["k_negb"] = np.where(a[None, :] <= a[:, None], 0.0, -30000.0).astype(np.float32)
    t = np.arange(SEQ)
    r = (t // 64).astype(np.float32)
    col = (t % 64).astype(np.float32)
    inv = (10000.0 ** (-np.arange(16, dtype=np.float32) / 16)).astype(np.float32)
    ang = np.concatenate([r[:, None] * inv[None, :], col[:, None] * inv[None, :]], axis=-1).astype(np.float32)
    cos = np.cos(ang).astype(np.float32).reshape(SEQ // 128, 128, 32).transpose(1, 0, 2)
    sin = np.sin(ang).astype(np.float32).reshape(SEQ // 128, 128, 32).transpose(1, 0, 2)
    k["k_cos"] = np.ascontiguousarray(cos)
    k["k_sin"] = np.ascontiguousarray(sin)
    k["k_iota32"] = np.tile(np.arange(32, dtype=np.float32)[None, :], (128, 1))
    k["k_iotap"] = np.arange(128, dtype=np.float32).reshape(128, 1)
    k["k_rowidx"] = (np.arange(8)[None, :] * 128 + np.arange(128)[:, None]).astype(np.float32)
    k["k_iopb"] = np.tile(np.arange(128, dtype=np.float32)[:, None], (1, BLK))
    k["k_thr"] = np.tile((np.arange((TALL * 4) // BLK + 1, dtype=np.float32) * BLK)[None, :], (128, 1))
    nbmax = (TALL * 4 + N_EXP * (BLK - 1)) // BLK
    k["k_bthr"] = np.tile((np.arange(nbmax, dtype=np.float32) * BLK)[None, :], (128, 1))
    return k


def core_inputs(inputs, b, big=True):
    m = {}
    m["x"] = np.ascontiguousarray(inputs["x"][b])
    m["c"] = np.ascontiguousarray(inputs["c"][b:b + 1])
    m["ctx"] = np.ascontiguousarray(inputs["ctx"][b])
    m["c_ctx"] = np.ascontiguousarray(inputs["c_ctx"].reshape(1, D))
    for k in ("w_mod", "b_mod", "norm1_g", "norm2_g", "w_in", "w_out", "lru_conv_w", "lru_conv_b",
              "lru_wa", "lru_ba", "lru_wx", "lru_bx", "lru_lam", "ssd_conv_w", "ssd_conv_b",
              "ssd_norm_g", "da_q_norm", "da_k_norm", "da_subln_g", "router_w", "router_b",
              "exp_b_gu", "exp_b_down", "ssd_d"):
        m[k] = np.ascontiguousarray(inputs[k])
    m["ssd_a_log"] = np.ascontiguousarray(inputs["ssd_a_log"].reshape(DEPTH, 8))
    m["ssd_dt_bias"] = np.ascontiguousarray(inputs["ssd_dt_bias"].reshape(DEPTH, 8))
    m["da_lam_q"] = np.ascontiguousarray(inputs["da_lam_q"].reshape(DEPTH, 128))
    m["da_lam_k"] = np.ascontiguousarray(inputs["da_lam_k"].reshape(DEPTH, 128))
    if big:
        m["exp_w_gu"] = inputs["exp_w_gu"].reshape(DEPTH * N_EXP * D, 2 * D)
        m["exp_w_down"] = inputs["exp_w_down"].reshape(DEPTH * N_EXP * D, D)
    m.update(host_consts())
    return m


def phase_C(P, C, l, need_ctx):
    with P.phase():
        LMAX = SEQ
        B = [P.lsb("lb%d" % j, [128, LMAX + 4], F32) for j in range(6)]
        xcb = P.lsb("xcb", [128, LMAX], BF16)
        yb = P.lsb("ylru", [128, LMAX], BF16)
        pg = [P.lps("pg%d" % j, [128, 512], F32) for j in range(2)]
        for ct in range(2):
            ch = slice(ct * 128, (ct + 1) * 128)
            cw = P.lsb("cw", [128, 4], F32)
            cbias = P.lsb("cbias", [128, 1], F32)
            P.dma("sync", cw[:], C.lru_conv_w[l][:, ch].re("j c -> c j"), allow_slow_non_contiguous=True)
            P.dma("sync", cbias[:], C.lru_conv_b[l:l + 1, ch].re("o c -> c o"), allow_slow_non_contiguous=True)
            wg = {}
            bg = {}
            sp = {}
            for d in range(2):
                for gi, (wsrc, bsrc) in enumerate(((C.lru_wa, C.lru_ba), (C.lru_wx, C.lru_bx))):
                    wf = P.lsb("wf", [128, 128], F32)
                    P.memset(wf[:], 0.0)
                    for hh in range(2):
                        P.dma("sync", wf[hh * 64:(hh + 1) * 64, hh * 64:(hh + 1) * 64],
                              wsrc[l][d][2 * ct + hh])
                    wb = P.lsb("wb", [128, 128], BF16)
                    P.copy(wb[:], wf[:])
                    wg[(d, gi)] = wb
                    bb = P.lsb("bb", [128, 1], F32)
                    P.dma("sync", bb[:], bsrc[l][d:d + 1, ch].re("o c -> c o"), allow_slow_non_contiguous=True)
                    bg[(d, gi)] = bb
                lam = P.lsb("lam", [128, 1], F32)
                P.dma("sync", lam[:], C.lru_lam[l][d:d + 1, ch].re("o c -> c o"), allow_slow_non_contiguous=True)
                e1 = P.lsb("e1", [128, 1], F32)
                P.act(e1[:], lam[:], AF.Exp, scale=-1.0)
                P.act(e1[:], e1[:], AF.Ln, bias=1.0)
                s8 = P.lsb("s8", [128, 1], F32)
                s16 = P.lsb("s16", [128, 1], F32)
                P.ts(s8[:], e1[:], -8.0, ALU.mult)
                P.ts(s16[:], e1[:], -16.0, ALU.mult)
                sp[d] = (s8, s16)
            h0 = {0: None, 1: None}
            hfin = [P.lsb("hfin%d" % d, [128, 1], F32) for d in range(2)]
            for (t0, L, is_ctx) in ((0, CTX, True), (CTX, SEQ, False)):
                xp, xc, b2, b3, b4, b5 = B
                P.memset(xp[:, 0:2], 0.0)
                P.memset(xp[:, L + 2:L + 4], 0.0)
                P.dma("sync", xp[:, 2:L + 2], C.fm[(2 + ct) * 128:(3 + ct) * 128, t0:t0 + L])
                P.ts(xc[:, 0:L], xp[:, 0:L], cw[:, 0:1], ALU.mult, cbias[:], ALU.add)
                for j in range(1, 4):
                    P.stt(xc[:, 0:L], xp[:, j:j + L], cw[:, j:j + 1], xc[:, 0:L], ALU.mult, ALU.add)
                P.copy(xcb[:, 0:L], xc[:, 0:L], eng="gpsimd")
                hs = {}
                for d in range(2):
                    if d == 0:
                        br, bi, ba = b2, b3, b4
                    else:
                        br, bi, ba = b3, b4, b5
                    nchunk = (L + 511) // 512
                    for gi, dst in ((0, br), (1, bi)):
                        for cix in range(nchunk):
                            n = min(512, L - cix * 512)
                            ps = pg[(cix + gi) % 2]
                            P.mm(ps[:, 0:n], wg[(d, gi)][:], xcb[:, cix * 512:cix * 512 + n])
                            P.act(dst[:, cix * 512:cix * 512 + n], ps[:, 0:n], AF.Sigmoid, bias=bg[(d, gi)][:])
                    s8, s16 = sp[d]
                    P.tt(bi[:, 0:L], bi[:, 0:L], xc[:, 0:L], ALU.mult, eng="gpsimd")
                    P.act(ba[:, 0:L], br[:, 0:L], AF.Exp, scale=s8[:])
                    P.act(br[:, 0:L], br[:, 0:L], AF.Exp, scale=s16[:])
                    P.act(br[:, 0:L], br[:, 0:L], AF.Sqrt, scale=-1.0, bias=1.0)
                    P.tt(bi[:, 0:L], bi[:, 0:L], br[:, 0:L], ALU.mult)
                    init = 0.0 if is_ctx else hfin[d][:]
                    if d == 0:
                        P.scan(br[:, 0:L], ba[:, 0:L], bi[:, 0:L], init)
                        if is_ctx:
                            P.copy(hfin[0][:], br[:, L - 1:L])
                    else:
                        P.scan(br[:, 0:L][:, ::-1], ba[:, 0:L][:, ::-1], bi[:, 0:L][:, ::-1], init)
                        if is_ctx:
                            P.copy(hfin[1][:], br[:, 0:1])
                    hs[d] = br
                if is_ctx and not need_ctx:
                    continue
                P.tt(b2[:, 0:L], b2[:, 0:L], b3[:, 0:L], ALU.add)
                P.dma("sync", b4[:, 0:L], C.fm[ct * 128:(ct + 1) * 128, t0:t0 + L])
                P.tt(b5[:, 0:L], b4[:, 0:L], b4[:, 0:L], ALU.mult, eng="gpsimd")
                P.ts(b5[:, 0:L], b5[:, 0:L], 0.044715, ALU.mult, 1.0, ALU.add)
                P.tt(b5[:, 0:L], b5[:, 0:L], b4[:, 0:L], ALU.mult, eng="gpsimd")
                P.act(b5[:, 0:L], b5[:, 0:L], AF.Sigmoid, scale=1.5957691216057308)
                P.tt(b4[:, 0:L], b4[:, 0:L], b5[:, 0:L], ALU.mult, eng="gpsimd")
                P.tt(yb[:, 0:L], b2[:, 0:L], b4[:, 0:L], ALU.mult)
                P.dma("sync", C.mixT.part((slice(ct * 128, (ct + 1) * 128), slice(t0, t0 + L))), yb[:, 0:L])


def phase_D(P, C, l, need_ctx, stop=99, nog=False):
    GP = "vector" if nog else "gpsimd"
    with P.phase():
        cv = [P.lsb("cv%d" % j, [128, TALL], BF16) for j in range(6)]
        with P.phase():
            xp = P.lsb("sxp", [128, SEQ + 4], F32)
            acc = P.lsb("sacc", [128, SEQ], F32)
            for j in range(6):
                ch = slice(j * 128, (j + 1) * 128)
                cw = P.lsb("scw", [128, 4], F32)
                cbias = P.lsb("scb", [128, 1], F32)
                P.dma("sync", cw[:], C.ssd_conv_w[l][:, ch].re("j c -> c j"), allow_slow_non_contiguous=True)
                P.dma("sync", cbias[:], C.ssd_conv_b[l:l + 1, ch].re("o c -> c o"), allow_slow_non_contiguous=True)
                for (t0, L) in ((0, CTX), (CTX, SEQ)):
                    P.memset(xp[:, 0:2], 0.0)
                    P.memset(xp[:, L + 2:L + 4], 0.0)
                    P.dma("sync", xp[:, 2:L + 2], C.fm[(4 + j) * 128:(5 + j) * 128, t0:t0 + L])
                    P.ts(acc[:, 0:L], xp[:, 0:L], cw[:, 0:1], ALU.mult, cbias[:], ALU.add)
                    for t in range(1, 4):
                        P.stt(acc[:, 0:L], xp[:, t:t + L], cw[:, t:t + 1], acc[:, 0:L], ALU.mult, ALU.add)
                    P.act(cv[j][:, t0:t0 + L], acc[:, 0:L], AF.Silu)
        if stop <= 1:
            return
        dt = P.lsb("dt", [128, NT, 8], F32)
        A = P.lsb("A", [128, NT, 8], F32)
        brow = P.lsb("brow", [128, 8], F32)
        arow = P.lsb("arow", [128, 8], F32)
        P.copy(dt[:], C.dt_sb[:], eng="gpsimd")
        bcast_rows(P, "sync", brow[:], C.ssd_dt_bias[l:l + 1, :])
        bcast_rows(P, "sync", arow[:], C.ssd_a_log[l:l + 1, :])
        P.tt(dt[:], dt[:], brow[:].m(lambda a: a.unsqueeze(1).broadcast_to([128, NT, 8])), ALU.add)
        P.act(dt[:], dt[:], AF.Exp)
        P.act(dt[:], dt[:], AF.Ln, bias=1.0)
        P.act(arow[:], arow[:], AF.Exp)
        P.ts(arow[:], arow[:], -1.0, ALU.mult)
        P.tt(A[:], dt[:], arow[:].m(lambda a: a.unsqueeze(1).broadcast_to([128, NT, 8])), ALU.mult)
        ones_f = P.lsb("ones_f", [128, 128], F32)
        P.memset(ones_f[:], 1.0)
        tri = [P.lsb("tri%d" % d, [128, 128], F32) for d in range(2)]
        neg = [P.lsb("neg%d" % d, [128, 128], F32) for d in range(2)]
        P.dma("sync", tri[0][:], C.k_tri[:])
        P.dma("sync", tri[1][:], C.k_triu[:])
        P.dma("sync", neg[0][:], C.k_negf[:])
        P.dma("sync", neg[1][:], C.k_negb[:])
        yacc = P.lsb("yacc", [128, NT, 256], F32)
        P.memset(yacc[:], 0.0, eng="gpsimd")
        xsave = P.lsb("xsave", [128, NT, 256], BF16)
        S = [P.lsb("S%d" % d, [128, 4, 64], F32) for d in range(2)]
        Sb = [P.lsb("Sb%d" % d, [128, 4, 64], BF16) for d in range(2)]
        DL = []
        for d in range(2):
            bankB = P.lps("dbB%d" % d, [128, 512], F32)
            bankC = P.lps("dbC%d" % d, [128, 512], F32)
            DL.append(dict(
                rA=P.lsb("rA%d" % d, [128, 4, 128], F32), tmp=P.lsb("stmp%d" % d, [128, 4, 128], F32),
                LT=P.lsb("LT%d" % d, [128, 4, 128], F32), EB=P.lsb("EB%d" % d, [128, 4, 128], F32),
                MT=P.lsb("MT%d" % d, [128, 4, 128], BF16), CTs=P.lsb("CTs%d" % d, [128, 4, 128], BF16),
                XB=P.lsb("XB%d" % d, [128, 4, 128], BF16), Xw=P.lsb("Xw%d" % d, [128, 4, 64], BF16),
                ncs=P.lsb("ncs%d" % d, [128, 4], F32), tot=P.lsb("tot%d" % d, [128, 4], F32),
                w=P.lsb("w%d" % d, [128, 4], F32),
                pcsB=P.lps("pcsB%d" % d, [128, 4, 128], F32),
                pGT=bankB[:, 0:256].re("p (g l) -> p g l", l=128),
                pst=bankB[:, 256:512].re("p (h c) -> p h c", c=64),
                py=bankC[:, 0:256].re("p (h c) -> p h c", c=64),
                pcs=bankC[:, 256:260],
                pXBt=P.lps("pXB%d" % d, [128, 8, 128], BF16)))
        pXB = DL[0]["pXBt"][:, 0:4, :]

        order = {0: list(range(NT)), 1: [1, 0] + list(range(NT - 1, 1, -1))}
        if stop <= 2:
            return
        for d in range(2):
            P.memset(S[d][:], 0.0)
            P.memset(Sb[d][:], 0.0)

        def sweep(d):
            L = DL[d]
            rA, tmp, LT, EB, MT, CTs, XB, Xw, ncs, tot, w = (L[k] for k in (
                "rA", "tmp", "LT", "EB", "MT", "CTs", "XB", "Xw", "ncs", "tot", "w"))
            pcsB, pGT, pst, py, pcs = L["pcsB"], L["pGT"], L["pst"], L["py"], L["pcs"]
            pXBl = L["pXBt"][:, 0:4, :]
            last = 127 if d == 0 else 0
            cols = slice(d * 4, d * 4 + 4)
            for i in order[d]:
                tk = slice(i * 128, (i + 1) * 128)
                need_y = (i >= 2) or need_ctx
                P.tt(rA[:], tri[d][:].m(lambda a: a.unsqueeze(1).broadcast_to([128, 4, 128])),
                     A[:, i, cols].m(lambda a: a.unsqueeze(2).broadcast_to([128, 4, 128])), ALU.mult, eng=GP)
                yield
                P.mm(pcsB[:].re("p h l -> p (h l)"), ones_f[:], rA[:].re("p h l -> p (h l)"))
                P.mm(pcs, tri[d][:], A[:, i, cols])
                yield
                P.ts(ncs[:], pcs, -1.0, ALU.mult)
                P.ts(EB[:], pcsB[:], -80.0, ALU.max)
                yield
                P.act(EB[:], EB[:], AF.Exp)
                P.copy(tot[:], pcsB[:, :, last])
                yield
                P.tt(w[:], ncs[:], tot[:], ALU.add)
                P.ts(w[:], w[:], -80.0, ALU.max)
                yield
                P.act(w[:], w[:], AF.Exp)
                for j in range(4):
                    P.tr(pXBl[:, j, :], cv[j][:, tk], C.ident_b[:])
                yield
                P.tt(w[:], w[:], dt[:, i, cols], ALU.mult)
                P.copy(XB[:], pXBl)
                Xv = XB[:, 0:2, :].re("p a (b c) -> p (a b) c", c=64)
                if d == 0:
                    P.copy(xsave[:, i, :], XB[:, 0:2, :].re("p a b -> p (a b)"), eng=GP)
                yield
                P.tt(Xw[:], Xv, w[:].m(lambda a: a.unsqueeze(2).broadcast_to([128, 4, 64])), ALU.mult)
                yield
                for h in range(4):
                    P.mm(pst[:, h, :], XB[:, 2 + h // 2, :], Xw[:, h, :])
                yield
                if need_y:
                    P.tt(tmp[:], pcsB[:], neg[d][:].m(lambda a: a.unsqueeze(1).broadcast_to([128, 4, 128])), ALU.add)
                    yield
                    for h in range(4):
                        P.ts(tmp[:, h, :], tmp[:, h, :], ncs[:, h:h + 1], ALU.add, -80.0, ALU.max)
                    yield
                    P.act(LT[:], tmp[:], AF.Exp)
                    for g in range(2):
                        P.mm(pGT[:, g, :], cv[2 + g][:, tk], cv[4 + g][:, tk])
                    yield
                    for h in range(4):
                        P.stt(MT[:, h, :], pGT[:, h // 2, :], dt[:, i, d * 4 + h:d * 4 + h + 1], LT[:, h, :],
                              ALU.mult, ALU.mult)
                    for g in range(2):
                        P.tt(CTs[:, 2 * g:2 * g + 2, :],
                             cv[4 + g][:, tk].m(lambda a: a.unsqueeze(1).broadcast_to([128, 2, 128])),
                             EB[:, 2 * g:2 * g + 2, :], ALU.mult, eng=GP)
                    yield
                    for h in range(4):
                        P.mm(py[:, h, :], MT[:, h, :], Xv[:, h, :], start=True, stop=False)
                        P.mm(py[:, h, :], CTs[:, h, :], Sb[d][:, h, :], start=False, stop=True)
                    yield
                    yv = yacc.sub(i)[:, i, :].re("p (h c) -> p h c", c=64)
                    P.tt(yv, yv, py, ALU.add, extra_reads=[yacc[:, 0:1, 0:1]])
                    yield
                for h in range(4):
                    P.stt(S[d][:, h, :], S[d][:, h, :], EB[:, h, last:last + 1], pst[:, h, :], ALU.mult, ALU.add)
                P.copy(Sb[d][:], S[d][:], eng=GP)
                yield

        run_lanes([lambda li: sweep(0), lambda li: sweep(1)], nlanes=2)
        if stop <= 3:
            return
        dsk = P.lsb("dsk", [128, 4], F32)
        bcast_rows(P, "sync", dsk[:], C.ssd_d[l:l + 1, :])
        gn = P.lsb("gn", [128, 256], F32)
        bcast_rows(P, "sync", gn[:], C.ssd_norm_g[l:l + 1, :])
        zt = [P.lsb("zt%d" % j, [128, 256], F32) for j in range(2)]
        t2 = P.lsb("t2", [128, 256], F32)
        junk = P.lsb("sjunk", [128, 256], F32)
        ss = P.lsb("sss", [128, 1], F32)
        t1 = P.lsb("st1", [128, 1], F32)
        rstd = P.lsb("srstd", [128, 1], F32)
        yo = P.lsb("yo", [128, 256], BF16)
        yT = P.lsb("yT", [128, 2, 128], BF16)
        for i in range(NT):
            if i < 2 and not need_ctx:
                continue
            z = zt[i % 2]
            P.dma("sync", z[:], C.zz[i * 128:(i + 1) * 128, :])
            P.tt(t2[:].re("p (h c) -> p h c", c=64), xsave[:, i, :].re("p (h c) -> p h c", c=64),
                 dsk[:].m(lambda a: a.unsqueeze(2).broadcast_to([128, 4, 64])), ALU.mult)
            P.tt(t2[:], t2[:], yacc.sub(i)[:, i, :], ALU.add)
            P.tt(t2[:], t2[:], z[:], ALU.mult)
            P.act(junk[:], t2[:], AF.Square, accum_out=ss[:])
            rstd_from_ss(P, rstd[:], ss[:], 256, t1[:])
            P.stt(yo[:], t2[:], rstd[:], gn[:], ALU.mult, ALU.mult)
            for j in range(2):
                P.tr(pXB[:, j, :], yo[:, j * 128:(j + 1) * 128], C.ident_b[:])
            P.copy(yT[:], pXB[:, 0:2, :])
            P.dma("sync", C.mixT.part((slice(256, 512), slice(i * 128, (i + 1) * 128))).re("(j p) t -> p j t", p=128), yT[:])


def phase_E(P, C, l, need_ctx):
    lam_init = 0.8 - 0.6 * float(np.exp(-0.3 * l))
    with P.phase():
        lq = P.lsb("lq", [128, 128], F32)
        lk = P.lsb("lk", [128, 128], F32)
        bcast_rows(P, "sync", lq[:], C.da_lam_q[l:l + 1, :])
        bcast_rows(P, "sync", lk[:], C.da_lam_k[l:l + 1, :])
        P.tt(lq[:], lq[:], lk[:], ALU.mult)
        l2 = P.lsb("l2", [128, 2], F32)
        P.reduce(l2[:], lq[:].re("p (a d) -> p a d", d=64), ALU.add)
        P.act(l2[:], l2[:], AF.Exp)
        nlam = P.lsb("nlam", [128, 1], F32)
        P.stt(nlam[:], l2[:, 1:2], -lam_init, l2[:, 0:1], ALU.add, ALU.subtract)
        gs = P.lsb("gs", [128, 128], F32)
        bcast_rows(P, "sync", gs[:], C.da_subln_g[l:l + 1, :])
        P.ts(gs[:], gs[:], 1.0 - lam_init, ALU.mult)

        KT = [P.lsb("KT%d" % j, [128, TALL], BF16) for j in range(2)]
        QT = [P.lsb("QT%d" % j, [128, 2, TALL], BF16) for j in range(2)]
        for j in range(2):
            P.memset(QT[j][64:128, 0, :], 0.0)
            P.memset(QT[j][0:64, 1, :], 0.0)
        VA = [[P.lsb("VA%d_%d" % (j, i), [128, 129], BF16) for i in range(NT)] for j in range(2)]
        for j in range(2):
            for i in range(NT):
                P.memset(VA[j][i][:, 128:129], 1.0, eng=("gpsimd" if i % 2 else "vector"))
        NBUF = 2
        Pt = [P.lsb("Pt%d" % j, [128, 2, 2, 256], BF16) for j in range(NBUF)]
        pS = [P.lps("pS%d" % j, [128, 2, 2, 256], F32) for j in range(NBUF)]
        pOb = [P.lps("pO%d" % q_, [128, 512], F32) for q_ in range(2)]
        pTr = P.lps("pTr", [128, 8, 128], BF16)
        pDum = P.lps("pDum", [128, 512], F32)
        mhalf = P.lsb("mhalf", [128, 1], F32)
        P.memset(mhalf[:], -0.5)
        oacc = [P.lsb("oacc%d" % j, [128, 4, 129], F32) for j in range(2)]
        fin = []
        for j in range(4):
            fin.append(dict(
                r1=P.lsb("r1_%d" % j, [128, 1], F32), r2=P.lsb("r2_%d" % j, [128, 1], F32),
                o1=P.lsb("o1_%d" % j, [128, 128], F32), o2=P.lsb("o2_%d" % j, [128, 128], F32),
                junk=P.lsb("aj_%d" % j, [128, 128], F32), ss=P.lsb("ass_%d" % j, [128, 1], F32),
                t1=P.lsb("at1_%d" % j, [128, 1], F32), rstd=P.lsb("ars_%d" % j, [128, 1], F32),
                ob=P.lsb("aob_%d" % j, [128, 128], BF16), oT=P.lsb("aoT_%d" % j, [128, 128], BF16)))

        def load_head(h):
            j = h % 2
            P.dma("sync", KT[j][:], C.kT[h])
            P.dma("sync", QT[j][0:64, 0, :], C.qT[h][0:64, :])
            P.dma("sync", QT[j][64:128, 1, :], C.qT[h][64:128, :])
            for i in range(NT):
                P.dma("sync", VA[j][i][:, 0:128], C.vv[i * 128:(i + 1) * 128, h * 128:(h + 1) * 128])

        conv = []
        for e in range(N_EXP if C.exp_w_gu is not None else 0):
            r0 = (l * N_EXP + e) * D
            for kc in range(8):
                conv.append((C.wgu_bf.part((slice(e * 128, (e + 1) * 128), slice(kc * 2 * D, (kc + 1) * 2 * D))),
                             C.exp_w_gu[r0 + kc * 128:r0 + (kc + 1) * 128, :]))
            for kc in range(8):
                conv.append((C.wd_bf.part((slice(e * 128, (e + 1) * 128), slice(kc * D, (kc + 1) * D))),
                             C.exp_w_down[r0 + kc * 128:r0 + (kc + 1) * 128, :]))
        conv_pos = [0]

        def trickle(n):
            for _ in range(n):
                if conv_pos[0] < len(conv):
                    o, i_ = conv[conv_pos[0]]
                    conv_pos[0] += 1
                    P.dma("gpsimd", o, i_)

        load_head(0)
        nfin = 0
        for h in range(4):
            if h + 1 < 4:
                load_head(h + 1)
            j = h % 2
            chunks = []
            if need_ctx:
                chunks.append((0, [0, 1]))
            for qc in range(SEQ // 256):
                chunks.append((CTX + qc * 256, list(range(NT))))
            its = []
            for ci, (q0, kts) in enumerate(chunks):
                for ki in range(0, len(kts), 2):
                    its.append((ci, q0, ki // 2, (kts[ki], kts[ki + 1]), len(kts) // 2))

            def issue_S(n):
                ci, q0, ki, ktp, nk = its[n]
                ps = pS[n % NBUF]
                for t_, kt in enumerate(ktp):
                    ks = slice(kt * 128, (kt + 1) * 128)
                    for s_ in range(2):
                        P.mm(ps[:, t_, s_, :], KT[j][:, ks], QT[j][:, s_, q0:q0 + 256])

            deferred = []
            issue_S(0)
            for n, (ci, q0, ki, ktp, nk) in enumerate(its):
                if n + 1 < len(its):
                    issue_S(n + 1)
                ps = pS[n % NBUF]
                pt = Pt[n % NBUF]
                for _ in range(N_DUMMY):
                    P.mm(pDum[:, 0:128], C.ident_b[:], C.ident_b[:])
                P.act(pt[:], ps[:], AF.Exp, scale=0.125)
                for t_, kt in enumerate(ktp):
                    for q_ in range(2):
                        for s_ in range(2):
                            first = (ki == 0 and t_ == 0)
                            P.mm(pOb[q_][:, s_ * 256:s_ * 256 + 129], pt[:, t_, s_, q_ * 128:(q_ + 1) * 128], VA[j][kt][:],
                                 start=(first and s_ == 0), stop=(ki == nk - 1 and t_ == 1),
                                 skip_group_check=True)
                if n % 3 == 0:
                    trickle(1)
                if ki == nk - 1:
                    oa = oacc[ci % 2]
                    for s_ in range(2):
                        for q_ in range(2):
                            P.copy(oa[:, s_ * 2 + q_, :], pOb[q_][:, s_ * 256:s_ * 256 + 129])
                    for q_ in range(2):
                        f = fin[nfin % 4]
                        nfin += 1
                        a1 = oa[:, q_, :]
                        a2 = oa[:, 2 + q_, :]
                        G = "gpsimd"
                        P.recip(f["r1"][:], a1[:, 128:129])
                        P.recip(f["r2"][:], a2[:, 128:129])
                        P.tt(f["r2"][:], f["r2"][:], nlam[:], ALU.mult)
                        P.ts(f["o1"][:], a1[:, 0:128], f["r1"][:], ALU.mult, eng=G)
                        P.stt(f["o2"][:], a2[:, 0:128], f["r2"][:], f["o1"][:], ALU.mult, ALU.add)
                        P.tt(f["junk"][:], f["o2"][:], f["o2"][:], ALU.mult, eng=G)
                        P.reduce(f["ss"][:], f["junk"][:], ALU.add)
                        P.ts(f["t1"][:], f["ss"][:], 1.0 / 128, ALU.mult, EPS, ALU.add)
                        P.tt(f["rstd"][:], f["t1"][:], mhalf[:], ALU.pow, eng=G)
                        P.stt(f["ob"][:], f["o2"][:], f["rstd"][:], gs[:], ALU.mult, ALU.mult)
                        tq = q0 + q_ * 128

                        def late(f=f, tq=tq, h=h):
                            P.tr(pTr[:, 0, :], f["ob"][:], C.ident_b[:])
                            P.copy(f["oT"][:], pTr[:, 0, :])
                            P.dma("sync", C.mixT.part((slice(512 + h * 128, 512 + (h + 1) * 128), slice(tq, tq + 128))),
                                  f["oT"][:])
                        deferred.append((n + 12 + q_, late))
                while deferred and deferred[0][0] <= n:
                    deferred.pop(0)[1]()
            for _, fn_ in deferred:
                fn_()
        trickle(len(conv))


N_DUMMY = 0
NBMAX = (TALL * 4 + N_EXP * (BLK - 1)) // BLK
JMAX = (TALL * 4) // BLK + 1


def phase_MoE(P, C, l, toks, last, stop=99):
    ntok = len(toks) * 128
    NB = (ntok * 4 + N_EXP * (BLK - 1)) // BLK
    NA = BLK // 128
    with P.phase():
        gates = P.lsb("gates", [128, NT, 4], F32)
        dest = P.lsb("dest", [128, NT, 4], I32)
        be = P.lsb("be", [128, NBMAX], F32)
        berow = P.lsb("berow", [128, NBMAX], F32)
        with P.phase():
            wout = P.lsb("wout", [128, 8, D], BF16)
            wov = C.w_out[l].re("(k p) n -> p k n", p=128)
            for kc in range(8):
                P.dma("gpsimd", wout[:, kc, :], wov[:, kc, :])
            rows = {}
            for r in sorted(set(1 if i < 2 else 0 for i in toks)):
                g1 = P.lsb("g1row%d" % r, [128, D], F32)
                s2 = P.lsb("s2row%d" % r, [128, D], F32)
                h2r = P.lsb("h2row%d" % r, [128, D], F32)
                bcast_rows(P, "sync", g1[:], C.modv[r:r + 1, 2 * D:3 * D])
                bcast_rows(P, "sync", h2r[:], C.modv[r:r + 1, 3 * D:4 * D])
                bcast_rows(P, "sync", s2[:], C.modv[r:r + 1, 4 * D:5 * D])
                rows[r] = (g1, s2, h2r)
            rw = P.lsb("rw", [128, 8, N_EXP], F32)
            P.dma("sync", rw[:], C.router_w[l].re("(k p) e -> p k e", p=128))
            rb = P.lsb("rb", [128, N_EXP], F32)
            bcast_rows(P, "sync", rb[:], C.router_b[l:l + 1, :])
            iota = P.lsb("iota", [128, N_EXP], F32)
            P.dma("sync", iota[:], C.k_iota32[:])
            trils = P.lsb("trils", [128, 128], F32)
            P.dma("sync", trils[:], C.k_trils[:])
            ones_f = P.lsb("ones_f2", [128, 128], F32)
            P.memset(ones_f[:], 1.0)
            OH = P.lsb("OH", [128, NT, 4, N_EXP], F32)
            POS = P.lsb("POS", [128, NT, N_EXP], F32)
            Acc = P.lsb("Acc", [128, N_EXP], F32)
            P.memset(Acc[:], 0.0)
            Ai = P.lsb("Ai", [128, N_EXP], F32)
            FL = []
            for li in range(2):
                FL.append(dict(
                    mx=P.lsb("mx%d" % li, [128, 8, 128], BF16), xt=P.lsb("fxt%d" % li, [128, D], F32),
                    tmp=P.lsb("ftmp%d" % li, [128, D], F32), xn=P.lsb("fxn%d" % li, [128, D], F32),
                    junk=P.lsb("fjunk%d" % li, [128, D], F32), h2f=P.lsb("h2f%d" % li, [128, D], F32),
                    h2b=P.lsb("h2b%d" % li, [128, D], BF16), h2T=P.lsb("h2T%d" % li, [128, 8, 128], F32),
                    ss=P.lsb("fss%d" % li, [128, 1], F32), t1=P.lsb("ft1%d" % li, [128, 1], F32),
                    rstd=P.lsb("frstd%d" % li, [128, 1], F32), lg=P.lsb("lg%d" % li, [128, N_EXP], F32),
                    t8=P.lsb("t8%d" % li, [128, 8], F32), i8=P.lsb("i8%d" % li, [128, 8], U32),
                    idxf=P.lsb("idxf%d" % li, [128, 4], F32), e4=P.lsb("e4%d" % li, [128, 4], F32),
                    es=P.lsb("es%d" % li, [128, 1], F32), Ai=P.lsb("Ai%d" % li, [128, N_EXP], F32),
                    po=[P.lps("po%d_%d" % (li, j), [128, 512], F32) for j in range(2)],
                    pTf=P.lps("pTf%d" % li, [128, 4, 128], F32), plp=P.lps("plp%d" % li, [128, 512], F32)))
            fmhalf = P.lsb("fmhalf", [128, 1], F32)
            P.memset(fmhalf[:], -0.5)

            def f_task(i):
                def gen(li):
                    d = FL[li]
                    r = 1 if i < 2 else 0
                    g1, s2, h2r = rows[r]
                    tk = slice(i * 128, (i + 1) * 128)
                    m_, x, tmp, xn, h2f, h2b, h2T = d["mx"], d["xt"], d["tmp"], d["xn"], d["h2f"], d["h2b"], d["h2T"]
                    po, pTf = d["po"], d["pTf"]
                    pl = d["plp"][:, 0:N_EXP]
                    ppos = d["plp"][:, 64:64 + N_EXP]
                    lg, t8, i8, idxf, e4, es, Ai = d["lg"], d["t8"], d["i8"], d["idxf"], d["e4"], d["es"], d["Ai"]
                    P.dma("sync", m_[:], C.mixT[:, tk].re("(k p) t -> p k t", p=128))
                    P.dma("sync", x[:], xrows(C, l, i))
                    for n in range(2):
                        for kc in range(8):
                            P.mm(po[n][:], m_[:, kc, :], wout[:, kc, n * 512:(n + 1) * 512], start=(kc == 0), stop=(kc == 7))
                        yield
                    for n in range(2):
                        P.tt(tmp[:, n * 512:(n + 1) * 512], po[n][:], g1[:, n * 512:(n + 1) * 512], ALU.mult)
                    yield
                    P.tt(xn[:], tmp[:], x[:], ALU.add, eng="gpsimd")
                    P.dma("sync", C.xa.part((tk, slice(None))), xn[:])
                    yield
                    P.act(d["junk"][:], xn[:], AF.Square, accum_out=d["ss"][:])
                    yield
                    P.ts(d["t1"][:], d["ss"][:], 1.0 / D, ALU.mult, EPS, ALU.add)
                    P.tt(d["rstd"][:], d["t1"][:], fmhalf[:], ALU.pow, eng="gpsimd")
                    yield
                    P.stt(tmp[:], xn[:], d["rstd"][:], s2[:], ALU.mult, ALU.mult)
                    yield
                    P.tt(h2f[:], tmp[:], h2r[:], ALU.add)
                    yield
                    P.copy(h2b[:], h2f[:], eng="gpsimd")
                    P.dma("sync", C.h2.sub(i)[tk, :], h2b[:])
                    for hh in range(2):
                        for kc in range(4):
                            P.tr(pTf[:, kc, :], h2f[:, (hh * 4 + kc) * 128:(hh * 4 + kc + 1) * 128], C.ident_f[:])
                        yield
                        P.copy(h2T[:, hh * 4:(hh + 1) * 4, :], pTf[:], eng="scalar")
                        yield
                    for kc in range(8):
                        P.mm(pl, h2T[:, kc, :], rw[:, kc, :], start=(kc == 0), stop=(kc == 7))
                    yield
                    P.tt(lg[:], pl, rb[:], ALU.add)
                    yield
                    t8a, i8a, lga = t8[:].ap, i8[:].ap, lg[:].ap
                    P.op("vector", lambda e, t8a=t8a, lga=lga: e.max(out=t8a, in_=lga), reads=[lg[:]], writes=[t8[:]])
                    P.op("vector", lambda e, t8a=t8a, lga=lga, i8a=i8a: e.max_index(out=i8a, in_max=t8a, in_values=lga),
                         reads=[lg[:], t8[:]], writes=[i8[:]])
                    yield
                    P.ts(e4[:], t8[:, 0:4], t8[:, 0:1], ALU.subtract)
                    P.act(e4[:], e4[:], AF.Exp, accum_out=es[:])
                    yield
                    P.recip(es[:], es[:])
                    P.ts(gates[:, i, :], e4[:], es[:], ALU.mult)
                    yield
                    P.copy(idxf[:], i8[:, 0:4])
                    for k in range(4):
                        P.ts(OH[:, i, k, :], iota[:], idxf[:, k:k + 1], ALU.is_equal)
                    yield
                    P.reduce(Ai[:], OH[:, i, :, :].re("p k e -> p e k"), ALU.add)
                    yield
                    P.mm(ppos, trils[:], Ai[:], start=True, stop=False)
                    P.mm(ppos, ones_f[:], Acc[:], start=False, stop=True)
                    P.copy(POS[:, i, :], ppos)
                    P.tt(Acc[:], Acc[:], Ai[:], ALU.add)
                    yield
                return gen

            run_lanes([f_task(i) for i in toks], nlanes=2)
            ppos_ = FL[0]["plp"]
            ppos = ppos_[:, 64:64 + N_EXP]
            cnt = P.lsb("cnt", [128, N_EXP], F32)
            P.mm(ppos, ones_f[:], Acc[:])
            P.copy(cnt[:], ppos)
            thr = P.lsb("thr", [128, JMAX], F32)
            P.dma("sync", thr[:], C.k_thr[:])
            cmpt = P.lsb("cmpt", [128, N_EXP, JMAX], F32)
            P.tt(cmpt[:], cnt[:].m(lambda a: a.unsqueeze(2).broadcast_to([128, N_EXP, JMAX])),
                 thr[:].m(lambda a: a.unsqueeze(1).broadcast_to([128, N_EXP, JMAX])), ALU.is_gt)
            padded = P.lsb("padded", [128, N_EXP], F32)
            P.reduce(padded[:], cmpt[:], ALU.add)
            P.ts(padded[:], padded[:], float(BLK), ALU.mult)
            pend = P.lsb("pend", [128, N_EXP], F32)
            ones32 = P.lsb("ones32", [128, N_EXP], F32)
            P.memset(ones32[:], 1.0)
            P.scan(pend[:], ones32[:], padded[:], 0.0)
            pstart = P.lsb("pstart", [128, N_EXP], F32)
            P.tt(pstart[:], pend[:], padded[:], ALU.subtract)
            bthr = P.lsb("bthr", [128, NBMAX], F32)
            P.dma("sync", bthr[:], C.k_bthr[:])
            cmpb = P.lsb("cmpb", [128, NBMAX, N_EXP], F32)
            P.tt(cmpb[:], pend[:].m(lambda a: a.unsqueeze(1).broadcast_to([128, NBMAX, N_EXP])),
                 bthr[:].m(lambda a: a.unsqueeze(2).broadcast_to([128, NBMAX, N_EXP])), ALU.is_le)
            P.reduce(be[:], cmpb[:], ALU.add)
            P.ts(be[:], be[:], float(N_EXP - 1), ALU.min)
            P.ts(berow[:], be[:], 128.0, ALU.mult)
            zt = P.lsb("zfill", [128, 8192], BF16)
            P.memset(zt[:], 0.0)
            per = NB * BLK * D // 128
            xv = C.xblk[0:NB * BLK, :].re("(p a) d -> p (a d)", p=128)
            for c0 in range(0, per, 8192):
                n = min(8192, per - c0)
                P.dma("sync", xv[:, c0:c0 + n], zt[:, 0:n])
            base = P.lsb("base", [128, N_EXP], F32)
            prod = P.lsb("prod", [128, 4, N_EXP], F32)
            destf = P.lsb("destf", [128, 4], F32)
            h2t = [P.lsb("h2t%d" % j, [128, D], BF16) for j in range(2)]
            for n_, i in enumerate(toks):
                tk = slice(i * 128, (i + 1) * 128)
                P.tt(base[:], pstart[:], POS[:, i, :], ALU.add)
                P.tt(prod[:], OH[:, i, :, :], base[:].m(lambda a: a.unsqueeze(1).broadcast_to([128, 4, N_EXP])), ALU.mult)
                P.reduce(destf[:], prod[:], ALU.add)
                P.copy(dest[:, i, :], destf[:])
                ht = h2t[n_ % 2]
                P.dma("sync", ht[:], C.h2.sub(i)[tk, :])
                for k in range(4):
                    o_ap = C.xblk[:].ap
                    i_ap = ht[:].ap
                    d_ap = dest[:, i, k:k + 1].ap
                    P.dma("gpsimd", C.xblk.part(slice(None)), ht[:], extra_reads=[dest[:], C.xblk[:]],
                          fn=lambda e, o_ap=o_ap, i_ap=i_ap, d_ap=d_ap: e.indirect_dma_start(
                              out=o_ap, out_offset=bass.IndirectOffsetOnAxis(ap=d_ap, axis=0), in_=i_ap, in_offset=None))
        if stop <= 1:
            return
        with P.phase():
            wgu = [P.lsb("wgu%d" % j, [128, 8, 2 * D], BF16) for j in range(2)]
            wd = [P.lsb("wd%d" % j, [128, 8, D], BF16) for j in range(2)]
            ridx_all = P.lsb("ridx_all", [128, NBMAX], I32)
            ridf = P.lsb("ridf", [128, NBMAX], F32)
            rowidx = P.lsb("rowidx", [128, 1], F32)
            P.dma("sync", rowidx[:], C.k_iotap[:])
            P.ts(ridf[:], berow[:], rowidx[:, 0:1], ALU.add)
            P.copy(ridx_all[:], ridf[:])
            bgu = P.lsb("bgu", [N_EXP, 2 * D], BF16)
            bd = P.lsb("bd", [N_EXP, D], BF16)
            P.dma("gpsimd", bgu[:], C.exp_b_gu[l])
            P.dma("gpsimd", bd[:], C.exp_b_down[l])
            iop = P.lsb("iop", [N_EXP, BLK], F32)
            P.dma("sync", iop[:], C.k_iopb[0:N_EXP, :])
            sel = [P.lsb("sel%d" % j, [N_EXP, BLK], BF16) for j in range(2)]
            xs = [P.lsb("xs%d" % j, [128, NA, D], BF16) for j in range(3)]
            xTs = [P.lsb("xT%d" % j, [128, 8, BLK], BF16) for j in range(2)]
            actT = [P.lsb("actT%d" % m, [128, BLK], BF16) for m in range(8)]
            gg = P.lsb("gg", [128, BLK], F32)
            sg = P.lsb("sg", [128, BLK], F32)
            uu = P.lsb("uu", [128, BLK], F32)
            yb = [P.lsb("yb%d" % j, [128, D], F32) for j in range(2)]
            pT = P.lps("gpT", [128, 8, 128], BF16)
            pg = [P.lps("gpg%d" % j, [128, 512], F32) for j in range(2)]
            pu = [P.lps("gpu%d" % j, [128, 512], F32) for j in range(2)]
            py = [P.lps("gpy%d" % j, [128, 512], F32) for j in range(2)]

            def load_block(b):
                j = b % 2
                for (wt, src) in ((wgu[j], C.wgu_bf), (wd[j], C.wd_bf)):
                    o_ap = wt[:].re("p k n -> p (k n)").ap
                    s_ap = src[:].ap
                    d_ap = ridx_all[:, b:b + 1].ap
                    P.dma("gpsimd", wt[:], src[:], extra_reads=[ridx_all[:]],
                          fn=lambda e, o_ap=o_ap, s_ap=s_ap, d_ap=d_ap: e.indirect_dma_start(
                              out=o_ap, out_offset=None, in_=s_ap,
                              in_offset=bass.IndirectOffsetOnAxis(ap=d_ap, axis=0)))
                P.ts(sel[j][:], iop[:], be[0:N_EXP, b:b + 1], ALU.is_equal)

            def load_x(b):
                P.dma("sync", xs[b % 3][:], C.xblk[b * BLK:(b + 1) * BLK, :].re("(a p) d -> p a d", p=128))

            def prep_x(b):
                jj = b % 2
                for a in range(NA):
                    for kc in range(8):
                        P.tr(pT[:, kc, :], xs[b % 3][:, a, kc * 128:(kc + 1) * 128], C.ident_b[:])
                    P.copy(xTs[jj][:, :, a * 128:(a + 1) * 128], pT[:], eng="scalar")

            load_x(0)
            if NB > 1:
                load_x(1)
            load_block(0)
            prep_x(0)
            ny = 0
            for b in range(NB):
                if b + 2 < NB:
                    load_x(b + 2)
                if b + 1 < NB:
                    load_block(b + 1)
                j = b % 2
                xT = xTs[j]
                for m in range(8):
                    if m == 3 and b + 1 < NB:
                        prep_x(b + 1)
                    pgm = pg[m % 2]
                    pum = pu[m % 2]
                    for (ps, off) in ((pgm, 0), (pum, 1)):
                        for kc in range(8):
                            P.mm(ps[:, 0:BLK], wgu[j][:, kc, m * 256 + off:(m + 1) * 256:2], xT[:, kc, :],
                                 start=(kc == 0), stop=False)
                        P.mm(ps[:, 0:BLK], bgu[:, m * 256 + off:(m + 1) * 256:2], sel[j][:], start=False, stop=True)
                    P.ts(gg[:], pgm[:, 0:BLK], 7.0, ALU.min)
                    P.act(sg[:], gg[:], AF.Sigmoid, scale=1.702)
                    P.ts(uu[:], pum[:, 0:BLK], 7.0, ALU.min, -7.0, ALU.max)
                    P.stt(uu[:], uu[:], 1.0, gg[:], ALU.add, ALU.mult)
                    P.tt(actT[m][:], uu[:], sg[:], ALU.mult)
                for a in range(NA):
                    y = yb[ny % 2]
                    ny += 1
                    for n in range(2):
                        ps = py[n]
                        for m in range(8):
                            P.mm(ps[:], actT[m][:, a * 128:(a + 1) * 128], wd[j][:, m, n * 512:(n + 1) * 512],
                                 start=(m == 0), stop=False)
                        P.mm(ps[:], sel[j][:, a * 128:(a + 1) * 128], bd[:, n * 512:(n + 1) * 512], start=False, stop=True)
                        P.copy(y[:, n * 512:(n + 1) * 512], ps[:], eng=("scalar" if n else "vector"))
                    P.dma("sync", C.yblk.part((slice(b * BLK + a * 128, b * BLK + (a + 1) * 128), slice(None))), y[:])
        if stop <= 2:
            return
        with P.phase():
            rows = {}
            for r in sorted(set(1 if i < 2 else 0 for i in toks)):
                g2 = P.lsb("g2row%d" % r, [128, D], F32)
                bcast_rows(P, "sync", g2[:], C.modv[r:r + 1, 5 * D:6 * D])
                rows[r] = g2
            xat = [P.lsb("xat%d" % j, [128, D], F32) for j in range(2)]
            yk = [[P.lsb("yk%d_%d" % (j, k), [128, D], F32) for k in range(4)] for j in range(2)]
            f = P.lsb("hf_", [128, D], F32)
            xo = [P.lsb("xo%d" % j, [128, D], F32) for j in range(2)]
            def h_loads(n_):
                i = toks[n_]
                tk = slice(i * 128, (i + 1) * 128)
                j = n_ % 2
                P.dma("sync", xat[j][:], C.xa[tk, :])
                for k in range(4):
                    o_ap = yk[j][k][:].ap
                    s_ap = C.yblk[:].ap
                    d_ap = dest[:, i, k:k + 1].ap
                    P.dma("gpsimd", yk[j][k][:], C.yblk[:], extra_reads=[dest[:]],
                          fn=lambda e, o_ap=o_ap, s_ap=s_ap, d_ap=d_ap: e.indirect_dma_start(
                              out=o_ap, out_offset=None, in_=s_ap,
                              in_offset=bass.IndirectOffsetOnAxis(ap=d_ap, axis=0)))

            h_loads(0)
            for n_, i in enumerate(toks):
                r = 1 if i < 2 else 0
                tk = slice(i * 128, (i + 1) * 128)
                j = n_ % 2
                if n_ + 1 < len(toks):
                    h_loads(n_ + 1)
                P.ts(f[:], yk[j][0][:], gates[:, i, 0:1], ALU.mult)
                for k in range(1, 4):
                    P.stt(f[:], yk[j][k][:], gates[:, i, k:k + 1], f[:], ALU.mult, ALU.add)
                P.tt(f[:], f[:], rows[r][:], ALU.mult, eng="gpsimd")
                P.tt(xo[j][:], f[:], xat[j][:], ALU.add)
                if last:
                    P.dma("sync", C.out.part((slice((i - 2) * 128, (i - 1) * 128), slice(None))), xo[j][:])
                else:
                    P.dma("sync", C.xb.part((tk, slice(None))), xo[j][:])


def layer(P, C, l, stop=99):
    need_ctx = l < DEPTH - 1
    last = l == DEPTH - 1
    phase_A(P, C, l)
    phase_B(P, C, l)
    phase_C(P, C, l, need_ctx)
    phase_D(P, C, l, need_ctx)
    phase_E(P, C, l, need_ctx)
    toks = list(range(NT)) if need_ctx else list(range(2, NT))
    phase_MoE(P, C, l, toks, last, stop=stop)


def build_program(layers=(0, 1), ext_in=(), ext_out=(), skip=(), stop=99):
    nc = bass.Bass("TRN2", target_bir_lowering=False)
    with contextlib.ExitStack() as st:
        P = Prog(nc, st)
        C = Ctx()
        declare_io(P, C, ext_in=ext_in, ext_out=ext_out, skip=skip)
        load_consts(P, C)
        for l in layers:
            layer(P, C, l, stop=stop)
        outs = [C.out.all()] + [getattr(C, n).all() for n in ext_out]
        P.finish("sync", outs)
        P.barrier()
        P.emit()
        C.n_ops = P.n_ops
    return nc, C


def kernel(**inputs):
    inputs = {k: np.asarray(v) for k, v in inputs.items()}
    nb = inputs["x"].shape[0]
    nc, C = build_program()
    in_maps = []
    shared = None
    for b in range(nb):
        m = core_inputs(inputs, b)
        if shared is None:
            shared = m
        else:
            for k in m:
                if k not in ("x", "c", "ctx"):
                    m[k] = shared[k]
        in_maps.append({k: v for k, v in m.items() if k in C.in_names})
    res = run_bass_kernel_spmd(nc, in_maps, core_ids=list(range(nb)))
    out = np.stack([np.asarray(r["out"]) for r in res.results], axis=0)
    return out.astype(np.float32)
```

```python
import contextlib
import numpy as np
import concourse.bass as bass
import concourse.mybir as mybir
from concourse.bass_utils import run_bass_kernel_spmd
from concourse.alu_op_type import AluOpType as ALU

AF = mybir.ActivationFunctionType
AX = mybir.AxisListType
F32 = mybir.dt.float32
BF16 = mybir.dt.bfloat16
I32 = mybir.dt.int32
U32 = mybir.dt.uint32

EPOCH = 30000
N_DMA_SEMS = 24


class Dep:
    __slots__ = ("w", "r")

    def __init__(self):
        self.w = None
        self.r = {}


class V:
    __slots__ = ("ap", "deps")

    def __init__(self, ap, deps):
        self.ap = ap
        self.deps = deps

    def __getitem__(self, idx):
        return V(self.ap[idx], self.deps)

    def m(self, fn):
        return V(fn(self.ap), self.deps)

    def re(self, s, **kw):
        return V(self.ap.rearrange(s, **kw), self.deps)

    def bc(self, shape):
        return V(self.ap.broadcast_to(shape), self.deps)

    def bitcast(self, dt):
        return V(self.ap.bitcast(dt), self.deps)


class T:
    def __init__(self, handle, name):
        self.h = handle
        self.name = name
        self.dep = Dep()
        self.subs = {}

    def __getitem__(self, idx):
        return V(self.h[idx], [self.dep])

    def all(self):
        return V(self.h[:], [self.dep] + [s.dep for s in self.subs.values()])

    def part(self, idx):
        return V(self.h[idx], [Dep()])

    def sub(self, key):
        if key not in self.subs:
            self.subs[key] = T(self.h, "%s.%s" % (self.name, key))
        return self.subs[key]


class Prog:
    ENGS = ("tensor", "vector", "scalar", "gpsimd", "sync")

    def __init__(self, nc, stack):
        self.nc = nc
        self.stack = stack
        self.q = {e: [] for e in self.ENGS}
        self.cnt = {e: 0 for e in self.ENGS}
        self.seen = {e: {} for e in self.ENGS}
        self.sems = {}
        self.dma_sems = [stack.enter_context(nc.semaphore("dma%d" % i)) for i in range(2 * N_DMA_SEMS)]
        self.dma_cnt = [0] * (2 * N_DMA_SEMS)
        self.dma_rr = {False: 0, True: 0}
        self.n_ops = 0

    def sb(self, name, shape, dt):
        return T(self.stack.enter_context(self.nc.sbuf_tensor(name, list(shape), dt)), name)

    def ps(self, name, shape, dt=F32):
        return T(self.stack.enter_context(self.nc.psum_tensor(name, list(shape), dt)), name)

    def dram(self, name, shape, dt, kind="Internal"):
        return T(self.nc.dram_tensor(name, list(shape), dt, kind=kind), name)

    def _eng_sem(self, eng, epoch):
        k = (eng, epoch)
        if k not in self.sems:
            self.sems[k] = self.stack.enter_context(self.nc.semaphore("s_%s%d" % (eng, epoch)))
        return k

    def _sem_handle(self, key):
        if key[0] == "dma":
            return self.dma_sems[key[1]]
        return self.sems[key]

    def _collect(self, eng, reads, writes):
        need = {}

        def add(tok):
            if tok is None:
                return
            k, v = tok
            if need.get(k, 0) < v:
                need[k] = v

        for v_ in reads:
            for d in v_.deps:
                add(d.w)
        for v_ in writes:
            for d in v_.deps:
                add(d.w)
                for k, val in d.r.items():
                    add((k, val))
        out = []
        seen = self.seen[eng]
        for k, val in need.items():
            if eng == "tensor" and k[0] == "tensor":
                continue
            if seen.get(k, 0) >= val:
                continue
            seen[k] = val
            out.append((k, val))
        return out

    def _commit(self, tok, reads, writes):
        k, val = tok
        for v_ in reads:
            for d in v_.deps:
                if d.r.get(k, 0) < val:
                    d.r[k] = val
        for v_ in writes:
            for d in v_.deps:
                d.w = tok
                d.r = {}

    def op(self, eng, fn, reads=(), writes=()):
        waits = self._collect(eng, reads, writes)
        self.cnt[eng] += 1
        n = self.cnt[eng]
        epoch = (n - 1) // EPOCH
        k = self._eng_sem(eng, epoch)
        tok = (k, n - epoch * EPOCH)
        self.q[eng].append((waits, fn, k, 1))
        self._commit(tok, reads, writes)
        self.n_ops += 1
        return tok

    def dma(self, eng, out, in_, extra_reads=(), fn=None, **kw):
        reads = [in_] + list(extra_reads)
        writes = [out]
        waits = self._collect(eng, reads, writes)
        sw = (eng == "gpsimd")
        i = self.dma_rr[sw] + (N_DMA_SEMS if sw else 0)
        self.dma_rr[sw] = (self.dma_rr[sw] + 1) % N_DMA_SEMS
        k = ("dma", i)
        if self.dma_cnt[i] > 0 and self.seen[eng].get(k, 0) < self.dma_cnt[i]:
            self.seen[eng][k] = self.dma_cnt[i]
            waits.append((k, self.dma_cnt[i]))
        self.dma_cnt[i] += 16
        tok = (k, self.dma_cnt[i])
        if fn is None:
            o_ap, i_ap = out.ap, in_.ap

            def fn(e, o_ap=o_ap, i_ap=i_ap, kw=kw):
                return e.dma_start(out=o_ap, in_=i_ap, **kw)
        self.q[eng].append((waits, fn, k, 16))
        self._commit(tok, reads, writes)
        self.n_ops += 1
        return tok

    def finish(self, eng, views):
        waits = self._collect(eng, views, ())
        self.q[eng].append((waits, None, None, 0))

    def emit(self):
        nc = self.nc
        with nc.Block() as block:
            def mk(name):
                def body(e):
                    for waits, fn, k, inc in self.q[name]:
                        for wk, wv in waits:
                            e.wait_ge(self._sem_handle(wk), wv)
                        if fn is not None:
                            ins = fn(e)
                            ins.then_inc(self._sem_handle(k), inc)
                return body
            block.tensor(mk("tensor"))
            block.vector(mk("vector"))
            block.scalar(mk("scalar"))
            block.gpsimd(mk("gpsimd"))
            block.sync(mk("sync"))

    def mm(self, out, lhsT, rhs, start=True, stop=True, **kw):
        o, l, r = out.ap, lhsT.ap, rhs.ap
        return self.op("tensor", lambda e: e.matmul(o, l, r, start=start, stop=stop, **kw),
                       reads=[lhsT, rhs], writes=[out])

    def tr(self, out, in_, ident):
        o, i, d = out.ap, in_.ap, ident.ap
        return self.op("tensor", lambda e: e.transpose(o, i, d), reads=[in_, ident], writes=[out])

    def act(self, out, in_, func, bias=None, scale=None, accum_out=None, eng="scalar"):
        o, i = out.ap, in_.ap
        reads = [in_]
        writes = [out]
        kw = {}
        if bias is not None:
            if isinstance(bias, V):
                reads.append(bias)
                kw["bias"] = bias.ap
            else:
                kw["bias"] = bias
        if scale is not None:
            if isinstance(scale, V):
                reads.append(scale)
                kw["scale"] = scale.ap
            else:
                kw["scale"] = scale
        if accum_out is not None:
            writes.append(accum_out)
            kw["accum_out"] = accum_out.ap
        return self.op("scalar", lambda e: e.activation(o, i, func, **kw), reads=reads, writes=writes)

    def tt(self, out, in0, in1, op, eng="vector"):
        o, a, b = out.ap, in0.ap, in1.ap
        return self.op(eng, lambda e: e.tensor_tensor(out=o, in0=a, in1=b, op=op),
                       reads=[in0, in1], writes=[out])

    def ts(self, out, in0, s1, op0, s2=None, op1=None, accum_out=None, eng="vector"):
        o, a = out.ap, in0.ap
        reads = [in0]
        writes = [out]
        if isinstance(s1, V):
            reads.append(s1)
            s1 = s1.ap
        if isinstance(s2, V):
            reads.append(s2)
            s2 = s2.ap
        kw = {}
        if op1 is not None:
            kw["op1"] = op1
        if accum_out is not None:
            writes.append(accum_out)
            kw["accum_out"] = accum_out.ap
        return self.op(eng, lambda e: e.tensor_scalar(out=o, in0=a, scalar1=s1, scalar2=s2, op0=op0, **kw),
                       reads=reads, writes=writes)

    def stt(self, out, in0, scalar, in1, op0, op1, eng="vector"):
        o, a, b = out.ap, in0.ap, in1.ap
        reads = [in0, in1]
        if isinstance(scalar, V):
            reads.append(scalar)
            scalar = scalar.ap
        return self.op(eng, lambda e: e.scalar_tensor_tensor(out=o, in0=a, scalar=scalar, in1=b, op0=op0, op1=op1),
                       reads=reads, writes=[out])

    def copy(self, out, in_, eng="vector"):
        o, i = out.ap, in_.ap
        if eng == "scalar":
            return self.op(eng, lambda e: e.copy(o, i), reads=[in_], writes=[out])
        return self.op(eng, lambda e: e.tensor_copy(out=o, in_=i), reads=[in_], writes=[out])

    def memset(self, out, val, eng="vector"):
        o = out.ap
        return self.op(eng, lambda e: e.memset(o, val), writes=[out])

    def reduce(self, out, in_, op, axis=AX.X, eng="vector"):
        o, i = out.ap, in_.ap
        return self.op(eng, lambda e: e.tensor_reduce(out=o, in_=i, axis=axis, op=op), reads=[in_], writes=[out])

    def recip(self, out, in_):
        o, i = out.ap, in_.ap
        return self.op("vector", lambda e: e.reciprocal(out=o, in_=i), reads=[in_], writes=[out])

    def scan(self, out, d0, d1, initial, op0=ALU.mult, op1=ALU.add):
        o, a, b = out.ap, d0.ap, d1.ap
        reads = [d0, d1]
        if isinstance(initial, V):
            reads.append(initial)
            initial = initial.ap
        return self.op("vector", lambda e: e.tensor_tensor_scan(out=o, data0=a, data1=b, initial=initial, op0=op0, op1=op1),
                       reads=reads, writes=[out])


def _phase(self):
    prog = self

    class _Ph:
        def __enter__(s):
            s.old = getattr(prog, "stack_local", None)
            s.st = contextlib.ExitStack()
            s.st.__enter__()
            prog.stack_local = s.st
            return s

        def __exit__(s, *a):
            prog.barrier()
            prog.stack_local = s.old
            return s.st.__exit__(*a)
    return _Ph()


def _barrier(self):
    latest = {}
    for e in self.ENGS:
        n = self.cnt[e]
        if n > 0:
            epoch = (n - 1) // EPOCH
            latest[(e, epoch)] = n - epoch * EPOCH
    for i in range(2 * N_DMA_SEMS):
        if self.dma_cnt[i] > 0:
            latest[("dma", i)] = self.dma_cnt[i]
    for e in self.ENGS:
        waits = []
        seen = self.seen[e]
        for k, val in latest.items():
            if k[0] == e:
                continue
            if seen.get(k, 0) >= val:
                continue
            seen[k] = val
            waits.append((k, val))
        if waits:
            self.q[e].append((waits, None, None, 0))


def _lsb(self, name, shape, dt):
    self._uid = getattr(self, "_uid", 0) + 1
    nm = "%s_%d" % (name, self._uid)
    return T(self.stack_local.enter_context(self.nc.sbuf_tensor(nm, list(shape), dt)), nm)


def _lps(self, name, shape, dt=F32):
    self._uid = getattr(self, "_uid", 0) + 1
    nm = "%s_%d" % (name, self._uid)
    return T(self.stack_local.enter_context(self.nc.psum_tensor(nm, list(shape), dt)), nm)


Prog.phase = _phase
Prog.barrier = _barrier
Prog.lsb = _lsb
Prog.lps = _lps

D = 1024
SEQ = 4096
CTX = 256
TALL = SEQ + CTX
NT = TALL // 128
DEPTH = 2
D_IN = 3080
EPS = 1e-6
N_EXP = 32
BLK = 256
FM_COLS = [0, 128, 256, 384, 768, 896, 1024, 1152, 1280, 1408]
OFF_Z, OFF_DT, OFF_Q, OFF_K, OFF_V = 512, 1536, 1544, 2056, 2568


def drow(t, r0, n):
    return t[r0:r0 + n, :]


def bcast_rows(P, eng, dst, src_row_view):
    p = dst.ap.shape[0]
    n = dst.ap.shape[1]
    P.dma(eng, dst, src_row_view.m(lambda a: a.broadcast_to([p, n])))


class Ctx:
    pass


def declare_io(P, C, ext_in=(), ext_out=(), skip=()):
    C.in_names = []

    def inp(name, shape, dt=F32):
        if name in skip:
            return None
        C.in_names.append(name)
        return P.dram(name, shape, dt, kind="ExternalInput")

    def scr(name, shape, dt):
        kind = "Internal"
        if name in ext_in:
            kind = "ExternalInput"
        if name in ext_out:
            kind = "ExternalOutput"
        return P.dram(name, shape, dt, kind=kind)

    C.x = inp("x", [SEQ, D])
    C.c = inp("c", [1, D])
    C.ctx = inp("ctx", [CTX, D])
    C.c_ctx = inp("c_ctx", [1, D])
    C.w_mod = inp("w_mod", [DEPTH, D, 6 * D])
    C.b_mod = inp("b_mod", [DEPTH, 6 * D])
    C.norm1_g = inp("norm1_g", [DEPTH, D])
    C.norm2_g = inp("norm2_g", [DEPTH, D])
    C.w_in = inp("w_in", [DEPTH, D, D_IN])
    C.w_out = inp("w_out", [DEPTH, D, D])
    C.lru_conv_w = inp("lru_conv_w", [DEPTH, 4, 256])
    C.lru_conv_b = inp("lru_conv_b", [DEPTH, 256])
    C.lru_wa = inp("lru_wa", [DEPTH, 2, 4, 64, 64])
    C.lru_ba = inp("lru_ba", [DEPTH, 2, 256])
    C.lru_wx = inp("lru_wx", [DEPTH, 2, 4, 64, 64])
    C.lru_bx = inp("lru_bx", [DEPTH, 2, 256])
    C.lru_lam = inp("lru_lam", [DEPTH, 2, 256])
    C.ssd_conv_w = inp("ssd_conv_w", [DEPTH, 4, 768])
    C.ssd_conv_b = inp("ssd_conv_b", [DEPTH, 768])
    C.ssd_a_log = inp("ssd_a_log", [DEPTH, 8])
    C.ssd_dt_bias = inp("ssd_dt_bias", [DEPTH, 8])
    C.ssd_d = inp("ssd_d", [DEPTH, 4])
    C.ssd_norm_g = inp("ssd_norm_g", [DEPTH, 256])
    C.da_q_norm = inp("da_q_norm", [DEPTH, 64])
    C.da_k_norm = inp("da_k_norm", [DEPTH, 64])
    C.da_lam_q = inp("da_lam_q", [DEPTH, 128])
    C.da_lam_k = inp("da_lam_k", [DEPTH, 128])
    C.da_subln_g = inp("da_subln_g", [DEPTH, 128])
    C.router_w = inp("router_w", [DEPTH, D, N_EXP])
    C.router_b = inp("router_b", [DEPTH, N_EXP])
    C.exp_w_gu = inp("exp_w_gu", [DEPTH * N_EXP * D, 2 * D])
    C.exp_b_gu = inp("exp_b_gu", [DEPTH, N_EXP, 2 * D])
    C.exp_w_down = inp("exp_w_down", [DEPTH * N_EXP * D, D])
    C.exp_b_down = inp("exp_b_down", [DEPTH, N_EXP, D])
    C.k_ident = inp("k_ident", [128, 128])
    C.k_tri = inp("k_tri", [128, 128])
    C.k_triu = inp("k_triu", [128, 128])
    C.k_trils = inp("k_trils", [128, 128])
    C.k_negf = inp("k_negf", [128, 128])
    C.k_negb = inp("k_negb", [128, 128])
    C.k_cos = inp("k_cos", [128, SEQ // 128, 32])
    C.k_sin = inp("k_sin", [128, SEQ // 128, 32])
    C.k_iota32 = inp("k_iota32", [128, 32])
    C.k_iotap = inp("k_iotap", [128, 1])
    C.k_rowidx = inp("k_rowidx", [128, 8])
    C.k_iopb = inp("k_iopb", [128, BLK])
    C.k_thr = inp("k_thr", [128, (TALL * 4) // BLK + 1])
    C.k_bthr = inp("k_bthr", [128, (TALL * 4 + N_EXP * (BLK - 1)) // BLK])

    C.out = P.dram("out", [SEQ, D], F32, kind="ExternalOutput")
    C.modv = scr("modv", [2, 6 * D], F32)
    C.fm = scr("fm", [10 * 128, TALL], F32)
    C.qT = scr("qT", [4, 128, TALL], BF16)
    C.kT = scr("kT", [4, 128, TALL], BF16)
    C.vv = scr("vv", [TALL, 512], BF16)
    C.zz = scr("zz", [TALL, 256], F32)
    C.dtr = scr("dtr", [TALL, 8], F32)
    C.mixT = scr("mixT", [D, TALL], BF16)
    C.xa = scr("xa", [TALL, D], F32)
    C.xb = scr("xb", [TALL, D], F32)
    C.h2 = scr("h2", [TALL, D], BF16)
    nslots = ((TALL * 4 + N_EXP * (BLK - 1)) // BLK) * BLK
    C.nslots = nslots
    C.xblk = scr("xblk", [nslots, D], BF16)
    C.yblk = scr("yblk", [nslots, D], F32)
    C.wgu_bf = scr("wgu_bf", [N_EXP * 128, 8 * 2 * D], BF16)
    C.wd_bf = scr("wd_bf", [N_EXP * 128, 8 * D], BF16)


def load_consts(P, C):
    C.dt_sb = P.sb("dt_sb", [128, NT, 8], F32)
    C.ident_f = P.sb("ident_f", [128, 128], F32)
    C.ident_b = P.sb("ident_b", [128, 128], BF16)
    P.dma("sync", C.ident_f[:], C.k_ident[:])
    P.copy(C.ident_b[:], C.ident_f[:])


def xrows(C, l, i):
    if l == 0:
        if i < 2:
            return C.ctx[i * 128:(i + 1) * 128, :]
        return C.x[(i - 2) * 128:(i - 1) * 128, :]
    return C.xb[i * 128:(i + 1) * 128, :]


def phase_A(P, C, l):
    with P.phase():
        cc = P.lsb("cc", [128, 2, 8], F32)
        cs = P.lsb("cs", [128, 2, 8], F32)
        P.dma("sync", cc[:, 0, :], C.c[0:1, :].re("o (p k) -> (o p) k", k=8))
        P.dma("sync", cc[:, 1, :], C.c_ctx[0:1, :].re("o (p k) -> (o p) k", k=8))
        P.act(cs[:], cc[:], AF.Silu)
        bm = P.lsb("bm", [2, 6 * D], F32)
        P.dma("sync", bm[0:1, :], C.b_mod[l:l + 1, :])
        P.dma("sync", bm[1:2, :], C.b_mod[l:l + 1, :])
        ng = P.lsb("ng", [2, 2, D], F32)
        for r in range(2):
            P.dma("sync", ng[r:r + 1, 0, :], C.norm1_g[l:l + 1, :])
            P.dma("sync", ng[r:r + 1, 1, :], C.norm2_g[l:l + 1, :])
        mrow = P.lsb("mrow", [2, 6 * D], F32)
        wm = [P.lsb("wm%d" % j, [128, 8, 512], F32) for j in range(2)]
        pm = [P.lps("pm%d" % j, [2, 512], F32) for j in range(2)]
        wv = C.w_mod[l].re("(p k) n -> p k n", k=8)
        for j in range(12):
            w = wm[j % 2]
            P.dma("sync", w[:], wv[:, :, j * 512:(j + 1) * 512])
            ps = pm[j % 2]
            for k in range(8):
                P.mm(ps[:], cs[:, :, k], w[:, k, :], start=(k == 0), stop=(k == 7))
            P.tt(mrow[:, j * 512:(j + 1) * 512], ps[:], bm[:, j * 512:(j + 1) * 512], ALU.add)
        P.stt(mrow[:, D:2 * D], mrow[:, D:2 * D], 1.0, ng[:, 0, :], ALU.add, ALU.mult)
        P.stt(mrow[:, 4 * D:5 * D], mrow[:, 4 * D:5 * D], 1.0, ng[:, 1, :], ALU.add, ALU.mult)
        P.dma("sync", C.modv[:], mrow[:])


def rstd_from_ss(P, rstd, ss, n, tmp):
    P.ts(tmp, ss, 1.0 / n, ALU.mult, EPS, ALU.add)
    P.act(tmp, tmp, AF.Sqrt)
    P.recip(rstd, tmp)


B_MODE = 0
B_LANES = 2
B_CUT = 99


def run_lanes(tasks, nlanes=2):
    lanes = [None] * nlanes
    it = iter(tasks)
    pending = True
    while True:
        for li in range(nlanes):
            if lanes[li] is None and pending:
                f = next(it, None)
                if f is None:
                    pending = False
                else:
                    lanes[li] = f(li)
        if all(g is None for g in lanes):
            break
        for li in range(nlanes):
            g = lanes[li]
            if g is None:
                continue
            try:
                next(g)
            except StopIteration:
                lanes[li] = None


def phase_B(P, C, l):
    with P.phase():
        win = P.lsb("win", [128, 8, D_IN + 264], BF16)
        wv = C.w_in[l].re("(k p) n -> p k n", p=128)
        for kc in range(8):
            for ci, (c0, c1) in enumerate(((0, 1540), (1540, 3080))):
                P.dma("gpsimd", win[:, kc, c0:c1], wv[:, kc, c0:c1])
            P.dma("gpsimd", win[:, kc, D_IN:D_IN + 256], wv[:, kc, OFF_Z:OFF_Z + 256])
            P.dma("gpsimd", win[:, kc, D_IN + 256:D_IN + 264], wv[:, kc, OFF_DT:OFF_DT + 8])
        winv = win.all()
        rows = {}
        for r in range(2):
            sh = P.lsb("shrow%d" % r, [128, D], F32)
            sc = P.lsb("scrow%d" % r, [128, D], F32)
            bcast_rows(P, "sync", sh[:], C.modv[r:r + 1, 0:D])
            bcast_rows(P, "sync", sc[:], C.modv[r:r + 1, D:2 * D])
            rows[r] = (sh, sc)
        gq = P.lsb("gq", [128, 64], F32)
        gk = P.lsb("gk", [128, 64], F32)
        bcast_rows(P, "sync", gq[:], C.da_q_norm[l:l + 1, :])
        bcast_rows(P, "sync", gk[:], C.da_k_norm[l:l + 1, :])
        cos = P.lsb("cos", [128, SEQ // 128, 32], F32)
        sin = P.lsb("sin", [128, SEQ // 128, 32], F32)
        P.dma("sync", cos[:], C.k_cos[:])
        P.dma("sync", sin[:], C.k_sin[:])
        mhalf = P.lsb("bmhalf", [128, 8], F32)
        P.memset(mhalf[:], -0.5)

        NL = 2
        L = []
        for li in range(NL):
            d = dict(
                xt=P.lsb("xt%d" % li, [128, D], F32), junk=P.lsb("junk%d" % li, [128, D], F32),
                hf=P.lsb("hf%d" % li, [128, D], F32), hb=P.lsb("hb%d" % li, [128, D], BF16),
                ss=P.lsb("ss%d" % li, [128, 1], F32), t1=P.lsb("t1%d" % li, [128, 1], F32),
                rstd=P.lsb("rstd%d" % li, [128, 1], F32),
                qsq=P.lsb("qsq%d" % li, [128, 512], F32), qss=P.lsb("qss%d" % li, [128, 8], F32),
                qt8=P.lsb("qt8%d" % li, [128, 8], F32), qrs=P.lsb("qrs%d" % li, [128, 8], F32),
                qn=P.lsb("qn%d" % li, [128, 8, 64], F32), ra=P.lsb("ra%d" % li, [128, 8, 32], F32),
                rb=P.lsb("rb%d" % li, [128, 8, 32], F32), rc=P.lsb("rc%d" % li, [128, 8, 32], F32),
                rd=P.lsb("rd%d" % li, [128, 8, 32], F32), qr=P.lsb("qr%d" % li, [128, 8, 64], BF16),
                qTs=P.lsb("qTs%d" % li, [128, 4, 128], BF16), vb=P.lsb("vb%d" % li, [128, 512], BF16),
                zs=P.lsb("zs%d" % li, [128, 256], F32), dts=P.lsb("dts%d" % li, [128, 8], F32),
                pA=P.lps("pA%d" % li, [128, 8, 128], BF16), pB=P.lps("pB%d" % li, [128, 512], F32),
                pC=P.lps("pC%d" % li, [128, 512], F32))
            L.append(d)
        hTs = [P.lsb("hT%d" % j, [128, 8, 512], BF16) for j in range(2)]
        fms = [P.lsb("fms%d" % j, [128, 512], F32) for j in range(2)]
        pfm = [P.lps("pfm%d" % j, [128, 512], F32) for j in range(2)]

        def qk_post(d, psum, gain, dst, i, t0):
            latent = i >= 2
            qsq, qss, qt8, qrs, qn, ra, rb, rc, rd, qr, qTs, pA = (d[k] for k in (
                "qsq", "qss", "qt8", "qrs", "qn", "ra", "rb", "rc", "rd", "qr", "qTs", "pA"))
            P.act(qsq[:], psum[:], AF.Square)
            yield
            P.reduce(qss[:], qsq[:].re("p (g d) -> p g d", d=64), ALU.add)
            rstd_from_ss(P, qrs[:], qss[:], 64, qt8[:])
            yield
            P.tt(qn[:], psum[:].re("p (g d) -> p g d", d=64),
                 qrs[:].m(lambda a: a.unsqueeze(2).broadcast_to([128, 8, 64])), ALU.mult)
            gb = gain[:].m(lambda a: a.unsqueeze(1).broadcast_to([128, 8, 64]))
            yield
            if not latent:
                P.tt(qr[:], qn[:], gb, ALU.mult)
            else:
                P.tt(qn[:], qn[:], gb, ALU.mult)
                yield
                cb = cos[:, i - 2, :].m(lambda a: a.unsqueeze(1).broadcast_to([128, 8, 32]))
                sb_ = sin[:, i - 2, :].m(lambda a: a.unsqueeze(1).broadcast_to([128, 8, 32]))
                q1 = qn[:, :, 0:32]
                q2 = qn[:, :, 32:64]
                P.tt(ra[:], q1, cb, ALU.mult)
                P.tt(rc[:], q2, cb, ALU.mult, eng="gpsimd")
                yield
                P.tt(rb[:], q2, sb_, ALU.mult)
                P.tt(rd[:], q1, sb_, ALU.mult, eng="gpsimd")
                yield
                P.tt(qr[:, :, 0:32], ra[:], rb[:], ALU.subtract)
                P.tt(qr[:, :, 32:64], rc[:], rd[:], ALU.add, eng="gpsimd")
            yield
            qrf = qr[:].re("p g d -> p (g d)")
            for h in range(4):
                P.tr(pA[:, h, :], qrf[:, h * 128:(h + 1) * 128], C.ident_b[:])
            yield
            P.copy(qTs[:], pA[:, 0:4, :])
            P.dma("sync", dst.part(slice(None)).re("h p t -> p h t")[:, :, t0:t0 + 128], qTs[:])
            yield

        def tile_task(i, gi, j):
            def gen(li):
                d = L[li]
                r = 1 if i < 2 else 0
                sh, sc = rows[r]
                hT = hTs[gi % 2]
                t0 = i * 128
                x = d["xt"]
                P.dma("sync", x[:], xrows(C, l, i))
                P.act(d["junk"][:], x[:], AF.Square, accum_out=d["ss"][:])
                yield
                rstd_from_ss(P, d["rstd"][:], d["ss"][:], D, d["t1"][:])
                yield
                P.stt(d["hf"][:], x[:], d["rstd"][:], sc[:], ALU.mult, ALU.mult)
                yield
                P.tt(d["hb"][:], d["hf"][:], sh[:], ALU.add)
                yield
                for kc in range(8):
                    P.tr(d["pA"][:, kc, :], d["hb"][:, kc * 128:(kc + 1) * 128], C.ident_b[:])
                yield
                P.copy(hT[:, :, j * 128:(j + 1) * 128], d["pA"][:])
                yield
                hs = hT[:, :, j * 128:(j + 1) * 128]

                def proj(ps, c0, n, o0):
                    for kc in range(8):
                        P.mm(ps[:, o0:o0 + n], hs[:, kc, :], winv[:, kc, c0:c0 + n], start=(kc == 0), stop=(kc == 7))
                if B_CUT <= 1:
                    return
                proj(d["pB"], OFF_Q, 512, 0)
                yield
                proj(d["pC"], OFF_K, 512, 0)
                yield
                if B_CUT <= 2:
                    return
                for _ in qk_post(d, d["pB"], gq, C.qT, i, t0):
                    yield
                if B_CUT <= 3:
                    return
                proj(d["pB"], OFF_V, 512, 0)
                yield
                for _ in qk_post(d, d["pC"], gk, C.kT, i, t0):
                    yield
                if B_CUT <= 4:
                    return
                proj(d["pC"], D_IN, 264, 0)
                yield
                P.copy(d["vb"][:], d["pB"][:], eng="scalar")
                P.dma("sync", C.vv.part((slice(t0, t0 + 128), slice(None))), d["vb"][:])
                yield
                if B_CUT <= 5:
                    return
                P.act(d["zs"][:], d["pC"][:, 0:256], AF.Silu)
                P.dma("sync", C.zz.part((slice(t0, t0 + 128), slice(None))), d["zs"][:])
                if B_CUT <= 6:
                    return
                P.copy(C.dt_sb[:, i, :], d["pC"][:, 256:264], eng="scalar")
                yield
            return gen

        def fm_task(gi, grp):
            def gen(li):
                hT = hTs[gi % 2]
                ntok = 128 * len(grp)
                t0 = grp[0] * 128
                for ci, c0 in enumerate(FM_COLS):
                    ps = pfm[ci % 2]
                    for kc in range(8):
                        P.mm(ps[:, 0:ntok], winv[:, kc, c0:c0 + 128], hT[:, kc, 0:ntok], start=(kc == 0), stop=(kc == 7))
                    f = fms[ci % 2]
                    P.copy(f[:, 0:ntok], ps[:, 0:ntok], eng=("scalar" if ci % 2 else "vector"))
                    P.dma("sync", C.fm.part((slice(ci * 128, (ci + 1) * 128), slice(t0, t0 + ntok))), f[:, 0:ntok])
                    yield
            return gen

        groups = [[0, 1]] + [[2 + 4 * g + j for j in range(4)] for g in range(8)]
        tasks = []
        pending_fm = None
        for gi, grp in enumerate(groups):
            for j, i in enumerate(grp):
                tasks.append(tile_task(i, gi, j))
                if j == 1 and pending_fm is not None:
                    tasks.append(pending_fm)
                    pending_fm = None
            pending_fm = fm_task(gi, grp)
        def nop_task(li):
            return iter(())
        tasks.append(lambda li: iter(()))
        tasks.append(lambda li: iter(()))
        if B_MODE == 0:
            run_lanes(tasks, nlanes=2)
            run_lanes([pending_fm], nlanes=1)
        else:
            for gi, grp in enumerate(groups):
                run_lanes([tile_task(i, gi, j) for j, i in enumerate(grp)], nlanes=B_LANES)
                run_lanes([fm_task(gi, grp)], nlanes=1)


def host_consts():
    k = {}
    k["k_ident"] = np.eye(128, dtype=np.float32)
    a = np.arange(128)
    k["k_tri"] = (a[:, None] <= a[None, :]).astype(np.float32)
    k["k_triu"] = (a[:, None] >= a[None, :]).astype(np.float32)
    k["k_trils"] = (a[:, None] < a[None, :]).astype(np.float32)
    k["k_negf"] = np.where(a[None, :] >= a[:, None], 0.0, -30000.0).astype(np.float32)
    k["k_negb"] = np.where(a[None, :] <= a[:, None], 0.0, -30000.0).astype(np.float32)
    t = np.arange(SEQ)
    r = (t // 64).astype(np.float32)
    col = (t % 64).astype(np.float32)
    inv = (10000.0 ** (-np.arange(16, dtype=np.float32) / 16)).astype(np.float32)
    ang = np.concatenate([r[:, None] * inv[None, :], col[:, None] * inv[None, :]], axis=-1).astype(np.float32)
    cos = np.cos(ang).astype(np.float32).reshape(SEQ // 128, 128, 32).transpose(1, 0, 2)
    sin = np.sin(ang).astype(np.float32).reshape(SEQ // 128, 128, 32).transpose(1, 0, 2)
    k["k_cos"] = np.ascontiguousarray(cos)
    k["k_sin"] = np.ascontiguousarray(sin)
    k["k_iota32"] = np.tile(np.arange(32, dtype=np.float32)[None, :], (128, 1))
    k["k_iotap"] = np.arange(128, dtype=np.float32).reshape(128, 1)
    k["k_rowidx"] = (np.arange(8)[None, :] * 128 + np.arange(128)[:, None]).astype(np.float32)
    k["k_iopb"] = np.tile(np.arange(128, dtype=np.float32)[:, None], (1, BLK))
    k["k_thr"] = np.tile((np.arange((TALL * 4) // BLK + 1, dtype=np.float32) * BLK)[None, :], (128, 1))
    nbmax = (TALL * 4 + N_EXP * (BLK - 1)) // BLK
    k["k_bthr"] = np.tile((np.arange(nbmax, dtype=np.float32) * BLK)[None, :], (128, 1))
    return k


def core_inputs(inputs, b, big=True):
    m = {}
    m["x"] = np.ascontiguousarray(inputs["x"][b])
    m["c"] = np.ascontiguousarray(inputs["c"][b:b + 1])
    m["ctx"] = np.ascontiguousarray(inputs["ctx"][b])
    m["c_ctx"] = np.ascontiguousarray(inputs["c_ctx"].reshape(1, D))
    for k in ("w_mod", "b_mod", "norm1_g", "norm2_g", "w_in", "w_out", "lru_conv_w", "lru_conv_b",
              "lru_wa", "lru_ba", "lru_wx", "lru_bx", "lru_lam", "ssd_conv_w", "ssd_conv_b",
              "ssd_norm_g", "da_q_norm", "da_k_norm", "da_subln_g", "router_w", "router_b",
              "exp_b_gu", "exp_b_down", "ssd_d"):
        m[k] = np.ascontiguousarray(inputs[k])
    m["ssd_a_log"] = np.ascontiguousarray(inputs["ssd_a_log"].reshape(DEPTH, 8))
    m["ssd_dt_bias"] = np.ascontiguousarray(inputs["ssd_dt_bias"].reshape(DEPTH, 8))
    m["da_lam_q"] = np.ascontiguousarray(inputs["da_lam_q"].reshape(DEPTH, 128))
    m["da_lam_k"] = np.ascontiguousarray(inputs["da_lam_k"].reshape(DEPTH, 128))
    if big:
        m["exp_w_gu"] = inputs["exp_w_gu"].reshape(DEPTH * N_EXP * D, 2 * D)
        m["exp_w_down"] = inputs["exp_w_down"].reshape(DEPTH * N_EXP * D, D)
    m.update(host_consts())
    return m


def phase_C(P, C, l, need_ctx):
    with P.phase():
        LMAX = SEQ
        B = [P.lsb("lb%d" % j, [128, LMAX + 4], F32) for j in range(6)]
        xcb = P.lsb("xcb", [128, LMAX], BF16)
        yb = P.lsb("ylru", [128, LMAX], BF16)
        pg = [P.lps("pg%d" % j, [128, 512], F32) for j in range(2)]
        for ct in range(2):
            ch = slice(ct * 128, (ct + 1) * 128)
            cw = P.lsb("cw", [128, 4], F32)
            cbias = P.lsb("cbias", [128, 1], F32)
            P.dma("sync", cw[:], C.lru_conv_w[l][:, ch].re("j c -> c j"), allow_slow_non_contiguous=True)
            P.dma("sync", cbias[:], C.lru_conv_b[l:l + 1, ch].re("o c -> c o"), allow_slow_non_contiguous=True)
            wg = {}
            bg = {}
            sp = {}
            for d in range(2):
                for gi, (wsrc, bsrc) in enumerate(((C.lru_wa, C.lru_ba), (C.lru_wx, C.lru_bx))):
                    wf = P.lsb("wf", [128, 128], F32)
                    P.memset(wf[:], 0.0)
                    for hh in range(2):
                        P.dma("sync", wf[hh * 64:(hh + 1) * 64, hh * 64:(hh + 1) * 64],
                              wsrc[l][d][2 * ct + hh])
                    wb = P.lsb("wb", [128, 128], BF16)
                    P.copy(wb[:], wf[:])
                    wg[(d, gi)] = wb
                    bb = P.lsb("bb", [128, 1], F32)
                    P.dma("sync", bb[:], bsrc[l][d:d + 1, ch].re("o c -> c o"), allow_slow_non_contiguous=True)
                    bg[(d, gi)] = bb
                lam = P.lsb("lam", [128, 1], F32)
                P.dma("sync", lam[:], C.lru_lam[l][d:d + 1, ch].re("o c -> c o"), allow_slow_non_contiguous=True)
                e1 = P.lsb("e1", [128, 1], F32)
                P.act(e1[:], lam[:], AF.Exp, scale=-1.0)
                P.act(e1[:], e1[:], AF.Ln, bias=1.0)
                s8 = P.lsb("s8", [128, 1], F32)
                s16 = P.lsb("s16", [128, 1], F32)
                P.ts(s8[:], e1[:], -8.0, ALU.mult)
                P.ts(s16[:], e1[:], -16.0, ALU.mult)
                sp[d] = (s8, s16)
            h0 = {0: None, 1: None}
            hfin = [P.lsb("hfin%d" % d, [128, 1], F32) for d in range(2)]
            for (t0, L, is_ctx) in ((0, CTX, True), (CTX, SEQ, False)):
                xp, xc, b2, b3, b4, b5 = B
                P.memset(xp[:, 0:2], 0.0)
                P.memset(xp[:, L + 2:L + 4], 0.0)
                P.dma("sync", xp[:, 2:L + 2], C.fm[(2 + ct) * 128:(3 + ct) * 128, t0:t0 + L])
                P.ts(xc[:, 0:L], xp[:, 0:L], cw[:, 0:1], ALU.mult, cbias[:], ALU.add)
                for j in range(1, 4):
                    P.stt(xc[:, 0:L], xp[:, j:j + L], cw[:, j:j + 1], xc[:, 0:L], ALU.mult, ALU.add)
                P.copy(xcb[:, 0:L], xc[:, 0:L], eng="gpsimd")
                hs = {}
                for d in range(2):
                    if d == 0:
                        br, bi, ba = b2, b3, b4
                    else:
                        br, bi, ba = b3, b4, b5
                    nchunk = (L + 511) // 512
                    for gi, dst in ((0, br), (1, bi)):
                        for cix in range(nchunk):
                            n = min(512, L - cix * 512)
                            ps = pg[(cix + gi) % 2]
                            P.mm(ps[:, 0:n], wg[(d, gi)][:], xcb[:, cix * 512:cix * 512 + n])
                            P.act(dst[:, cix * 512:cix * 512 + n], ps[:, 0:n], AF.Sigmoid, bias=bg[(d, gi)][:])
                    s8, s16 = sp[d]
                    P.tt(bi[:, 0:L], bi[:, 0:L], xc[:, 0:L], ALU.mult, eng="gpsimd")
                    P.act(ba[:, 0:L], br[:, 0:L], AF.Exp, scale=s8[:])
                    P.act(br[:, 0:L], br[:, 0:L], AF.Exp, scale=s16[:])
                    P.act(br[:, 0:L], br[:, 0:L], AF.Sqrt, scale=-1.0, bias=1.0)
                    P.tt(bi[:, 0:L], bi[:, 0:L], br[:, 0:L], ALU.mult)
                    init = 0.0 if is_ctx else hfin[d][:]
                    if d == 0:
                        P.scan(br[:, 0:L], ba[:, 0:L], bi[:, 0:L], init)
                        if is_ctx:
                            P.copy(hfin[0][:], br[:, L - 1:L])
                    else:
                        P.scan(br[:, 0:L][:, ::-1], ba[:, 0:L][:, ::-1], bi[:, 0:L][:, ::-1], init)
                        if is_ctx:
                            P.copy(hfin[1][:], br[:, 0:1])
                    hs[d] = br
                if is_ctx and not need_ctx:
                    continue
                P.tt(b2[:, 0:L], b2[:, 0:L], b3[:, 0:L], ALU.add)
                P.dma("sync", b4[:, 0:L], C.fm[ct * 128:(ct + 1) * 128, t0:t0 + L])
                P.tt(b5[:, 0:L], b4[:, 0:L], b4[:, 0:L], ALU.mult, eng="gpsimd")
                P.ts(b5[:, 0:L], b5[:, 0:L], 0.044715, ALU.mult, 1.0, ALU.add)
                P.tt(b5[:, 0:L], b5[:, 0:L], b4[:, 0:L], ALU.mult, eng="gpsimd")
                P.act(b5[:, 0:L], b5[:, 0:L], AF.Sigmoid, scale=1.5957691216057308)
                P.tt(b4[:, 0:L], b4[:, 0:L], b5[:, 0:L], ALU.mult, eng="gpsimd")
                P.tt(yb[:, 0:L], b2[:, 0:L], b4[:, 0:L], ALU.mult)
                P.dma("sync", C.mixT.part((slice(ct * 128, (ct + 1) * 128), slice(t0, t0 + L))), yb[:, 0:L])


def phase_D(P, C, l, need_ctx, stop=99, nog=False):
    GP = "vector" if nog else "gpsimd"
    with P.phase():
        cv = [P.lsb("cv%d" % j, [128, TALL], BF16) for j in range(6)]
        with P.phase():
            xp = P.lsb("sxp", [128, SEQ + 4], F32)
            acc = P.lsb("sacc", [128, SEQ], F32)
            for j in range(6):
                ch = slice(j * 128, (j + 1) * 128)
                cw = P.lsb("scw", [128, 4], F32)
                cbias = P.lsb("scb", [128, 1], F32)
                P.dma("sync", cw[:], C.ssd_conv_w[l][:, ch].re("j c -> c j"), allow_slow_non_contiguous=True)
                P.dma("sync", cbias[:], C.ssd_conv_b[l:l + 1, ch].re("o c -> c o"), allow_slow_non_contiguous=True)
                for (t0, L) in ((0, CTX), (CTX, SEQ)):
                    P.memset(xp[:, 0:2], 0.0)
                    P.memset(xp[:, L + 2:L + 4], 0.0)
                    P.dma("sync", xp[:, 2:L + 2], C.fm[(4 + j) * 128:(5 + j) * 128, t0:t0 + L])
                    P.ts(acc[:, 0:L], xp[:, 0:L], cw[:, 0:1], ALU.mult, cbias[:], ALU.add)
                    for t in range(1, 4):
                        P.stt(acc[:, 0:L], xp[:, t:t + L], cw[:, t:t + 1], acc[:, 0:L], ALU.mult, ALU.add)
                    P.act(cv[j][:, t0:t0 + L], acc[:, 0:L], AF.Silu)
        if stop <= 1:
            return
        dt = P.lsb("dt", [128, NT, 8], F32)
        A = P.lsb("A", [128, NT, 8], F32)
        brow = P.lsb("brow", [128, 8], F32)
        arow = P.lsb("arow", [128, 8], F32)
        P.copy(dt[:], C.dt_sb[:], eng="gpsimd")
        bcast_rows(P, "sync", brow[:], C.ssd_dt_bias[l:l + 1, :])
        bcast_rows(P, "sync", arow[:], C.ssd_a_log[l:l + 1, :])
        P.tt(dt[:], dt[:], brow[:].m(lambda a: a.unsqueeze(1).broadcast_to([128, NT, 8])), ALU.add)
        P.act(dt[:], dt[:], AF.Exp)
        P.act(dt[:], dt[:], AF.Ln, bias=1.0)
        P.act(arow[:], arow[:], AF.Exp)
        P.ts(arow[:], arow[:], -1.0, ALU.mult)
        P.tt(A[:], dt[:], arow[:].m(lambda a: a.unsqueeze(1).broadcast_to([128, NT, 8])), ALU.mult)
        ones_f = P.lsb("ones_f", [128, 128], F32)
        P.memset(ones_f[:], 1.0)
        tri = [P.lsb("tri%d" % d, [128, 128], F32) for d in range(2)]
        neg = [P.lsb("neg%d" % d, [128, 128], F32) for d in range(2)]
        P.dma("sync", tri[0][:], C.k_tri[:])
        P.dma("sync", tri[1][:], C.k_triu[:])
        P.dma("sync", neg[0][:], C.k_negf[:])
        P.dma("sync", neg[1][:], C.k_negb[:])
        yacc = P.lsb("yacc", [128, NT, 256], F32)
        P.memset(yacc[:], 0.0, eng="gpsimd")
        xsave = P.lsb("xsave", [128, NT, 256], BF16)
        S = [P.lsb("S%d" % d, [128, 4, 64], F32) for d in range(2)]
        Sb = [P.lsb("Sb%d" % d, [128, 4, 64], BF16) for d in range(2)]
        DL = []
        for d in range(2):
            bankB = P.lps("dbB%d" % d, [128, 512], F32)
            bankC = P.lps("dbC%d" % d, [128, 512], F32)
            DL.append(dict(
                rA=P.lsb("rA%d" % d, [128, 4, 128], F32), tmp=P.lsb("stmp%d" % d, [128, 4, 128], F32),
                LT=P.lsb("LT%d" % d, [128, 4, 128], F32), EB=P.lsb("EB%d" % d, [128, 4, 128], F32),
                MT=P.lsb("MT%d" % d, [128, 4, 128], BF16), CTs=P.lsb("CTs%d" % d, [128, 4, 128], BF16),
                XB=P.lsb("XB%d" % d, [128, 4, 128], BF16), Xw=P.lsb("Xw%d" % d, [128, 4, 64], BF16),
                ncs=P.lsb("ncs%d" % d, [128, 4], F32), tot=P.lsb("tot%d" % d, [128, 4], F32),
                w=P.lsb("w%d" % d, [128, 4], F32),
                pcsB=P.lps("pcsB%d" % d, [128, 4, 128], F32),
                pGT=bankB[:, 0:256].re("p (g l) -> p g l", l=128),
                pst=bankB[:, 256:512].re("p (h c) -> p h c", c=64),
                py=bankC[:, 0:256].re("p (h c) -> p h c", c=64),
                pcs=bankC[:, 256:260],
                pXBt=P.lps("pXB%d" % d, [128, 8, 128], BF16)))
        pXB = DL[0]["pXBt"][:, 0:4, :]

        order = {0: list(range(NT)), 1: [1, 0] + list(range(NT - 1, 1, -1))}
        if stop <= 2:
            return
        for d in range(2):
            P.memset(S[d][:], 0.0)
            P.memset(Sb[d][:], 0.0)

        def sweep(d):
            L = DL[d]
            rA, tmp, LT, EB, MT, CTs, XB, Xw, ncs, tot, w = (L[k] for k in (
                "rA", "tmp", "LT", "EB", "MT", "CTs", "XB", "Xw", "ncs", "tot", "w"))
            pcsB, pGT, pst, py, pcs = L["pcsB"], L["pGT"], L["pst"], L["py"], L["pcs"]
            pXBl = L["pXBt"][:, 0:4, :]
            last = 127 if d == 0 else 0
            cols = slice(d * 4, d * 4 + 4)
            for i in order[d]:
                tk = slice(i * 128, (i + 1) * 128)
                need_y = (i >= 2) or need_ctx
                P.tt(rA[:], tri[d][:].m(lambda a: a.unsqueeze(1).broadcast_to([128, 4, 128])),
                     A[:, i, cols].m(lambda a: a.unsqueeze(2).broadcast_to([128, 4, 128])), ALU.mult, eng=GP)
                yield
                P.mm(pcsB[:].re("p h l -> p (h l)"), ones_f[:], rA[:].re("p h l -> p (h l)"))
                P.mm(pcs, tri[d][:], A[:, i, cols])
                yield
                P.ts(ncs[:], pcs, -1.0, ALU.mult)
                P.ts(EB[:], pcsB[:], -80.0, ALU.max)
                yield
                P.act(EB[:], EB[:], AF.Exp)
                P.copy(tot[:], pcsB[:, :, last])
                yield
                P.tt(w[:], ncs[:], tot[:], ALU.add)
                P.ts(w[:], w[:], -80.0, ALU.max)
                yield
                P.act(w[:], w[:], AF.Exp)
                for j in range(4):
                    P.tr(pXBl[:, j, :], cv[j][:, tk], C.ident_b[:])
                yield
                P.tt(w[:], w[:], dt[:, i, cols], ALU.mult)
                P.copy(XB[:], pXBl)
                Xv = XB[:, 0:2, :].re("p a (b c) -> p (a b) c", c=64)
                if d == 0:
                    P.copy(xsave[:, i, :], XB[:, 0:2, :].re("p a b -> p (a b)"), eng=GP)
                yield
                P.tt(Xw[:], Xv, w[:].m(lambda a: a.unsqueeze(2).broadcast_to([128, 4, 64])), ALU.mult)
                yield
                for h in range(4):
                    P.mm(pst[:, h, :], XB[:, 2 + h // 2, :], Xw[:, h, :])
                yield
                if need_y:
                    P.tt(tmp[:], pcsB[:], neg[d][:].m(lambda a: a.unsqueeze(1).broadcast_to([128, 4, 128])), ALU.add)
                    yield
                    for h in range(4):
                        P.ts(tmp[:, h, :], tmp[:, h, :], ncs[:, h:h + 1], ALU.add, -80.0, ALU.max)
                    yield
                    P.act(LT[:], tmp[:], AF.Exp)
                    for g in range(2):
                        P.mm(pGT[:, g, :], cv[2 + g][:, tk], cv[4 + g][:, tk])
                    yield
                    for h in range(4):
                        P.stt(MT[:, h, :], pGT[:, h // 2, :], dt[:, i, d * 4 + h:d * 4 + h + 1], LT[:, h, :],
                              ALU.mult, ALU.mult)
                    for g in range(2):
                        P.tt(CTs[:, 2 * g:2 * g + 2, :],
                             cv[4 + g][:, tk].m(lambda a: a.unsqueeze(1).broadcast_to([128, 2, 128])),
                             EB[:, 2 * g:2 * g + 2, :], ALU.mult, eng=GP)
                    yield
                    for h in range(4):
                        P.mm(py[:, h, :], MT[:, h, :], Xv[:, h, :], start=True, stop=False)
                        P.mm(py[:, h, :], CTs[:, h, :], Sb[d][:, h, :], start=False, stop=True)
                    yield
                    yv = yacc[:, i, :].re("p (h c) -> p h c", c=64)
                    P.tt(yv, yv, py, ALU.add)
                    yield
                for h in range(4):
                    P.stt(S[d][:, h, :], S[d][:, h, :], EB[:, h, last:last + 1], pst[:, h, :], ALU.mult, ALU.add)
                P.copy(Sb[d][:], S[d][:], eng=GP)
                yield

        run_lanes([lambda li: sweep(0), lambda li: sweep(1)], nlanes=2)
        if stop <= 3:
            return
        dsk = P.lsb("dsk", [128, 4], F32)
        bcast_rows(P, "sync", dsk[:], C.ssd_d[l:l + 1, :])
        gn = P.lsb("gn", [128, 256], F32)
        bcast_rows(P, "sync", gn[:], C.ssd_norm_g[l:l + 1, :])
        zt = [P.lsb("zt%d" % j, [128, 256], F32) for j in range(2)]
        t2 = P.lsb("t2", [128, 256], F32)
        junk = P.lsb("sjunk", [128, 256], F32)
        ss = P.lsb("sss", [128, 1], F32)
        t1 = P.lsb("st1", [128, 1], F32)
        rstd = P.lsb("srstd", [128, 1], F32)
        yo = P.lsb("yo", [128, 256], BF16)
        yT = P.lsb("yT", [128, 2, 128], BF16)
        for i in range(NT):
            if i < 2 and not need_ctx:
                continue
            z = zt[i % 2]
            P.dma("sync", z[:], C.zz[i * 128:(i + 1) * 128, :])
            P.tt(t2[:].re("p (h c) -> p h c", c=64), xsave[:, i, :].re("p (h c) -> p h c", c=64),
                 dsk[:].m(lambda a: a.unsqueeze(2).broadcast_to([128, 4, 64])), ALU.mult)
            P.tt(t2[:], t2[:], yacc[:, i, :], ALU.add)
            P.tt(t2[:], t2[:], z[:], ALU.mult)
            P.act(junk[:], t2[:], AF.Square, accum_out=ss[:])
            rstd_from_ss(P, rstd[:], ss[:], 256, t1[:])
            P.stt(yo[:], t2[:], rstd[:], gn[:], ALU.mult, ALU.mult)
            for j in range(2):
                P.tr(pXB[:, j, :], yo[:, j * 128:(j + 1) * 128], C.ident_b[:])
            P.copy(yT[:], pXB[:, 0:2, :])
            P.dma("sync", C.mixT.part((slice(256, 512), slice(i * 128, (i + 1) * 128))).re("(j p) t -> p j t", p=128), yT[:])


def phase_E(P, C, l, need_ctx):
    lam_init = 0.8 - 0.6 * float(np.exp(-0.3 * l))
    with P.phase():
        lq = P.lsb("lq", [128, 128], F32)
        lk = P.lsb("lk", [128, 128], F32)
        bcast_rows(P, "sync", lq[:], C.da_lam_q[l:l + 1, :])
        bcast_rows(P, "sync", lk[:], C.da_lam_k[l:l + 1, :])
        P.tt(lq[:], lq[:], lk[:], ALU.mult)
        l2 = P.lsb("l2", [128, 2], F32)
        P.reduce(l2[:], lq[:].re("p (a d) -> p a d", d=64), ALU.add)
        P.act(l2[:], l2[:], AF.Exp)
        nlam = P.lsb("nlam", [128, 1], F32)
        P.stt(nlam[:], l2[:, 1:2], -lam_init, l2[:, 0:1], ALU.add, ALU.subtract)
        gs = P.lsb("gs", [128, 128], F32)
        bcast_rows(P, "sync", gs[:], C.da_subln_g[l:l + 1, :])
        P.ts(gs[:], gs[:], 1.0 - lam_init, ALU.mult)

        KT = [P.lsb("KT%d" % j, [128, TALL], BF16) for j in range(2)]
        QT = [P.lsb("QT%d" % j, [128, 2, TALL], BF16) for j in range(2)]
        for j in range(2):
            P.memset(QT[j][64:128, 0, :], 0.0)
            P.memset(QT[j][0:64, 1, :], 0.0)
        VA = [[P.lsb("VA%d_%d" % (j, i), [128, 129], BF16) for i in range(NT)] for j in range(2)]
        for j in range(2):
            for i in range(NT):
                P.memset(VA[j][i][:, 128:129], 1.0, eng=("gpsimd" if i % 2 else "vector"))
        NBUF = 2
        Pt = [P.lsb("Pt%d" % j, [128, 2, 2, 256], BF16) for j in range(NBUF)]
        pS = [P.lps("pS%d" % j, [128, 2, 2, 256], F32) for j in range(NBUF)]
        pOb = [P.lps("pO%d" % q_, [128, 512], F32) for q_ in range(2)]
        pTr = P.lps("pTr", [128, 8, 128], BF16)
        pDum = P.lps("pDum", [128, 512], F32)
        mhalf = P.lsb("mhalf", [128, 1], F32)
        P.memset(mhalf[:], -0.5)
        oacc = [P.lsb("oacc%d" % j, [128, 4, 129], F32) for j in range(2)]
        fin = []
        for j in range(4):
            fin.append(dict(
                r1=P.lsb("r1_%d" % j, [128, 1], F32), r2=P.lsb("r2_%d" % j, [128, 1], F32),
                o1=P.lsb("o1_%d" % j, [128, 128], F32), o2=P.lsb("o2_%d" % j, [128, 128], F32),
                junk=P.lsb("aj_%d" % j, [128, 128], F32), ss=P.lsb("ass_%d" % j, [128, 1], F32),
                t1=P.lsb("at1_%d" % j, [128, 1], F32), rstd=P.lsb("ars_%d" % j, [128, 1], F32),
                ob=P.lsb("aob_%d" % j, [128, 128], BF16), oT=P.lsb("aoT_%d" % j, [128, 128], BF16)))

        def load_head(h):
            j = h % 2
            P.dma("sync", KT[j][:], C.kT[h])
            P.dma("sync", QT[j][0:64, 0, :], C.qT[h][0:64, :])
            P.dma("sync", QT[j][64:128, 1, :], C.qT[h][64:128, :])
            for i in range(NT):
                P.dma("sync", VA[j][i][:, 0:128], C.vv[i * 128:(i + 1) * 128, h * 128:(h + 1) * 128])

        conv = []
        for e in range(N_EXP if C.exp_w_gu is not None else 0):
            r0 = (l * N_EXP + e) * D
            for kc in range(8):
                conv.append((C.wgu_bf.part((slice(e * 128, (e + 1) * 128), slice(kc * 2 * D, (kc + 1) * 2 * D))),
                             C.exp_w_gu[r0 + kc * 128:r0 + (kc + 1) * 128, :]))
            for kc in range(8):
                conv.append((C.wd_bf.part((slice(e * 128, (e + 1) * 128), slice(kc * D, (kc + 1) * D))),
                             C.exp_w_down[r0 + kc * 128:r0 + (kc + 1) * 128, :]))
        conv_pos = [0]

        def trickle(n):
            for _ in range(n):
                if conv_pos[0] < len(conv):
                    o, i_ = conv[conv_pos[0]]
                    conv_pos[0] += 1
                    P.dma("gpsimd", o, i_)

        load_head(0)
        nfin = 0
        for h in range(4):
            if h + 1 < 4:
                load_head(h + 1)
            j = h % 2
            chunks = []
            if need_ctx:
                chunks.append((0, [0, 1]))
            for qc in range(SEQ // 256):
                chunks.append((CTX + qc * 256, list(range(NT))))
            its = []
            for ci, (q0, kts) in enumerate(chunks):
                for ki in range(0, len(kts), 2):
                    its.append((ci, q0, ki // 2, (kts[ki], kts[ki + 1]), len(kts) // 2))

            def issue_S(n):
                ci, q0, ki, ktp, nk = its[n]
                ps = pS[n % NBUF]
                for t_, kt in enumerate(ktp):
                    ks = slice(kt * 128, (kt + 1) * 128)
                    for s_ in range(2):
                        P.mm(ps[:, t_, s_, :], KT[j][:, ks], QT[j][:, s_, q0:q0 + 256])

            deferred = []
            issue_S(0)
            for n, (ci, q0, ki, ktp, nk) in enumerate(its):
                if n + 1 < len(its):
                    issue_S(n + 1)
                ps = pS[n % NBUF]
                pt = Pt[n % NBUF]
                for _ in range(N_DUMMY):
                    P.mm(pDum[:, 0:128], C.ident_b[:], C.ident_b[:])
                P.act(pt[:], ps[:], AF.Exp, scale=0.125)
                for t_, kt in enumerate(ktp):
                    for q_ in range(2):
                        for s_ in range(2):
                            first = (ki == 0 and t_ == 0)
                            P.mm(pOb[q_][:, s_ * 256:s_ * 256 + 129], pt[:, t_, s_, q_ * 128:(q_ + 1) * 128], VA[j][kt][:],
                                 start=(first and s_ == 0), stop=(ki == nk - 1 and t_ == 1),
                                 skip_group_check=True)
                if n % 3 == 0:
                    trickle(1)
                if ki == nk - 1:
                    oa = oacc[ci % 2]
                    for s_ in range(2):
                        for q_ in range(2):
                            P.copy(oa[:, s_ * 2 + q_, :], pOb[q_][:, s_ * 256:s_ * 256 + 129])
                    for q_ in range(2):
                        f = fin[nfin % 4]
                        nfin += 1
                        a1 = oa[:, q_, :]
                        a2 = oa[:, 2 + q_, :]
                        G = "gpsimd"
                        P.recip(f["r1"][:], a1[:, 128:129])
                        P.recip(f["r2"][:], a2[:, 128:129])
                        P.tt(f["r2"][:], f["r2"][:], nlam[:], ALU.mult)
                        P.ts(f["o1"][:], a1[:, 0:128], f["r1"][:], ALU.mult, eng=G)
                        P.stt(f["o2"][:], a2[:, 0:128], f["r2"][:], f["o1"][:], ALU.mult, ALU.add)
                        P.tt(f["junk"][:], f["o2"][:], f["o2"][:], ALU.mult, eng=G)
                        P.reduce(f["ss"][:], f["junk"][:], ALU.add)
                        P.ts(f["t1"][:], f["ss"][:], 1.0 / 128, ALU.mult, EPS, ALU.add)
                        P.tt(f["rstd"][:], f["t1"][:], mhalf[:], ALU.pow, eng=G)
                        P.stt(f["ob"][:], f["o2"][:], f["rstd"][:], gs[:], ALU.mult, ALU.mult)
                        tq = q0 + q_ * 128

                        def late(f=f, tq=tq, h=h):
                            P.tr(pTr[:, 0, :], f["ob"][:], C.ident_b[:])
                            P.copy(f["oT"][:], pTr[:, 0, :])
                            P.dma("sync", C.mixT.part((slice(512 + h * 128, 512 + (h + 1) * 128), slice(tq, tq + 128))),
                                  f["oT"][:])
                        deferred.append((n + 12 + q_, late))
                while deferred and deferred[0][0] <= n:
                    deferred.pop(0)[1]()
            for _, fn_ in deferred:
                fn_()
        trickle(len(conv))


N_DUMMY = 0
NBMAX = (TALL * 4 + N_EXP * (BLK - 1)) // BLK
JMAX = (TALL * 4) // BLK + 1


def phase_MoE(P, C, l, toks, last, stop=99):
    ntok = len(toks) * 128
    NB = (ntok * 4 + N_EXP * (BLK - 1)) // BLK
    NA = BLK // 128
    with P.phase():
        gates = P.lsb("gates", [128, NT, 4], F32)
        dest = P.lsb("dest", [128, NT, 4], I32)
        be = P.lsb("be", [128, NBMAX], F32)
        berow = P.lsb("berow", [128, NBMAX], F32)
        with P.phase():
            wout = P.lsb("wout", [128, 8, D], BF16)
            wov = C.w_out[l].re("(k p) n -> p k n", p=128)
            for kc in range(8):
                P.dma("gpsimd", wout[:, kc, :], wov[:, kc, :])
            rows = {}
            for r in sorted(set(1 if i < 2 else 0 for i in toks)):
                g1 = P.lsb("g1row%d" % r, [128, D], F32)
                s2 = P.lsb("s2row%d" % r, [128, D], F32)
                h2r = P.lsb("h2row%d" % r, [128, D], F32)
                bcast_rows(P, "sync", g1[:], C.modv[r:r + 1, 2 * D:3 * D])
                bcast_rows(P, "sync", h2r[:], C.modv[r:r + 1, 3 * D:4 * D])
                bcast_rows(P, "sync", s2[:], C.modv[r:r + 1, 4 * D:5 * D])
                rows[r] = (g1, s2, h2r)
            rw = P.lsb("rw", [128, 8, N_EXP], F32)
            P.dma("sync", rw[:], C.router_w[l].re("(k p) e -> p k e", p=128))
            rb = P.lsb("rb", [128, N_EXP], F32)
            bcast_rows(P, "sync", rb[:], C.router_b[l:l + 1, :])
            iota = P.lsb("iota", [128, N_EXP], F32)
            P.dma("sync", iota[:], C.k_iota32[:])
            trils = P.lsb("trils", [128, 128], F32)
            P.dma("sync", trils[:], C.k_trils[:])
            ones_f = P.lsb("ones_f2", [128, 128], F32)
            P.memset(ones_f[:], 1.0)
            OH = P.lsb("OH", [128, NT, 4, N_EXP], F32)
            POS = P.lsb("POS", [128, NT, N_EXP], F32)
            Acc = P.lsb("Acc", [128, N_EXP], F32)
            P.memset(Acc[:], 0.0)
            Ai = P.lsb("Ai", [128, N_EXP], F32)
            FL = []
            for li in range(2):
                FL.append(dict(
                    mx=P.lsb("mx%d" % li, [128, 8, 128], BF16), xt=P.lsb("fxt%d" % li, [128, D], F32),
                    tmp=P.lsb("ftmp%d" % li, [128, D], F32), xn=P.lsb("fxn%d" % li, [128, D], F32),
                    junk=P.lsb("fjunk%d" % li, [128, D], F32), h2f=P.lsb("h2f%d" % li, [128, D], F32),
                    h2b=P.lsb("h2b%d" % li, [128, D], BF16), h2T=P.lsb("h2T%d" % li, [128, 8, 128], F32),
                    ss=P.lsb("fss%d" % li, [128, 1], F32), t1=P.lsb("ft1%d" % li, [128, 1], F32),
                    rstd=P.lsb("frstd%d" % li, [128, 1], F32), lg=P.lsb("lg%d" % li, [128, N_EXP], F32),
                    t8=P.lsb("t8%d" % li, [128, 8], F32), i8=P.lsb("i8%d" % li, [128, 8], U32),
                    idxf=P.lsb("idxf%d" % li, [128, 4], F32), e4=P.lsb("e4%d" % li, [128, 4], F32),
                    es=P.lsb("es%d" % li, [128, 1], F32), Ai=P.lsb("Ai%d" % li, [128, N_EXP], F32),
                    po=[P.lps("po%d_%d" % (li, j), [128, 512], F32) for j in range(2)],
                    pTf=P.lps("pTf%d" % li, [128, 4, 128], F32), plp=P.lps("plp%d" % li, [128, 512], F32)))
            fmhalf = P.lsb("fmhalf", [128, 1], F32)
            P.memset(fmhalf[:], -0.5)

            def f_task(i):
                def gen(li):
                    d = FL[li]
                    r = 1 if i < 2 else 0
                    g1, s2, h2r = rows[r]
                    tk = slice(i * 128, (i + 1) * 128)
                    m_, x, tmp, xn, h2f, h2b, h2T = d["mx"], d["xt"], d["tmp"], d["xn"], d["h2f"], d["h2b"], d["h2T"]
                    po, pTf = d["po"], d["pTf"]
                    pl = d["plp"][:, 0:N_EXP]
                    ppos = d["plp"][:, 64:64 + N_EXP]
                    lg, t8, i8, idxf, e4, es, Ai = d["lg"], d["t8"], d["i8"], d["idxf"], d["e4"], d["es"], d["Ai"]
                    P.dma("sync", m_[:], C.mixT[:, tk].re("(k p) t -> p k t", p=128))
                    P.dma("sync", x[:], xrows(C, l, i))
                    for n in range(2):
                        for kc in range(8):
                            P.mm(po[n][:], m_[:, kc, :], wout[:, kc, n * 512:(n + 1) * 512], start=(kc == 0), stop=(kc == 7))
                        yield
                    for n in range(2):
                        P.tt(tmp[:, n * 512:(n + 1) * 512], po[n][:], g1[:, n * 512:(n + 1) * 512], ALU.mult)
                    yield
                    P.tt(xn[:], tmp[:], x[:], ALU.add, eng="gpsimd")
                    P.dma("sync", C.xa.part((tk, slice(None))), xn[:])
                    yield
                    P.act(d["junk"][:], xn[:], AF.Square, accum_out=d["ss"][:])
                    yield
                    P.ts(d["t1"][:], d["ss"][:], 1.0 / D, ALU.mult, EPS, ALU.add)
                    P.tt(d["rstd"][:], d["t1"][:], fmhalf[:], ALU.pow, eng="gpsimd")
                    yield
                    P.stt(tmp[:], xn[:], d["rstd"][:], s2[:], ALU.mult, ALU.mult)
                    yield
                    P.tt(h2f[:], tmp[:], h2r[:], ALU.add)
                    yield
                    P.copy(h2b[:], h2f[:], eng="gpsimd")
                    P.dma("sync", C.h2.sub(i)[tk, :], h2b[:])
                    for hh in range(2):
                        for kc in range(4):
                            P.tr(pTf[:, kc, :], h2f[:, (hh * 4 + kc) * 128:(hh * 4 + kc + 1) * 128], C.ident_f[:])
                        yield
                        P.copy(h2T[:, hh * 4:(hh + 1) * 4, :], pTf[:], eng="scalar")
                        yield
                    for kc in range(8):
                        P.mm(pl, h2T[:, kc, :], rw[:, kc, :], start=(kc == 0), stop=(kc == 7))
                    yield
                    P.tt(lg[:], pl, rb[:], ALU.add)
                    yield
                    t8a, i8a, lga = t8[:].ap, i8[:].ap, lg[:].ap
                    P.op("vector", lambda e, t8a=t8a, lga=lga: e.max(out=t8a, in_=lga), reads=[lg[:]], writes=[t8[:]])
                    P.op("vector", lambda e, t8a=t8a, lga=lga, i8a=i8a: e.max_index(out=i8a, in_max=t8a, in_values=lga),
                         reads=[lg[:], t8[:]], writes=[i8[:]])
                    yield
                    P.ts(e4[:], t8[:, 0:4], t8[:, 0:1], ALU.subtract)
                    P.act(e4[:], e4[:], AF.Exp, accum_out=es[:])
                    yield
                    P.recip(es[:], es[:])
                    P.ts(gates[:, i, :], e4[:], es[:], ALU.mult)
                    yield
                    P.copy(idxf[:], i8[:, 0:4])
                    for k in range(4):
                        P.ts(OH[:, i, k, :], iota[:], idxf[:, k:k + 1], ALU.is_equal)
                    yield
                    P.reduce(Ai[:], OH[:, i, :, :].re("p k e -> p e k"), ALU.add)
                    yield
                    P.mm(ppos, trils[:], Ai[:], start=True, stop=False)
                    P.mm(ppos, ones_f[:], Acc[:], start=False, stop=True)
                    P.copy(POS[:, i, :], ppos)
                    P.tt(Acc[:], Acc[:], Ai[:], ALU.add)
                    yield
                return gen

            run_lanes([f_task(i) for i in toks], nlanes=2)
            ppos_ = FL[0]["plp"]
            ppos = ppos_[:, 64:64 + N_EXP]
            cnt = P.lsb("cnt", [128, N_EXP], F32)
            P.mm(ppos, ones_f[:], Acc[:])
            P.copy(cnt[:], ppos)
            thr = P.lsb("thr", [128, JMAX], F32)
            P.dma("sync", thr[:], C.k_thr[:])
            cmpt = P.lsb("cmpt", [128, N_EXP, JMAX], F32)
            P.tt(cmpt[:], cnt[:].m(lambda a: a.unsqueeze(2).broadcast_to([128, N_EXP, JMAX])),
                 thr[:].m(lambda a: a.unsqueeze(1).broadcast_to([128, N_EXP, JMAX])), ALU.is_gt)
            padded = P.lsb("padded", [128, N_EXP], F32)
            P.reduce(padded[:], cmpt[:], ALU.add)
            P.ts(padded[:], padded[:], float(BLK), ALU.mult)
            pend = P.lsb("pend", [128, N_EXP], F32)
            ones32 = P.lsb("ones32", [128, N_EXP], F32)
            P.memset(ones32[:], 1.0)
            P.scan(pend[:], ones32[:], padded[:], 0.0)
            pstart = P.lsb("pstart", [128, N_EXP], F32)
            P.tt(pstart[:], pend[:], padded[:], ALU.subtract)
            bthr = P.lsb("bthr", [128, NBMAX], F32)
            P.dma("sync", bthr[:], C.k_bthr[:])
            cmpb = P.lsb("cmpb", [128, NBMAX, N_EXP], F32)
            P.tt(cmpb[:], pend[:].m(lambda a: a.unsqueeze(1).broadcast_to([128, NBMAX, N_EXP])),
                 bthr[:].m(lambda a: a.unsqueeze(2).broadcast_to([128, NBMAX, N_EXP])), ALU.is_le)
            P.reduce(be[:], cmpb[:], ALU.add)
            P.ts(be[:], be[:], float(N_EXP - 1), ALU.min)
            P.ts(berow[:], be[:], 128.0, ALU.mult)
            zt = P.lsb("zfill", [128, 8192], BF16)
            P.memset(zt[:], 0.0)
            per = NB * BLK * D // 128
            xv = C.xblk[0:NB * BLK, :].re("(p a) d -> p (a d)", p=128)
            for c0 in range(0, per, 8192):
                n = min(8192, per - c0)
                P.dma("sync", xv[:, c0:c0 + n], zt[:, 0:n])
            base = P.lsb("base", [128, N_EXP], F32)
            prod = P.lsb("prod", [128, 4, N_EXP], F32)
            destf = P.lsb("destf", [128, 4], F32)
            h2t = [P.lsb("h2t%d" % j, [128, D], BF16) for j in range(2)]
            for n_, i in enumerate(toks):
                tk = slice(i * 128, (i + 1) * 128)
                P.tt(base[:], pstart[:], POS[:, i, :], ALU.add)
                P.tt(prod[:], OH[:, i, :, :], base[:].m(lambda a: a.unsqueeze(1).broadcast_to([128, 4, N_EXP])), ALU.mult)
                P.reduce(destf[:], prod[:], ALU.add)
                P.copy(dest[:, i, :], destf[:])
                ht = h2t[n_ % 2]
                P.dma("sync", ht[:], C.h2.sub(i)[tk, :])
                for k in range(4):
                    o_ap = C.xblk[:].ap
                    i_ap = ht[:].ap
                    d_ap = dest[:, i, k:k + 1].ap
                    P.dma("gpsimd", C.xblk.part(slice(None)), ht[:], extra_reads=[dest[:], C.xblk[:]],
                          fn=lambda e, o_ap=o_ap, i_ap=i_ap, d_ap=d_ap: e.indirect_dma_start(
                              out=o_ap, out_offset=bass.IndirectOffsetOnAxis(ap=d_ap, axis=0), in_=i_ap, in_offset=None))
        if stop <= 1:
            return
        with P.phase():
            wgu = [P.lsb("wgu%d" % j, [128, 8, 2 * D], BF16) for j in range(2)]
            wd = [P.lsb("wd%d" % j, [128, 8, D], BF16) for j in range(2)]
            ridx_all = P.lsb("ridx_all", [128, NBMAX], I32)
            ridf = P.lsb("ridf", [128, NBMAX], F32)
            rowidx = P.lsb("rowidx", [128, 1], F32)
            P.dma("sync", rowidx[:], C.k_iotap[:])
            P.ts(ridf[:], berow[:], rowidx[:, 0:1], ALU.add)
            P.copy(ridx_all[:], ridf[:])
            bgu = P.lsb("bgu", [128, 2 * D], BF16)
            bd = P.lsb("bd", [128, D], BF16)
            P.memset(bgu[:], 0.0)
            P.memset(bd[:], 0.0, eng="gpsimd")
            P.dma("gpsimd", bgu[0:N_EXP, :], C.exp_b_gu[l])
            P.dma("gpsimd", bd[0:N_EXP, :], C.exp_b_down[l])
            iop = P.lsb("iop", [128, BLK], F32)
            P.dma("sync", iop[:], C.k_iopb[:])
            sel = [P.lsb("sel%d" % j, [128, BLK], BF16) for j in range(2)]
            xs = [P.lsb("xs%d" % j, [128, NA, D], BF16) for j in range(3)]
            xTs = [P.lsb("xT%d" % j, [128, 8, BLK], BF16) for j in range(2)]
            actT = [P.lsb("actT%d" % m, [128, BLK], BF16) for m in range(8)]
            gg = P.lsb("gg", [128, BLK], F32)
            sg = P.lsb("sg", [128, BLK], F32)
            uu = P.lsb("uu", [128, BLK], F32)
            yb = [P.lsb("yb%d" % j, [128, D], F32) for j in range(2)]
            pT = P.lps("gpT", [128, 8, 128], BF16)
            pg = [P.lps("gpg%d" % j, [128, 512], F32) for j in range(2)]
            pu = [P.lps("gpu%d" % j, [128, 512], F32) for j in range(2)]
            py = [P.lps("gpy%d" % j, [128, 512], F32) for j in range(2)]

            def load_block(b):
                j = b % 2
                for (wt, src) in ((wgu[j], C.wgu_bf), (wd[j], C.wd_bf)):
                    o_ap = wt[:].re("p k n -> p (k n)").ap
                    s_ap = src[:].ap
                    d_ap = ridx_all[:, b:b + 1].ap
                    P.dma("gpsimd", wt[:], src[:], extra_reads=[ridx_all[:]],
                          fn=lambda e, o_ap=o_ap, s_ap=s_ap, d_ap=d_ap: e.indirect_dma_start(
                              out=o_ap, out_offset=None, in_=s_ap,
                              in_offset=bass.IndirectOffsetOnAxis(ap=d_ap, axis=0)))
                P.ts(sel[j][:], iop[:], be[:, b:b + 1], ALU.is_equal)

            def load_x(b):
                P.dma("sync", xs[b % 3][:], C.xblk[b * BLK:(b + 1) * BLK, :].re("(a p) d -> p a d", p=128))

            def prep_x(b):
                jj = b % 2
                for a in range(NA):
                    for kc in range(8):
                        P.tr(pT[:, kc, :], xs[b % 3][:, a, kc * 128:(kc + 1) * 128], C.ident_b[:])
                    P.copy(xTs[jj][:, :, a * 128:(a + 1) * 128], pT[:], eng="scalar")

            load_x(0)
            if NB > 1:
                load_x(1)
            load_block(0)
            prep_x(0)
            ny = 0
            for b in range(NB):
                if b + 2 < NB:
                    load_x(b + 2)
                if b + 1 < NB:
                    load_block(b + 1)
                j = b % 2
                xT = xTs[j]
                for m in range(8):
                    if m == 3 and b + 1 < NB:
                        prep_x(b + 1)
                    pgm = pg[m % 2]
                    pum = pu[m % 2]
                    for (ps, off) in ((pgm, 0), (pum, 1)):
                        for kc in range(8):
                            P.mm(ps[:, 0:BLK], wgu[j][:, kc, m * 256 + off:(m + 1) * 256:2], xT[:, kc, :],
                                 start=(kc == 0), stop=False)
                        P.mm(ps[:, 0:BLK], bgu[:, m * 256 + off:(m + 1) * 256:2], sel[j][:], start=False, stop=True)
                    P.ts(gg[:], pgm[:, 0:BLK], 7.0, ALU.min)
                    P.act(sg[:], gg[:], AF.Sigmoid, scale=1.702)
                    P.ts(uu[:], pum[:, 0:BLK], 7.0, ALU.min, -7.0, ALU.max)
                    P.stt(uu[:], uu[:], 1.0, gg[:], ALU.add, ALU.mult)
                    P.tt(actT[m][:], uu[:], sg[:], ALU.mult)
                for a in range(NA):
                    y = yb[ny % 2]
                    ny += 1
                    for n in range(2):
                        ps = py[n]
                        for m in range(8):
                            P.mm(ps[:], actT[m][:, a * 128:(a + 1) * 128], wd[j][:, m, n * 512:(n + 1) * 512],
                                 start=(m == 0), stop=False)
                        P.mm(ps[:], sel[j][:, a * 128:(a + 1) * 128], bd[:, n * 512:(n + 1) * 512], start=False, stop=True)
                        P.copy(y[:, n * 512:(n + 1) * 512], ps[:], eng=("scalar" if n else "vector"))
                    P.dma("sync", C.yblk.part((slice(b * BLK + a * 128, b * BLK + (a + 1) * 128), slice(None))), y[:])
        if stop <= 2:
            return
        with P.phase():
            rows = {}
            for r in sorted(set(1 if i < 2 else 0 for i in toks)):
                g2 = P.lsb("g2row%d" % r, [128, D], F32)
                bcast_rows(P, "sync", g2[:], C.modv[r:r + 1, 5 * D:6 * D])
                rows[r] = g2
            xat = [P.lsb("xat%d" % j, [128, D], F32) for j in range(2)]
            yk = [[P.lsb("yk%d_%d" % (j, k), [128, D], F32) for k in range(4)] for j in range(2)]
            f = P.lsb("hf_", [128, D], F32)
            xo = [P.lsb("xo%d" % j, [128, D], F32) for j in range(2)]
            def h_loads(n_):
                i = toks[n_]
                tk = slice(i * 128, (i + 1) * 128)
                j = n_ % 2
                P.dma("sync", xat[j][:], C.xa[tk, :])
                for k in range(4):
                    o_ap = yk[j][k][:].ap
                    s_ap = C.yblk[:].ap
                    d_ap = dest[:, i, k:k + 1].ap
                    P.dma("gpsimd", yk[j][k][:], C.yblk[:], extra_reads=[dest[:]],
                          fn=lambda e, o_ap=o_ap, s_ap=s_ap, d_ap=d_ap: e.indirect_dma_start(
                              out=o_ap, out_offset=None, in_=s_ap,
                              in_offset=bass.IndirectOffsetOnAxis(ap=d_ap, axis=0)))

            h_loads(0)
            for n_, i in enumerate(toks):
                r = 1 if i < 2 else 0
                tk = slice(i * 128, (i + 1) * 128)
                j = n_ % 2
                if n_ + 1 < len(toks):
                    h_loads(n_ + 1)
                P.ts(f[:], yk[j][0][:], gates[:, i, 0:1], ALU.mult)
                for k in range(1, 4):
                    P.stt(f[:], yk[j][k][:], gates[:, i, k:k + 1], f[:], ALU.mult, ALU.add)
                P.tt(f[:], f[:], rows[r][:], ALU.mult, eng="gpsimd")
                P.tt(xo[j][:], f[:], xat[j][:], ALU.add)
                if last:
                    P.dma("sync", C.out.part((slice((i - 2) * 128, (i - 1) * 128), slice(None))), xo[j][:])
                else:
                    P.dma("sync", C.xb.part((tk, slice(None))), xo[j][:])


def layer(P, C, l, stop=99):
    need_ctx = l < DEPTH - 1
    last = l == DEPTH - 1
    phase_A(P, C, l)
    phase_B(P, C, l)
    phase_C(P, C, l, need_ctx)
    phase_D(P, C, l, need_ctx)
    phase_E(P, C, l, need_ctx)
    toks = list(range(NT)) if need_ctx else list(range(2, NT))
    phase_MoE(P, C, l, toks, last, stop=stop)


def build_program(layers=(0, 1), ext_in=(), ext_out=(), skip=(), stop=99):
    nc = bass.Bass("TRN2", target_bir_lowering=False)
    with contextlib.ExitStack() as st:
        P = Prog(nc, st)
        C = Ctx()
        declare_io(P, C, ext_in=ext_in, ext_out=ext_out, skip=skip)
        load_consts(P, C)
        for l in layers:
            layer(P, C, l, stop=stop)
        outs = [C.out.all()] + [getattr(C, n).all() for n in ext_out]
        P.finish("sync", outs)
        P.barrier()
        P.emit()
        C.n_ops = P.n_ops
    return nc, C


def kernel(**inputs):
    inputs = {k: np.asarray(v) for k, v in inputs.items()}
    nb = inputs["x"].shape[0]
    nc, C = build_program()
    in_maps = []
    shared = None
    for b in range(nb):
        m = core_inputs(inputs, b)
        if shared is None:
            shared = m
        else:
            for k in m:
                if k not in ("x", "c", "ctx"):
                    m[k] = shared[k]
        in_maps.append({k: v for k, v in m.items() if k in C.in_names})
    res = run_bass_kernel_spmd(nc, in_maps, core_ids=list(range(nb)))
    out = np.stack([np.asarray(r["out"]) for r in res.results], axis=0)
    return out.astype(np.float32)
```
